# Optimizing a Trainium2 kernel written in Bass

```python
import jax, jax.numpy as jnp
from jax import lax
import numpy as np

D_MODEL = 1024
BATCH = 2
SEQ = 8192
DEPTH = 1

GRID_W = 64
CTX_LEN = 256
D_MIX = D_MODEL
NA_WIDTH = D_MIX // 2
NA_HEADS = 8
NA_HEAD_DIM = NA_WIDTH // NA_HEADS
NA_KH_MAX = 8
NA_KW = 16
SGU_WIDTH = D_MIX - NA_WIDTH
SGU_GROUPS = 4
SGU_GROUP_DIM = SGU_WIDTH // SGU_GROUPS
CHUNK = 128
N_EXPERTS = 32
TOP_K = 4
D_FF_EXPERT = D_MODEL
SWIGLU_LIMIT = 7.0
SWIGLU_ALPHA = 1.702
LN_EPS = 1e-5
DEEPNORM_ALPHA = (2.0 * DEPTH) ** 0.25
DEEPNORM_BETA = (8.0 * DEPTH) ** -0.25
Q_OFF = 0
K_OFF = NA_WIDTH
V_OFF = 2 * NA_WIDTH
U_OFF = 3 * NA_WIDTH
D_IN = U_OFF + 2 * SGU_WIDTH

kernel_name = "hybrid_natten_sgu_moe_dit_block"


def layer_norm(x, g=None, b=None):
    xf = x.astype(jnp.float32)
    mu = jnp.mean(xf, axis=-1, keepdims=True)
    var = jnp.mean(jnp.square(xf - mu), axis=-1, keepdims=True)
    y = (xf - mu) * lax.rsqrt(var + LN_EPS)
    if g is not None:
        y = y * g.astype(jnp.float32) + b.astype(jnp.float32)
    return y.astype(x.dtype)


def ada_params(cond, ada_w, ada_b):
    m = jax.nn.silu(cond) @ ada_w + ada_b
    return jnp.split(m, 6, axis=-1)


def modulate(h, shift, scale):
    return layer_norm(h) * (1 + scale) + shift


def split_heads(t):
    b, l, _ = t.shape
    return t.reshape(b, l, NA_HEADS, NA_HEAD_DIM)


def neighbourhood_attention(q, k, v, k_ctx, v_ctx, rpb):
    b, s, h, dh = q.shape
    rows = s // GRID_W
    kh = min(NA_KH_MAX, rows)
    kw = NA_KW
    scale = dh ** -0.5
    qg = q.reshape(b, rows, GRID_W, h, dh)
    kg = k.reshape(b, rows, GRID_W, h, dh)
    vg = v.reshape(b, rows, GRID_W, h, dh)
    col = jnp.arange(GRID_W)
    col_start = jnp.clip(col - kw // 2, 0, GRID_W - kw)
    col_idx = col_start[:, None] + jnp.arange(kw)[None, :]
    dc_idx = col_idx - col[:, None] + (NA_KW - 1)

    def row_block(r):
        rs = jnp.clip(r - kh // 2, 0, rows - kh)
        k_rows = lax.dynamic_slice_in_dim(kg, rs, kh, axis=1)
        v_rows = lax.dynamic_slice_in_dim(vg, rs, kh, axis=1)
        k_nb = k_rows[:, :, col_idx]
        v_nb = v_rows[:, :, col_idx]
        q_r = lax.dynamic_index_in_dim(qg, r, axis=1, keepdims=False)
        dr_idx = rs + jnp.arange(kh) - r + (NA_KH_MAX - 1)
        bias = rpb[:, dr_idx[None, :, None], dc_idx[:, None, :]]
        s_nb = jnp.einsum('bwhd,biwjhd->bhwij', q_r, k_nb).astype(jnp.float32) * scale \
            + bias.astype(jnp.float32)
        s_ctx = jnp.einsum('bwhd,bchd->bhwc', q_r, k_ctx).astype(jnp.float32) * scale
        scores = jnp.concatenate([s_nb.reshape(b, h, GRID_W, kh * kw), s_ctx], axis=-1)
        p = jax.nn.softmax(scores, axis=-1).astype(q.dtype)
        p_nb = p[..., :kh * kw].reshape(b, h, GRID_W, kh, kw)
        p_ctx = p[..., kh * kw:]
        return jnp.einsum('bhwij,biwjhd->bwhd', p_nb, v_nb) + jnp.einsum('bhwc,bchd->bwhd', p_ctx, v_ctx)

    out = lax.map(row_block, jnp.arange(rows))
    return jnp.moveaxis(out, 0, 1).reshape(b, s, h * dh)


def context_attention(q, k, v):
    b, l, h, dh = q.shape
    s = jnp.einsum('bqhd,bkhd->bhqk', q, k).astype(jnp.float32) * (dh ** -0.5)
    p = jax.nn.softmax(s, axis=-1).astype(q.dtype)
    return jnp.einsum('bhqk,bkhd->bqhd', p, v).reshape(b, l, h * dh)


def spatial_gating(z, ln_g, ln_b, w_s, b_s):
    b, l, _ = z.shape
    u, g = jnp.split(z, 2, axis=-1)
    g = layer_norm(g, ln_g, ln_b)
    g = g.reshape(b, l // CHUNK, CHUNK, SGU_GROUPS, SGU_GROUP_DIM)
    g = jnp.einsum('gij,bnjgc->bnigc', w_s, g) + b_s.T[None, None, :, :, None]
    return u * g.reshape(b, l, SGU_WIDTH)


def moe_ffn(h, router_w, router_b, w_gu, b_gu, w_down, b_down):
    shp = h.shape
    t = h.reshape(-1, shp[-1])
    logits = (t @ router_w + router_b).astype(jnp.float32)
    top_vals, top_idx = lax.top_k(logits, TOP_K)
    gates = jax.nn.softmax(top_vals, axis=-1)
    combine = jnp.sum(jax.nn.one_hot(top_idx, N_EXPERTS, dtype=jnp.float32) * gates[..., None],
                      axis=1).astype(t.dtype)

    def expert_step(acc, xs):
        wgu, bgu, wd, bd, wgt = xs
        gu = t @ wgu + bgu
        gate = jnp.minimum(gu[:, :D_FF_EXPERT], SWIGLU_LIMIT)
        lin = jnp.clip(gu[:, D_FF_EXPERT:], -SWIGLU_LIMIT, SWIGLU_LIMIT)
        y = ((lin + 1) * (gate * jax.nn.sigmoid(SWIGLU_ALPHA * gate))) @ wd + bd
        return acc + wgt[:, None] * y, None

    out, _ = lax.scan(expert_step, jnp.zeros_like(t), (w_gu, b_gu, w_down, b_down, combine.T))
    return out.reshape(shp)


def hybrid_layer(x, ctx, c, c_ctx, ada_w, ada_b, w_in, rpb, sgu_ln_g, sgu_ln_b, sgu_w, sgu_b,
                 w_out, ln1_g, ln1_b, ln2_g, ln2_b, router_w, router_b,
                 exp_w_gu, exp_b_gu, exp_w_down, exp_b_down, last):
    sh1, sc1, g1, sh2, sc2, g2 = ada_params(c[:, None, :], ada_w, ada_b)
    csh1, csc1, cg1, csh2, csc2, cg2 = ada_params(c_ctx, ada_w, ada_b)

    hc = modulate(ctx, csh1, csc1)
    if last:
        kv_c = hc @ w_in[:, K_OFF:U_OFF]
        k_c, v_c = split_heads(kv_c[..., :NA_WIDTH]), split_heads(kv_c[..., NA_WIDTH:])
    else:
        proj_c = hc @ w_in
        q_c = split_heads(proj_c[..., Q_OFF:K_OFF])
        k_c = split_heads(proj_c[..., K_OFF:V_OFF])
        v_c = split_heads(proj_c[..., V_OFF:U_OFF])
        att_c = context_attention(q_c, k_c, v_c)
        sgu_c = spatial_gating(jax.nn.gelu(proj_c[..., U_OFF:]), sgu_ln_g, sgu_ln_b, sgu_w, sgu_b)
        mix_c = jnp.concatenate([att_c, sgu_c], axis=-1) @ w_out

    hx = modulate(x, sh1, sc1)
    proj_x = hx @ w_in
    q_x = split_heads(proj_x[..., Q_OFF:K_OFF])
    k_x = split_heads(proj_x[..., K_OFF:V_OFF])
    v_x = split_heads(proj_x[..., V_OFF:U_OFF])
    att_x = neighbourhood_attention(q_x, k_x, v_x, k_c, v_c, rpb)
    sgu_x = spatial_gating(jax.nn.gelu(proj_x[..., U_OFF:]), sgu_ln_g, sgu_ln_b, sgu_w, sgu_b)
    mix_x = jnp.concatenate([att_x, sgu_x], axis=-1) @ w_out
    x = layer_norm(DEEPNORM_ALPHA * x + g1 * mix_x, ln1_g, ln1_b)
    ffn_x = moe_ffn(modulate(x, sh2, sc2), router_w, router_b, exp_w_gu, exp_b_gu, exp_w_down, exp_b_down)
    x = layer_norm(DEEPNORM_ALPHA * x + g2 * ffn_x, ln2_g, ln2_b)

    if not last:
        ctx = layer_norm(DEEPNORM_ALPHA * ctx + cg1 * mix_c, ln1_g, ln1_b)
        ffn_c = moe_ffn(modulate(ctx, csh2, csc2), router_w, router_b, exp_w_gu, exp_b_gu,
                        exp_w_down, exp_b_down)
        ctx = layer_norm(DEEPNORM_ALPHA * ctx + cg2 * ffn_c, ln2_g, ln2_b)
    return x, ctx


def setup_inputs(seed: int = 0) -> dict:
    key = jax.random.key(seed)
    ks = jax.random.split(key, 24)
    f32 = jnp.float32

    def nrm(k, shape, std):
        return jax.random.normal(k, shape, f32) * std

    L = DEPTH
    return {
        "x": nrm(ks[0], (BATCH, SEQ, D_MODEL), 1.0),
        "c": nrm(ks[1], (BATCH, D_MODEL), 1.0),
        "ctx": nrm(ks[2], (BATCH, CTX_LEN, D_MODEL), 1.0),
        "c_ctx": nrm(ks[3], (D_MODEL,), 1.0),
        "ada_w": nrm(ks[4], (L, D_MODEL, 6 * D_MODEL), 0.5 * D_MODEL ** -0.5),
        "ada_b": nrm(ks[5], (L, 6 * D_MODEL), 0.02),
        "w_in": nrm(ks[6], (L, D_MODEL, D_IN), D_MODEL ** -0.5),
        "rpb": nrm(ks[7], (L, NA_HEADS, 2 * NA_KH_MAX - 1, 2 * NA_KW - 1), 0.1),
        "sgu_ln_g": 1.0 + nrm(ks[8], (L, SGU_WIDTH), 0.01),
        "sgu_ln_b": nrm(ks[9], (L, SGU_WIDTH), 0.01),
        "sgu_w": nrm(ks[10], (L, SGU_GROUPS, CHUNK, CHUNK), CHUNK ** -0.5),
        "sgu_b": 1.0 + nrm(ks[11], (L, SGU_GROUPS, CHUNK), 0.1),
        "w_out": nrm(ks[12], (L, D_MIX, D_MODEL), DEEPNORM_BETA * D_MIX ** -0.5),
        "ln1_g": 1.0 + nrm(ks[13], (L, D_MODEL), 0.01),
        "ln1_b": nrm(ks[14], (L, D_MODEL), 0.01),
        "ln2_g": 1.0 + nrm(ks[15], (L, D_MODEL), 0.01),
        "ln2_b": nrm(ks[16], (L, D_MODEL), 0.01),
        "router_w": nrm(ks[17], (L, D_MODEL, N_EXPERTS), D_MODEL ** -0.5),
        "router_b": nrm(ks[18], (L, N_EXPERTS), 0.01),
        "exp_w_gu": nrm(ks[19], (L, N_EXPERTS, D_MODEL, 2 * D_FF_EXPERT), D_MODEL ** -0.5),
        "exp_b_gu": nrm(ks[20], (L, N_EXPERTS, 2 * D_FF_EXPERT), 0.01),
        "exp_w_down": nrm(ks[21], (L, N_EXPERTS, D_FF_EXPERT, D_MODEL), DEEPNORM_BETA * D_FF_EXPERT ** -0.5),
        "exp_b_down": nrm(ks[22], (L, N_EXPERTS, D_MODEL), 0.01),
    }


def reference(x, c, ctx, c_ctx, ada_w, ada_b, w_in, rpb, sgu_ln_g, sgu_ln_b, sgu_w, sgu_b,
              w_out, ln1_g, ln1_b, ln2_g, ln2_b, router_w, router_b,
              exp_w_gu, exp_b_gu, exp_w_down, exp_b_down):
    for i in range(DEPTH):
        x, ctx = hybrid_layer(
            x, ctx, c, c_ctx, ada_w[i], ada_b[i], w_in[i], rpb[i], sgu_ln_g[i], sgu_ln_b[i],
            sgu_w[i], sgu_b[i], w_out[i], ln1_g[i], ln1_b[i], ln2_g[i], ln2_b[i],
            router_w[i], router_b[i], exp_w_gu[i], exp_b_gu[i], exp_w_down[i], exp_b_down[i],
            last=(i == DEPTH - 1))
    return x
```

```python
import numpy as np
import ml_dtypes
from contextlib import ExitStack
import concourse.bass as bass
import concourse.mybir as mybir
from concourse.bass_utils import run_bass_kernel_spmd

F32 = mybir.dt.float32
BF16 = mybir.dt.bfloat16
I32 = mybir.dt.int32
AF = mybir.ActivationFunctionType
ALU = mybir.AluOpType
AX = mybir.AxisListType

NCORES = 8
D = 1024
NEXT_T = 20
NT_ALL = 22
OWN0 = 2
NOWN = 16
NTOK = NOWN * 128
NEG = -30000.0
ALPHA = 2.0 ** 0.25
LN_EPS = 1e-5
NE = 32
TS = 384
NSUB = TS // 128
NTILE = (4 * 2048 + NE * (TS - 1)) // TS
NSLOT = NTILE * TS
C_J = 288
C_E = C_J + NTILE
C_PK = C_E + NE
C_BIG = C_PK + 8
C_P = C_BIG + 1
CW = 384
assert C_P < CW
BIGIDX = 4.0e6
SPECIAL = {0: (0, 12), 1: (2, 10), 14: (28, 9), 15: (28, 11)}
SP_ORDER = [0, 1, 14, 15]
PAIR_ORDER = [0, 2, 3, 4, 1, 5, 6, 7, 8, 14, 9, 10, 11, 15, 12, 13]


class Eng:
    def __init__(s, es, nc, eng, name):
        s.eng = eng
        s.name = name
        s.sem = es.enter_context(nc.semaphore("sem_" + name))
        s.n = 0
        s.seen = {}

    def wait(s, toks):
        for t in toks:
            if t is None:
                continue
            e, c = t
            if s.seen.get(e, 0) < c:
                s.eng.wait_ge(e.sem, c)
                s.seen[e] = c

    def fin(s, inst):
        inst.then_inc(s.sem, 1)
        s.n += 1
        return (s, s.n)

    def last(s):
        return (s, s.n) if s.n > 0 else None


class DSem:
    def __init__(s, es, nc, name):
        s.sem = es.enter_context(nc.semaphore("dsem_" + name))
        s.n = 0


def flat(deps):
    out = []
    for d in deps:
        if d is None:
            continue
        if isinstance(d, list):
            out.extend(flat(d))
        else:
            out.append(d)
    return out


class Ring:
    def __init__(s, bufs):
        s.bufs = bufs
        s.free = [None] * len(bufs)
        s.i = 0

    def next(s):
        i = s.i % len(s.bufs)
        s.i += 1
        return i, s.bufs[i], s.free[i]

    def rel(s, i, tok):
        s.free[i] = tok


def build(debug=False):
    nc = bass.Bass("TRN2", target_bir_lowering=False)

    def din(name, shape):
        return nc.dram_tensor(name, shape, F32, kind="ExternalInput").ap()

    xe = din("xe", [NT_ALL * 128, D])
    cT = din("cT", [128, 16])
    ada_w = din("ada_w", [D, 6 * D])
    adabT = din("adabT", [128, 48])
    ada_b = din("ada_b", [6 * D])
    w_in = din("w_in", [D, 2560])
    w_out = din("w_out", [D, D])
    bias_gen = din("bias_gen", [128, 8 * 832])
    bias_sp = din("bias_sp", [4, 128, 8 * 1024])
    sgu_ln_g = din("sgu_ln_g", [512])
    sgu_ln_b = din("sgu_ln_b", [512])
    wsT = din("wsT", [128, 512])
    sgu_bv = din("sgu_bv", [512])
    ln1_g = din("ln1_g", [D])
    ln1_b = din("ln1_b", [D])
    ln2_g = din("ln2_g", [D])
    ln2_b = din("ln2_b", [D])
    router_w = din("router_w", [D, NE])
    router_b = din("router_b", [NE])
    exp_w_gu = din("exp_w_gu", [NE, D, 2 * D])
    exp_w_down = din("exp_w_down", [NE, D, D])
    exp_b_down = din("exp_b_down", [NE, D])
    ident = din("ident", [128, 128])
    cst = din("cst", [128, CW])
    zeros = nc.dram_tensor("zeros", [512, D], BF16, kind="ExternalInput").ap()
    bgu_rows = din("bgu_rows", [NE * 128, 16])
    out = nc.dram_tensor("out", [NTOK, D], F32, kind="ExternalOutput").ap()
    Hx = nc.dram_tensor("Hx", [NTOK, D], BF16, kind="Internal").ap()
    Hs = nc.dram_tensor("Hs", [NSLOT, D], BF16, kind="Internal").ap()
    Ys = nc.dram_tensor("Ys", [NSLOT, D], F32, kind="Internal").ap()
    accd = nc.dram_tensor("accd", [NTOK, D], F32, kind="Internal").ap()
    wgu_rows = exp_w_gu.rearrange("e r n -> (e r) n")
    wd_rows = exp_w_down.rearrange("e r n -> (e r) n")
    dbg = {}
    if debug:
        for nm, shp in [("d_KT", [128, 4 * 2816]), ("d_V", [128, 22 * 512]), ("d_QT", [128, 4 * 2048]),
                        ("d_sguT", [128, 4 * 2048]), ("d_attT", [128, 4 * 2048]), ("d_acc", [128, 16 * 1024]),
                        ("d_h2T", [128, 8 * 2048]), ("d_comb", [128, 16 * 32]), ("d_ada", [128, 96]),
                        ("d_g1bc", [128, 1024])]:
            dbg[nm] = nc.dram_tensor(nm, shp, F32, kind="ExternalOutput").ap()

    with ExitStack() as es:
        PE = Eng(es, nc, nc.tensor, "pe")
        ACT = Eng(es, nc, nc.scalar, "act")
        DVE = Eng(es, nc, nc.vector, "dve")
        POOL = Eng(es, nc, nc.gpsimd, "pool")
        SP = Eng(es, nc, nc.sync, "sp")
        ENGS = [PE, ACT, DVE, POOL, SP]
        nds = [0]

        def dsem():
            nds[0] += 1
            return DSem(es, nc, "d%d" % nds[0])

        def op(E, deps, fn, *a, **k):
            E.wait(flat(deps))
            return E.fin(fn(*a, **k))

        def dma(Q, deps, ds, out_, in_):
            Q.wait(flat(deps))
            Q.eng.dma_start(out=out_, in_=in_).then_inc(ds.sem, 16)
            ds.n += 16
            return (ds, ds.n)

        def barrier():
            toks = [e.last() for e in ENGS]
            for e in ENGS:
                e.wait([t for t in toks if t is not None and t[0] is not e])

        dbg_sem = dsem()

        dummy = es.enter_context(nc.sbuf_tensor("dummy_t", [128, 8], F32))

        def dump(name, src_ap, deps, dt=F32, shape=None):
            if not debug:
                return
            W = src_ap.shape[-1]
            with ExitStack() as tmp:
                CH = 1024
                stg = tmp.enter_context(nc.sbuf_tensor("stg_" + name, [128, CH], F32))
                t2 = None
                for c0 in range(0, W, CH):
                    c1 = min(W, c0 + CH)
                    t = op(DVE, flat([deps, t2]), nc.vector.tensor_copy, out=stg[:, 0:c1 - c0], in_=src_ap[:, c0:c1])
                    t2 = dma(SP, [t], dbg_sem, dbg[name][:, c0:c1], stg[:, 0:c1 - c0])
                SP.wait([t2])
                op(DVE, [t2], nc.vector.memset, dummy[:], 0.0)

        def sbt(stack, name, shape, dt):
            return stack.enter_context(nc.sbuf_tensor(name, shape, dt))

        def pst(stack, name, shape, dt):
            return stack.enter_context(nc.psum_tensor(name, shape, dt))

        identf = sbt(es, "identf", [128, 128], F32)
        identb = sbt(es, "identb", [128, 128], BF16)
        adaT = sbt(es, "adaT", [128, 48, 2], F32)
        modv = sbt(es, "modv", [128, 6, 8], F32)
        g1bc = sbt(es, "g1bc", [128, D], F32)
        g2bc = sbt(es, "g2bc", [128, D], F32)
        m05 = sbt(es, "m05", [128, 1], F32)
        sh2bc = sbt(es, "sh2bc", [128, D], F32)
        sc2bc = sbt(es, "sc2bc", [128, D], F32)
        comb = sbt(es, "comb", [128, NOWN, NE], F32)
        slot_i = sbt(es, "slot_i", [128, NOWN * 4], I32)
        gate2 = sbt(es, "gate2", [128, NOWN, 4], F32)
        widx_i = sbt(es, "widx_i", [128, NTILE * 8], I32)
        bidx_i = sbt(es, "bidx_i", [128, NTILE], I32)
        dc0 = dsem()
        dc1 = dsem()
        t_idf = dma(SP, [], dc0, identf[:], ident[:, :])
        t_idb = dma(POOL, [], dc1, identb[:], ident[:, :])
        t_m05 = op(POOL, [], nc.gpsimd.memset, m05[:], -0.5)

        def ln_stats(src, width, deps, st_t, mv_t, rs_t):
            nchunk = width // 512
            toks = []
            for c in range(nchunk):
                toks.append(op(DVE, deps, nc.vector.bn_stats, out=st_t[:, c * 6:(c + 1) * 6], in_=src[:, c * 512:(c + 1) * 512]))
            t = op(DVE, toks, nc.vector.bn_aggr, out=mv_t[:, 0:2], in_=st_t[:, 0:6 * nchunk])
            t = op(POOL, [t, t_m05], nc.gpsimd.tensor_scalar, out=rs_t[:, 1:2], in0=mv_t[:, 1:2], scalar1=LN_EPS, scalar2=None, op0=ALU.add)
            t = op(POOL, [t], nc.gpsimd.tensor_tensor, out=rs_t[:, 0:1], in0=rs_t[:, 1:2], in1=m05[:], op=ALU.pow)
            return t

        with ExitStack() as ph:
            cT_sb = sbt(ph, "cT_sb", [128, 8, 2], F32)
            sT = sbt(ph, "sT", [128, 8, 2], F32)
            srep = sbt(ph, "srep", [128, 8, 128], F32)
            adab_sb = sbt(ph, "adab_sb", [128, 48], F32)
            wb = [sbt(ph, "adaw%d" % i, [128, 8, D], F32) for i in range(2)]
            wb_ds = [dsem(), dsem()]
            bb = [sbt(ph, "adabb%d" % i, [128, D], F32) for i in range(4)]
            ada_ps = pst(ph, "ada_ps", [128, 48, 2], F32)
            bc_ps = pst(ph, "bc_ps", [128, D], F32)
            t1 = dma(SP, [], dc0, cT_sb[:], cT.rearrange("p (k m) -> p k m", m=2))
            t2 = dma(SP, [], dc0, adab_sb[:], adabT[:, :])
            for i_, s_ in enumerate((2, 3, 4, 5)):
                dma(SP, [], dc0, bb[i_][:], ada_b[s_ * D:(s_ + 1) * D].partition_broadcast(128))
            tc_all = (dc0, dc0.n)
            t_idf = tc_all
            t_s = op(ACT, [tc_all], nc.scalar.activation, out=sT[:], in_=cT_sb[:], func=AF.Silu)
            t_rep = op(DVE, [t_s], nc.vector.tensor_copy, out=srep[:], in_=sT[:, :, 0:1].to_broadcast([128, 8, 128]))
            ada_w_v = ada_w.rearrange("(k p) n -> p k n", p=128)
            wfree = [None, None]
            t_last_mm = None
            bc_toks = {}
            for s in range(6):
                b = s % 2
                tl = dma(SP, [wfree[b]], wb_ds[b], wb[b][:], ada_w_v[:, :, s * D:(s + 1) * D])
                PE.wait([tl, t_s])
                for cc in range(8):
                    for k in range(8):
                        mm = nc.tensor.matmul(ada_ps[:, s * 8 + cc, :], lhsT=wb[b][:, k, cc * 128:(cc + 1) * 128], rhs=sT[:, k, :],
                                              start=(k == 0), stop=(k == 7))
                t_last_mm = PE.fin(mm)
                if s in (2, 3, 4, 5):
                    PE.wait([t_rep])
                    for hf in range(2):
                        for k in range(8):
                            mm = nc.tensor.matmul(bc_ps[:, hf * 512:(hf + 1) * 512], lhsT=srep[:, k, :], rhs=wb[b][:, k, hf * 512:(hf + 1) * 512],
                                                  start=(k == 0), stop=(k == 7))
                    t_last_mm = PE.fin(mm)
                    dst = {2: g1bc, 3: sh2bc, 4: sc2bc, 5: g2bc}[s]
                    if s == 4:
                        tt = op(DVE, [t_last_mm, tc_all], nc.vector.scalar_tensor_tensor, out=dst[:], in0=bc_ps[:], scalar=1.0, in1=bb[s - 2][:], op0=ALU.add, op1=ALU.add)
                    else:
                        tt = op(DVE, [t_last_mm, tc_all], nc.vector.tensor_tensor, out=dst[:], in0=bc_ps[:], in1=bb[s - 2][:], op=ALU.add)
                    bc_toks[s] = tt
                    PE.wait([tt])
                wfree[b] = t_last_mm
            t_ada = op(DVE, [t_last_mm, tc_all], nc.vector.tensor_tensor, out=adaT[:], in0=ada_ps[:],
                       in1=adab_sb[:].unsqueeze(2).to_broadcast([128, 48, 2]), op=ALU.add)
            tm = []
            tm.append(op(DVE, [t_ada], nc.vector.tensor_scalar, out=modv[:, 0, :], in0=adaT[:, 8:16, 0], scalar1=1.0, scalar2=None, op0=ALU.add))
            tm.append(op(DVE, [t_ada], nc.vector.tensor_copy, out=modv[:, 1, :], in_=adaT[:, 0:8, 0]))
            tm.append(op(DVE, [t_ada], nc.vector.tensor_scalar, out=modv[:, 2, :], in0=adaT[:, 8:16, 1], scalar1=1.0, scalar2=None, op0=ALU.add))
            tm.append(op(DVE, [t_ada], nc.vector.tensor_copy, out=modv[:, 3, :], in_=adaT[:, 0:8, 1]))
            tm.append(op(DVE, [t_ada], nc.vector.tensor_scalar, out=modv[:, 4, :], in0=adaT[:, 32:40, 0], scalar1=1.0, scalar2=None, op0=ALU.add))
            t_mod = op(DVE, [t_ada], nc.vector.tensor_copy, out=modv[:, 5, :], in_=adaT[:, 24:32, 0])
            dump("d_ada", adaT[:].rearrange("p a b -> p (a b)"), [t_mod])
            dump("d_g1bc", g1bc[:], [t_mod])
            barrier()

        lg_all = sbt(es, "lg_all", [128, NOWN, NE], F32)
        m8_all = sbt(es, "m8_all", [128, NOWN, 8], F32)
        maskall = sbt(es, "maskall", [128, NOWN, NE], F32)
        mid = ExitStack()
        mixT = sbt(mid, "mixT", [128, 8, NTOK], BF16)
        front = ExitStack()
        KT = sbt(front, "KT", [128, 4, 2816], BF16)
        V = sbt(front, "V", [128, NT_ALL, 512], BF16)
        QT = sbt(front, "QT", [128, 4, NTOK], BF16)

        with ExitStack() as ph:
            w_in_bf = sbt(ph, "w_in_bf", [128, 8, 2560], BF16)
            wsT_bf = sbt(ph, "wsT_bf", [128, 4, 128], BF16)
            lngbc = sbt(ph, "lngbc", [128, 512], F32)
            lnbbc = sbt(ph, "lnbbc", [128, 512], F32)
            bsbc = sbt(ph, "bsbc", [128, 512], F32)
            dw = dsem()
            dcc = dsem()
            w_in_v = w_in.rearrange("(k p) n -> p k n", p=128)
            for k in range(8):
                t_win = dma(POOL, [], dw, w_in_bf[:, k, :], w_in_v[:, k, :])
            t_win = dma(POOL, [], dw, wsT_bf[:], wsT.rearrange("p (g i) -> p g i", g=4))
            dma(SP, [], dcc, lngbc[:], sgu_ln_g.partition_broadcast(128))
            dma(SP, [], dcc, lnbbc[:], sgu_ln_b.partition_broadcast(128))
            t_cc = dma(SP, [], dcc, bsbc[:], sgu_bv.partition_broadcast(128))

            xt = [sbt(ph, "xt%d" % i, [128, D], F32) for i in range(2)]
            xt_ds = [dsem(), dsem()]
            xt_free = [None, None]
            xn = [sbt(ph, "xn%d" % i, [128, D], BF16) for i in range(2)]
            xn_free = [None, None]
            st = [sbt(ph, "st%d" % i, [128, 12], F32) for i in range(2)]
            mv = [sbt(ph, "mv%d" % i, [128, 2], F32) for i in range(2)]
            rs = [sbt(ph, "rs%d" % i, [128, 2], F32) for i in range(2)]
            tp_ps = Ring([pst(ph, "tp_ps%d" % i, [128, 8, 128], BF16) for i in range(2)])
            hT = [sbt(ph, "hT%d" % i, [128, 8, 512], BF16) for i in range(2)]
            hT_free = [None, None]
            acc_ps = Ring([pst(ph, "acc_ps%d" % i, [128, 512], F32) for i in range(4)])
            sg_ps = pst(ph, "sg_ps", [128, 4, 128], F32)
            sg_free = [None]
            Gg = [sbt(ph, "Gg%d" % i, [128, 512], F32) for i in range(2)]
            Gn = [sbt(ph, "Gn%d" % i, [128, 512], F32) for i in range(2)]
            Gb = [sbt(ph, "Gb%d" % i, [128, 512], BF16) for i in range(2)]
            Gfree = [None, None]
            gst = [sbt(ph, "gst%d" % i, [128, 6], F32) for i in range(2)]
            gmv = [sbt(ph, "gmv%d" % i, [128, 2], F32) for i in range(2)]
            grs = [sbt(ph, "grs%d" % i, [128, 2], F32) for i in range(2)]
            stmp = [sbt(ph, "stmp%d" % i, [128, 512], F32) for i in range(2)]
            stmp_free = [None, None]
            evq = [0]

            def evac_copy(deps, out_, in_, scale=None, func=None):
                evq[0] += 1
                if func is not None:
                    return op(ACT, deps, nc.scalar.activation, out=out_, in_=in_, func=func)
                if evq[0] % 2 == 0:
                    if scale is None:
                        return op(ACT, deps, nc.scalar.activation, out=out_, in_=in_, func=AF.Identity)
                    return op(ACT, deps, nc.scalar.activation, out=out_, in_=in_, func=AF.Identity, scale=float(scale))
                if scale is None:
                    return op(DVE, deps, nc.vector.tensor_copy, out=out_, in_=in_)
                return op(DVE, deps, nc.vector.tensor_scalar, out=out_, in0=in_, scalar1=float(scale), scalar2=None, op0=ALU.mult)

            groups = [list(range(g * 4, min(g * 4 + 4, NT_ALL))) for g in range(6)]
            ti_glob = 0
            gcount = 0
            for gi, tiles in enumerate(groups):
                hb = gi % 2
                ntok = 128 * len(tiles)
                ev_toks = []
                for sl, ti in enumerate(tiles):
                    b = ti_glob % 2
                    ti_glob += 1
                    is_ctx = ti >= NEXT_T
                    tl = dma(SP, [xt_free[b]], xt_ds[b], xt[b][:], xe[ti * 128:(ti + 1) * 128, :])
                    trs = ln_stats(xt[b], D, [tl], st[b], mv[b], rs[b])
                    tn = op(DVE, [trs, xn_free[b]], nc.vector.tensor_scalar, out=xn[b][:], in0=xt[b][:], scalar1=mv[b][:, 0:1], scalar2=rs[b][:, 0:1],
                            op0=ALU.subtract, op1=ALU.mult)
                    xt_free[b] = tn
                    pi, pt, pfree = tp_ps.next()
                    PE.wait(flat([tn, pfree, t_idb]))
                    for k in range(8):
                        mm = nc.tensor.transpose(pt[:, k, :], xn[b][:, k * 128:(k + 1) * 128], identb[:])
                    ttp = PE.fin(mm)
                    xn_free[b] = ttp
                    msc, msh = (2, 3) if is_ctx else (0, 1)
                    ACT.wait(flat([ttp, hT_free[hb], t_mod]))
                    for k in range(8):
                        a = nc.scalar.activation(out=hT[hb][:, k, sl * 128:(sl + 1) * 128], in_=pt[:, k, :], func=AF.Identity,
                                                 scale=modv[:, msc, k:k + 1], bias=modv[:, msh, k:k + 1])
                    tev = ACT.fin(a)
                    tp_ps.rel(pi, tev)
                    ev_toks.append(tev)
                hready = ev_toks[-1]
                tok0 = tiles[0] * 128
                mm_last = None
                for c in range(4):
                    ai, ap_, afree = acc_ps.next()
                    PE.wait(flat([hready, afree, t_win]))
                    for k in range(8):
                        mm = nc.tensor.matmul(ap_[:, 0:ntok], lhsT=w_in_bf[:, k, 512 + c * 128:512 + (c + 1) * 128], rhs=hT[hb][:, k, 0:ntok],
                                              start=(k == 0), stop=(k == 7))
                    tmm = PE.fin(mm)
                    te = evac_copy([tmm], KT[:, c, tok0:tok0 + ntok], ap_[:, 0:ntok])
                    acc_ps.rel(ai, te)
                for sl, ti in enumerate(tiles):
                    ai, ap_, afree = acc_ps.next()
                    PE.wait(flat([hready, afree, t_win]))
                    for k in range(8):
                        mm = nc.tensor.matmul(ap_[:, :], lhsT=hT[hb][:, k, sl * 128:(sl + 1) * 128], rhs=w_in_bf[:, k, 1024:1536],
                                              start=(k == 0), stop=(k == 7))
                    tmm = PE.fin(mm)
                    te = evac_copy([tmm], V[:, ti, :], ap_[:, :])
                    acc_ps.rel(ai, te)
                    mm_last = tmm
                own = [(sl, ti) for sl, ti in enumerate(tiles) if OWN0 <= ti < OWN0 + NOWN]
                if own:
                    s0 = own[0][0] * 128
                    nown = 128 * len(own)
                    o0 = (own[0][1] - OWN0) * 128
                    for c in range(4):
                        ai, ap_, afree = acc_ps.next()
                        PE.wait(flat([hready, afree]))
                        for k in range(8):
                            mm = nc.tensor.matmul(ap_[:, 0:nown], lhsT=w_in_bf[:, k, c * 128:(c + 1) * 128], rhs=hT[hb][:, k, s0:s0 + nown],
                                                  start=(k == 0), stop=(k == 7))
                        tmm = PE.fin(mm)
                        te = evac_copy([tmm], QT[:, c, o0:o0 + nown], ap_[:, 0:nown], scale=0.125)
                        acc_ps.rel(ai, te)
                    ut_toks = []
                    for c in range(4):
                        ai, ap_, afree = acc_ps.next()
                        PE.wait(flat([hready, afree]))
                        for k in range(8):
                            mm = nc.tensor.matmul(ap_[:, 0:nown], lhsT=w_in_bf[:, k, 1536 + c * 128:1536 + (c + 1) * 128], rhs=hT[hb][:, k, s0:s0 + nown],
                                                  start=(k == 0), stop=(k == 7))
                        tmm = PE.fin(mm)
                        te = evac_copy([tmm], mixT[:, 4 + c, o0:o0 + nown], ap_[:, 0:nown], func=AF.Gelu_apprx_tanh)
                        acc_ps.rel(ai, te)
                        ut_toks.append(te)
                    for sl, ti in own:
                        gb = gcount % 2
                        gcount += 1
                        ot = (ti - OWN0) * 128
                        ai, ap_, afree = acc_ps.next()
                        PE.wait(flat([hready, afree]))
                        for k in range(8):
                            mm = nc.tensor.matmul(ap_[:, :], lhsT=hT[hb][:, k, sl * 128:(sl + 1) * 128], rhs=w_in_bf[:, k, 2048:2560],
                                                  start=(k == 0), stop=(k == 7))
                        tmm = PE.fin(mm)
                        mm_last = tmm
                        tg = op(ACT, [tmm, Gfree[gb]], nc.scalar.activation, out=Gg[gb][:], in_=ap_[:, :], func=AF.Gelu_apprx_tanh)
                        acc_ps.rel(ai, tg)
                        trs = ln_stats(Gg[gb], 512, [tg], gst[gb], gmv[gb], grs[gb])
                        t1_ = op(DVE, [trs], nc.vector.tensor_scalar, out=Gn[gb][:], in0=Gg[gb][:], scalar1=gmv[gb][:, 0:1], scalar2=grs[gb][:, 0:1],
                                 op0=ALU.subtract, op1=ALU.mult)
                        t2_ = op(DVE, [t1_, t_cc], nc.vector.tensor_tensor, out=Gn[gb][:], in0=Gn[gb][:], in1=lngbc[:], op=ALU.mult)
                        t3_ = op(DVE, [t2_], nc.vector.tensor_tensor, out=Gb[gb][:], in0=Gn[gb][:], in1=lnbbc[:], op=ALU.add)
                        PE.wait(flat([t3_, sg_free[0], t_win]))
                        for g in range(4):
                            mm = nc.tensor.matmul(sg_ps[:, g, :], lhsT=Gb[gb][:, g * 128:(g + 1) * 128], rhs=wsT_bf[:, g, :], start=True, stop=True)
                        tsg = PE.fin(mm)
                        Gfree[gb] = tsg
                        t4_ = op(DVE, [tsg, stmp_free[gb], t_cc], nc.vector.tensor_tensor, out=stmp[gb][:], in0=sg_ps[:].rearrange("p g i -> p (g i)"), in1=bsbc[:], op=ALU.add)
                        sg_free[0] = t4_
                        t5_ = op(DVE, [t4_, ut_toks], nc.vector.tensor_tensor, out=mixT[:, 4:8, ot:ot + 128],
                                 in0=stmp[gb][:].rearrange("p (g i) -> p g i", g=4), in1=mixT[:, 4:8, ot:ot + 128], op=ALU.mult)
                        stmp_free[gb] = t5_
                hT_free[hb] = mm_last
            barrier()
            if debug:
                dump("d_KT", KT[:].rearrange("p a b -> p (a b)"), [], BF16, [128, 4 * 2816])
                dump("d_V", V[:].rearrange("p a b -> p (a b)"), [], BF16, [128, 22 * 512])
                dump("d_QT", QT[:].rearrange("p a b -> p (a b)"), [], BF16, [128, 4 * 2048])
                dump("d_sguT", mixT[:, 4:8, :].rearrange("p a b -> p (a b)"), [], BF16, [128, 4 * 2048])
                barrier()

        with ExitStack() as ph:
            bgen = sbt(ph, "bgen", [128, 8, 832], F32)
            bsp = sbt(ph, "bsp", [128, 8, 1024], F32)
            db = dsem()
            dbs = dsem()
            t_bgen = dma(SP, [], db, bgen[:], bias_gen.rearrange("p (h k) -> p h k", h=8))
            dz = dsem()
            S_ps = Ring([pst(ph, "S_ps%d" % i, [128, 1024], F32) for i in range(2)])
            PT_ps = Ring([pst(ph, "PT_ps%d" % i, [128, 8, 128], BF16) for i in range(2)])
            O_ps = pst(ph, "O_ps", [128, 8, 64], F32)
            O_free = [None]
            AT_ps = pst(ph, "AT_ps", [128, 4, 128], BF16)
            AT_free = [None]
            S_sb = Ring([sbt(ph, "S_sb%d" % i, [128, 1024], F32) for i in range(2)])
            P_sb = Ring([sbt(ph, "P_sb%d" % i, [128, 1024], BF16) for i in range(2)])
            PT_sb = Ring([sbt(ph, "PT_sb%d" % i, [128, 8, 128], BF16) for i in range(3)])
            att_sb = Ring([sbt(ph, "att_sb%d" % i, [128, 512], BF16) for i in range(2)])
            mx_all = sbt(ph, "mx_all", [128, 128], F32)
            nmx_all = sbt(ph, "nmx_all", [128, 128], F32)
            rs_all = sbt(ph, "rs_all", [128, 128], F32)
            rinv_all = sbt(ph, "rinv_all", [128, 128], F32)

            items = []
            sp_load_tok = {}
            for p in PAIR_ORDER:
                for h in range(8):
                    items.append((p, h))
            N = len(items)
            st_ = {}
            bsp_free = [None]
            sp_next = [0]

            def issue_sp_load():
                i = sp_next[0]
                if i >= len(SP_ORDER):
                    return
                p = SP_ORDER[i]
                sp_load_tok[p] = dma(SP, [bsp_free[0]], dbs, bsp[:], bias_sp[i].rearrange("p (h k) -> p h k", h=8))
                sp_next[0] += 1

            issue_sp_load()
            for i_ in range(NTILE):
                dma(SP, [], dz, Hs[i_ * TS:(i_ + 1) * TS, :], zeros[0:TS, :])
            t_zero = (dz, dz.n)

            def blocks_for(p):
                a, nrows = SPECIAL.get(p, (2 * p, 9))
                nk = nrows * 64
                bl = []
                for j in range(nk // 128):
                    bl.append((j * 128, 128, a // 2 + j))
                if nk % 128:
                    bl.append((nk - 64, 64, a // 2 + nk // 128))
                bl.append((nk, 128, NEXT_T))
                bl.append((nk + 128, 128, NEXT_T + 1))
                return a, nrows, nk, bl

            def emit_qk(i):
                p, h = items[i]
                a, nrows, nk, bl = blocks_for(p)
                nt = nk + 256
                c = h // 2
                pb = (h % 2) * 64
                si, sp_, sfree = S_ps.next()
                PE.wait(flat([sfree]))
                q_ap = QT[pb:pb + 64, c, p * 128:(p + 1) * 128]
                k0 = a * 64
                nc.tensor.matmul(sp_[:, 0:512], lhsT=q_ap, rhs=KT[pb:pb + 64, c, k0:k0 + 512], start=True, stop=True)
                nc.tensor.matmul(sp_[:, 512:nk], lhsT=q_ap, rhs=KT[pb:pb + 64, c, k0 + 512:k0 + nk], start=True, stop=True)
                tqk = PE.fin(nc.tensor.matmul(sp_[:, nk:nt], lhsT=q_ap, rhs=KT[pb:pb + 64, c, 2560:2816], start=True, stop=True))
                if p in SPECIAL:
                    btab = bsp[:, h, 0:nt]
                    bdep = sp_load_tok[p]
                else:
                    btab = bgen[:, h, 0:nt]
                    bdep = t_bgen
                bi, sb_, sbfree = S_sb.next()
                ts = op(DVE, [tqk, bdep, sbfree], nc.vector.tensor_tensor, out=sb_[:, 0:nt], in0=sp_[:, 0:nt], in1=btab, op=ALU.add)
                S_ps.rel(si, ts)
                col = i
                tm1 = op(DVE, [ts], nc.vector.tensor_reduce, out=mx_all[:, col:col + 1], in_=sb_[:, 0:nt], axis=AX.X, op=ALU.max)
                tm2 = op(DVE, [tm1], nc.vector.tensor_scalar, out=nmx_all[:, col:col + 1], in0=mx_all[:, col:col + 1], scalar1=-1.0, scalar2=None, op0=ALU.mult)
                pi, pp, pfree = P_sb.next()
                tp = op(ACT, [tm2, pfree], nc.scalar.activation, out=pp[:, 0:nt], in_=sb_[:, 0:nt], func=AF.Exp, bias=nmx_all[:, col:col + 1], scale=1.0,
                        accum_out=rs_all[:, col:col + 1])
                S_sb.rel(bi, tp)
                st_[i] = dict(pi=pi, pp=pp, tp=tp, bl=bl, last_sp=(p in SPECIAL and h == 7))
                if p in SPECIAL and h == 7:
                    bsp_free[0] = ts
                    issue_sp_load()

            def emit_tr(i):
                d = st_[i]
                ti_, tps, tfree = PT_ps.next()
                PE.wait(flat([d["tp"], tfree, t_idb]))
                for j, (off, ln, vt) in enumerate(d["bl"]):
                    mm = nc.tensor.transpose(tps[0:ln, j, :], d["pp"][:, off:off + ln], identb[:])
                ttr = PE.fin(mm)
                P_sb.rel(d["pi"], ttr)
                nb = len(d["bl"])
                qi, qsb, qfree = PT_sb.next()
                if i % 2 == 0:
                    te = op(ACT, [ttr, qfree], nc.scalar.activation, out=qsb[:, 0:nb, :], in_=tps[:, 0:nb, :], func=AF.Identity)
                else:
                    te = op(DVE, [ttr, qfree], nc.vector.tensor_copy, out=qsb[:, 0:nb, :], in_=tps[:, 0:nb, :])
                PT_ps.rel(ti_, te)
                d["qi"] = qi
                d["qsb"] = qsb
                d["te"] = te

            def emit_pv(i):
                p, h = items[i]
                d = st_[i]
                PE.wait(flat([d["te"], O_free[0] if h == 0 else None]))
                nb = len(d["bl"])
                for j, (off, ln, vt) in enumerate(d["bl"]):
                    mm = nc.tensor.matmul(O_ps[:, h, :], lhsT=d["qsb"][0:ln, j, :], rhs=V[0:ln, vt, h * 64:(h + 1) * 64], start=(j == 0), stop=(j == nb - 1))
                tpv = PE.fin(mm)
                PT_sb.rel(d["qi"], tpv)
                if h == 7:
                    c0 = i - 7
                    tr_ = op(DVE, [d["tp"]], nc.vector.reciprocal, out=rinv_all[:, c0:c0 + 8], in_=rs_all[:, c0:c0 + 8])
                    ai, asb, afree = att_sb.next()
                    ta = op(DVE, [tr_, tpv, afree], nc.vector.tensor_tensor, out=asb[:].rearrange("p (h d) -> p h d", h=8), in0=O_ps[:],
                            in1=rinv_all[:, c0:c0 + 8].unsqueeze(2).to_broadcast([128, 8, 64]), op=ALU.mult)
                    O_free[0] = ta
                    PE.wait(flat([ta, AT_free[0]]))
                    for c in range(4):
                        mm = nc.tensor.transpose(AT_ps[:, c, :], asb[:, c * 128:(c + 1) * 128], identb[:])
                    tat = PE.fin(mm)
                    att_sb.rel(ai, tat)
                    te = op(ACT, [tat], nc.scalar.activation, out=mixT[:, 0:4, p * 128:(p + 1) * 128], in_=AT_ps[:], func=AF.Identity)
                    AT_free[0] = te
                del st_[i]

            for i in range(N + 2):
                if i < N:
                    emit_qk(i)
                if 1 <= i <= N:
                    emit_tr(i - 1)
                if i >= 2:
                    emit_pv(i - 2)
            barrier()
            if debug:
                dump("d_attT", mixT[:, 0:4, :].rearrange("p a b -> p (a b)"), [], BF16, [128, 4 * 2048])
                barrier()
        front.close()

        hrb_ds = [dsem(), dsem()]
        acct_ds = [dsem(), dsem()]
        with ExitStack() as ph:
            w_out_bf = sbt(ph, "w_out_bf", [128, 8, D], BF16)
            ln1gbc = sbt(ph, "ln1gbc", [128, D], F32)
            ln1bbc = sbt(ph, "ln1bbc", [128, D], F32)
            rw = sbt(ph, "rw", [128, 8, NE], F32)
            rbbc = sbt(ph, "rbbc", [128, NE], F32)
            bd_sb = sbt(ph, "bd_sb", [NE, D], F32)
            dw = dsem()
            dcc = dsem()
            t_wout = dma(POOL, [], dw, w_out_bf[:], w_out.rearrange("(k p) n -> p k n", p=128))
            dma(SP, [], dcc, ln1gbc[:], ln1_g.partition_broadcast(128))
            dma(SP, [], dcc, ln1bbc[:], ln1_b.partition_broadcast(128))
            dma(SP, [], dcc, rw[:], router_w.rearrange("(k p) n -> p k n", p=128))
            dma(SP, [], dcc, rbbc[:], router_b.partition_broadcast(128))
            t_cc = dma(SP, [], dcc, bd_sb[:], exp_b_down[:, :])
            xr = [sbt(ph, "xr%d" % i, [128, D], F32) for i in range(2)]
            xr_ds = [dsem(), dsem()]
            xr_free = [None, None]
            mix_ps = pst(ph, "mix_ps", [128, D], F32)
            mix_free = [None]
            tr_ps = pst(ph, "tr_ps", [128, 8, 128], F32)
            tr_free = [None]
            lg_ps = pst(ph, "lg_ps", [128, NE], F32)
            lg_free = [None]
            ct_ps = pst(ph, "ct_ps", [NE, 128], F32)
            ct_free = [None]
            bd_ps = pst(ph, "bd_ps", [128, D], F32)
            bd_free = [None]
            wk = sbt(ph, "wk", [128, D], F32)
            wk_free = [None]
            x1n = sbt(ph, "x1n", [128, D], F32)
            x1n_free = [None]
            h2f = sbt(ph, "h2f", [128, 8, 128], F32)
            h2f_free = [None]
            hrf = sbt(ph, "hrf", [128, D], F32)
            hrb = [sbt(ph, "hrb%d" % i, [128, D], BF16) for i in range(2)]
            hrb_free = [None, None]
            acct = [sbt(ph, "acct%d" % i, [128, D], F32) for i in range(2)]
            acct_free = [None, None]
            st = sbt(ph, "st4", [128, 12], F32)
            mv = sbt(ph, "mv4", [128, 2], F32)
            rs = sbt(ph, "rs4", [128, 2], F32)
            st2 = sbt(ph, "st5", [128, 12], F32)
            mv2 = sbt(ph, "mv5", [128, 2], F32)
            rs2 = sbt(ph, "rs5", [128, 2], F32)
            rt = sbt(ph, "rt", [128, 4], F32)
            ex = sbt(ph, "ex", [128, NE], F32)
            combT = sbt(ph, "combT", [NE, 128], F32)
            combT_free = [None]
            ex_free = [None]

            ones1 = sbt(ph, "ones1", [1, 128], F32)
            rb1 = sbt(ph, "rb1", [1, NE], F32)
            bdt = sbt(ph, "bdt", [128, D], F32)
            bdt_free = [None]
            t_ones = op(DVE, [], nc.vector.memset, ones1[:], 1.0)
            drb = dsem()
            t_rb1 = dma(SP, [], drb, rb1[:], router_b.rearrange("(o n) -> o n", o=1))
            s1 = {}

            def stage1(t):
                b = t % 2
                tok = slice(t * 128, (t + 1) * 128)
                tl = dma(SP, [xr_free[b]], xr_ds[b], xr[b][:], xe[(OWN0 + t) * 128:(OWN0 + t + 1) * 128, :])
                PE.wait(flat([mix_free[0], t_wout]))
                for hf in range(2):
                    for k in range(8):
                        mm = nc.tensor.matmul(mix_ps[:, hf * 512:(hf + 1) * 512], lhsT=mixT[:, k, tok], rhs=w_out_bf[:, k, hf * 512:(hf + 1) * 512],
                                              start=(k == 0), stop=(k == 7))
                tmm = PE.fin(mm)
                ta = op(DVE, [tmm, wk_free[0]], nc.vector.tensor_tensor, out=wk[:], in0=mix_ps[:], in1=g1bc[:], op=ALU.mult)
                mix_free[0] = ta
                tb_ = op(DVE, [ta, tl], nc.vector.scalar_tensor_tensor, out=wk[:], in0=xr[b][:], scalar=float(ALPHA), in1=wk[:], op0=ALU.mult, op1=ALU.add)
                xr_free[b] = tb_
                trs = ln_stats(wk, D, [tb_], st, mv, rs)
                tc_ = op(DVE, [trs], nc.vector.tensor_scalar, out=wk[:], in0=wk[:], scalar1=mv[:, 0:1], scalar2=rs[:, 0:1], op0=ALU.subtract, op1=ALU.mult)
                td_ = op(DVE, [tc_, t_cc], nc.vector.tensor_tensor, out=wk[:], in0=wk[:], in1=ln1gbc[:], op=ALU.mult)
                te_ = op(DVE, [td_], nc.vector.tensor_tensor, out=wk[:], in0=wk[:], in1=ln1bbc[:], op=ALU.add)
                tf_ = op(ACT, [te_, acct_free[b]], nc.scalar.activation, out=acct[b][:], in_=wk[:], func=AF.Identity, scale=float(ALPHA))
                trs2 = ln_stats(wk, D, [te_], st2, mv2, rs2)
                tg_ = op(DVE, [trs2, x1n_free[0]], nc.vector.tensor_scalar, out=x1n[:], in0=wk[:], scalar1=mv2[:, 0:1], scalar2=rs2[:, 0:1],
                         op0=ALU.subtract, op1=ALU.mult)
                wk_free[0] = [tf_, tg_]
                hr1 = op(DVE, [tg_], nc.vector.tensor_tensor, out=hrf[:], in0=x1n[:], in1=sc2bc[:], op=ALU.mult)
                hr2 = op(DVE, [hr1, hrb_free[b]], nc.vector.tensor_tensor, out=hrb[b][:], in0=hrf[:], in1=sh2bc[:], op=ALU.add)
                hrb_free[b] = dma(SP, [hr2], hrb_ds[b], Hx[t * 128:(t + 1) * 128, :], hrb[b][:])
                PE.wait(flat([tg_, tr_free[0], t_idf]))
                for k in range(8):
                    mm = nc.tensor.transpose(tr_ps[:, k, :], x1n[:, k * 128:(k + 1) * 128], identf[:])
                ttr = PE.fin(mm)
                x1n_free[0] = [ttr, hr1]
                ACT.wait(flat([ttr, h2f_free[0]]))
                for k in range(8):
                    a = nc.scalar.activation(out=h2f[:, k, :], in_=tr_ps[:, k, :], func=AF.Identity, scale=modv[:, 4, k:k + 1], bias=modv[:, 5, k:k + 1])
                th = ACT.fin(a)
                tr_free[0] = th
                PE.wait(flat([th, lg_free[0], t_cc, t_ones, t_rb1]))
                for k in range(8):
                    nc.tensor.matmul(lg_ps[:, :], lhsT=h2f[:, k, :], rhs=rw[:, k, :], start=(k == 0), stop=False)
                tlg = PE.fin(nc.tensor.matmul(lg_ps[:, :], lhsT=ones1[0:1, :], rhs=rb1[0:1, :], start=False, stop=True))
                h2f_free[0] = [tlg]
                r1 = op(ACT, [tlg], nc.scalar.activation, out=lg_all[:, t, :], in_=lg_ps[:], func=AF.Identity)
                lg_free[0] = r1
                s1[t] = dict(tf_=tf_, r1=r1)

            def stage2(t):
                b = t % 2
                lg = lg_all[:, t, :]
                m8 = m8_all[:, t, :]
                msk = maskall[:, t, :]
                r1 = s1[t]["r1"]
                tf_ = s1[t]["tf_"]
                r2 = op(DVE, [r1], nc.vector.max, out=m8, in_=lg)
                r3 = op(DVE, [r2], nc.vector.tensor_scalar, out=rt[:, 0:1], in0=m8_all[:, t, 0:1], scalar1=-1.0, scalar2=None, op0=ALU.mult)
                r4 = op(ACT, [r3, ex_free[0]], nc.scalar.activation, out=ex[:], in_=lg, func=AF.Exp, bias=rt[:, 0:1], scale=1.0)
                r5 = op(DVE, [r2], nc.vector.tensor_scalar, out=msk, in0=lg, scalar1=m8_all[:, t, 3:4], scalar2=None, op0=ALU.is_ge)
                r6 = op(DVE, [r4, r5], nc.vector.tensor_tensor, out=ex[:], in0=ex[:], in1=msk, op=ALU.mult)
                r7 = op(DVE, [r6], nc.vector.tensor_reduce, out=rt[:, 1:2], in_=ex[:], axis=AX.X, op=ALU.add)
                r8 = op(DVE, [r7], nc.vector.reciprocal, out=rt[:, 2:3], in_=rt[:, 1:2])
                r9 = op(DVE, [r8], nc.vector.tensor_scalar, out=comb[:, t, :], in0=ex[:], scalar1=rt[:, 2:3], scalar2=None, op0=ALU.mult)
                ex_free[0] = r9
                PE.wait(flat([r9, ct_free[0]]))
                tct = PE.fin(nc.tensor.transpose(ct_ps[:, :], comb[:, t, :], identf[:]))
                tcc_ = op(ACT, [tct, combT_free[0]], nc.scalar.activation, out=combT[:], in_=ct_ps[:], func=AF.Identity)
                ct_free[0] = tcc_
                PE.wait(flat([tcc_, bd_free[0]]))
                for hf in range(2):
                    mm = nc.tensor.matmul(bd_ps[:, hf * 512:(hf + 1) * 512], lhsT=combT[:, :], rhs=bd_sb[:, hf * 512:(hf + 1) * 512], start=True, stop=True)
                tbd = PE.fin(mm)
                combT_free[0] = tbd
                u1 = op(DVE, [tbd, bdt_free[0]], nc.vector.tensor_tensor, out=bdt[:], in0=bd_ps[:], in1=g2bc[:], op=ALU.mult)
                bd_free[0] = u1
                u2 = op(DVE, [u1, tf_], nc.vector.tensor_tensor, out=acct[b][:], in0=acct[b][:], in1=bdt[:], op=ALU.add)
                bdt_free[0] = u2
                acct_free[b] = dma(SP, [u2], acct_ds[b], accd[t * 128:(t + 1) * 128, :], acct[b][:])
                del s1[t]

            stage1(0)
            for t in range(NOWN):
                if t + 1 < NOWN:
                    stage1(t + 1)
                stage2(t)
            SP.wait([(d_, d_.n) for d_ in hrb_ds + acct_ds])
            barrier()
        t_hx = [(d_, d_.n) for d_ in hrb_ds]
        t_accd = [(d_, d_.n) for d_ in acct_ds]
        mid.close()

        bcreg_slot = nc.gpsimd.alloc_register("bc_slot")
        nc.gpsimd.reg_mov(bcreg_slot, NSLOT - 1)
        bcreg_w = nc.gpsimd.alloc_register("bc_w")
        nc.gpsimd.reg_mov(bcreg_w, NE * 1024 - 1)
        bcreg_b = nc.gpsimd.alloc_register("bc_b")
        nc.gpsimd.reg_mov(bcreg_b, NE * 128 - 1)
        with ExitStack() as ph:
            cst_sb = sbt(ph, "cst_sb", [128, CW], F32)
            dcs = dsem()
            t_cst = dma(SP, [], dcs, cst_sb[:], cst[:, :])
            U128 = cst_sb[:, 0:128]
            ONES = cst_sb[:, 128:256]
            USTR = cst_sb[0:NE, 256:288]
            jrow = cst_sb[:, C_J:C_J + NTILE]
            erow = cst_sb[:, C_E:C_E + NE]
            pk = cst_sb[:, C_PK:C_PK + 8]
            bigp = cst_sb[:, C_BIG:C_BIG + 1]
            prow = cst_sb[:, C_P:C_P + 1]
            cum = sbt(ph, "cum", [128, NOWN, NE], F32)
            pos_ps = pst(ph, "pos_ps", [128, NOWN, NE], F32)
            cnt_ps = pst(ph, "cnt_ps", [128, NE], F32)
            ntT_ps = pst(ph, "ntT_ps", [NE, 128], F32)
            ts_ps = pst(ph, "ts_ps", [128, NE], F32)
            cnt_sb = sbt(ph, "cnt_sb", [128, NE], F32)
            nt = sbt(ph, "nt", [128, NE], F32)
            ntT = sbt(ph, "ntT", [NE, 128], F32)
            tstart = sbt(ph, "tstart", [128, NE], F32)
            tend = sbt(ph, "tend", [128, NE], F32)
            slotf = sbt(ph, "slotf", [128, NOWN, NE], F32)
            oh = sbt(ph, "oh", [128, NOWN, NE], F32)
            tmpm = sbt(ph, "tmpm", [128, NOWN, NE], F32)
            slotsel = sbt(ph, "slotsel", [128, NOWN, 4], F32)
            gate = sbt(ph, "gate", [128, NOWN, 4], F32)
            A3 = sbt(ph, "A3", [128, NTILE, NE], F32)
            B3 = sbt(ph, "B3", [128, NTILE, NE], F32)
            used = sbt(ph, "used", [128, NTILE], F32)
            texp = sbt(ph, "texp", [128, NTILE], F32)
            cfill = sbt(ph, "cfill", [128, NTILE], F32)
            wf = sbt(ph, "wf", [128, NTILE, 8], F32)
            bf_ = sbt(ph, "bf_", [128, NTILE], F32)

            tq = op(DVE, [], nc.vector.memset, cum[:, 0, :], 0.0)
            for t in range(1, NOWN):
                tq = op(DVE, [tq], nc.vector.tensor_tensor, out=cum[:, t, :], in0=cum[:, t - 1, :], in1=maskall[:, t - 1, :], op=ALU.add)
            PE.wait(flat([tq, t_cst]))
            for t in range(NOWN):
                nc.tensor.matmul(pos_ps[:, t, :], lhsT=U128, rhs=maskall[:, t, :], start=True, stop=False)
                mm = nc.tensor.matmul(pos_ps[:, t, :], lhsT=ONES, rhs=cum[:, t, :], start=False, stop=True)
            nc.tensor.matmul(cnt_ps[:, :], lhsT=ONES, rhs=cum[:, NOWN - 1, :], start=True, stop=False)
            tpos = PE.fin(nc.tensor.matmul(cnt_ps[:, :], lhsT=ONES, rhs=maskall[:, NOWN - 1, :], start=False, stop=True))
            q = op(DVE, [tpos], nc.vector.tensor_copy, out=cnt_sb[:], in_=cnt_ps[:])
            q = op(DVE, [q], nc.vector.tensor_scalar, out=nt[:], in0=cnt_sb[:], scalar1=0.0, scalar2=None, op0=ALU.is_gt)
            for thr in [float(TS * i_) for i_ in range(1, 2048 // TS + 1) if TS * i_ < 2048]:
                q = op(DVE, [q], nc.vector.scalar_tensor_tensor, out=nt[:], in0=cnt_sb[:], scalar=thr, in1=nt[:], op0=ALU.is_gt, op1=ALU.add)
            PE.wait(flat([q, t_idf]))
            tnt = PE.fin(nc.tensor.transpose(ntT_ps[:, :], nt[:], identf[:]))
            q2 = op(DVE, [tnt], nc.vector.tensor_copy, out=ntT[:], in_=ntT_ps[:])
            PE.wait(flat([q2]))
            tts = PE.fin(nc.tensor.matmul(ts_ps[:, :], lhsT=ntT[:, :], rhs=USTR, start=True, stop=True))
            q = op(DVE, [tts], nc.vector.tensor_copy, out=tstart[:], in_=ts_ps[:])
            q = op(DVE, [q], nc.vector.tensor_tensor, out=tend[:], in0=tstart[:], in1=nt[:], op=ALU.add)
            q = op(DVE, [q], nc.vector.scalar_tensor_tensor, out=slotf[:], in0=tstart[:].unsqueeze(1).to_broadcast([128, NOWN, NE]), scalar=float(TS), in1=pos_ps[:],
                   op0=ALU.mult, op1=ALU.add)
            for k in range(4):
                q = op(DVE, [q], nc.vector.tensor_tensor, out=oh[:], in0=lg_all[:], in1=m8_all[:, :, k:k + 1].to_broadcast([128, NOWN, NE]), op=ALU.is_equal)
                q = op(DVE, [q], nc.vector.tensor_tensor, out=tmpm[:], in0=oh[:], in1=slotf[:], op=ALU.mult)
                q = op(DVE, [q], nc.vector.tensor_reduce, out=slotsel[:, :, k], in_=tmpm[:], axis=AX.X, op=ALU.add)
                q = op(DVE, [q], nc.vector.tensor_tensor, out=tmpm[:], in0=oh[:], in1=comb[:], op=ALU.mult)
                q = op(DVE, [q], nc.vector.tensor_reduce, out=gate[:, :, k], in_=tmpm[:], axis=AX.X, op=ALU.add)
            q = op(DVE, [q], nc.vector.tensor_copy, out=slot_i[:], in_=slotsel[:].rearrange("p t k -> p (t k)"))
            q = op(DVE, [q], nc.vector.tensor_scalar, out=gate2[:], in0=gate[:], scalar1=float(1.0 / 1.702), scalar2=None, op0=ALU.mult)
            q = op(DVE, [q], nc.vector.tensor_tensor, out=A3[:], in0=tstart[:].unsqueeze(1).to_broadcast([128, NTILE, NE]),
                   in1=jrow.unsqueeze(2).to_broadcast([128, NTILE, NE]), op=ALU.is_le)
            q = op(DVE, [q], nc.vector.tensor_tensor, out=B3[:], in0=tend[:].unsqueeze(1).to_broadcast([128, NTILE, NE]),
                   in1=jrow.unsqueeze(2).to_broadcast([128, NTILE, NE]), op=ALU.is_gt)
            q = op(DVE, [q], nc.vector.tensor_tensor, out=A3[:], in0=A3[:], in1=B3[:], op=ALU.mult)
            q = op(DVE, [q], nc.vector.tensor_reduce, out=used[:], in_=A3[:], axis=AX.X, op=ALU.add)
            q = op(DVE, [q], nc.vector.tensor_tensor, out=B3[:], in0=A3[:], in1=erow.unsqueeze(1).to_broadcast([128, NTILE, NE]), op=ALU.mult)
            q = op(DVE, [q], nc.vector.tensor_reduce, out=texp[:], in_=B3[:], axis=AX.X, op=ALU.add)
            q = op(DVE, [q], nc.vector.tensor_scalar, out=cfill[:], in0=used[:], scalar1=-1.0, scalar2=1.0, op0=ALU.mult, op1=ALU.add)
            q = op(DVE, [q], nc.vector.tensor_scalar, out=cfill[:], in0=cfill[:], scalar1=bigp, scalar2=None, op0=ALU.mult)
            q = op(DVE, [q], nc.vector.tensor_scalar, out=bf_[:], in0=texp[:], scalar1=1024.0, scalar2=None, op0=ALU.mult)
            q = op(DVE, [q], nc.vector.tensor_tensor, out=wf[:], in0=bf_[:].unsqueeze(2).to_broadcast([128, NTILE, 8]),
                   in1=pk.unsqueeze(1).to_broadcast([128, NTILE, 8]), op=ALU.add)
            q = op(DVE, [q], nc.vector.tensor_tensor, out=wf[:], in0=wf[:], in1=used[:].unsqueeze(2).to_broadcast([128, NTILE, 8]), op=ALU.mult)
            q = op(DVE, [q], nc.vector.tensor_tensor, out=wf[:], in0=wf[:], in1=cfill[:].unsqueeze(2).to_broadcast([128, NTILE, 8]), op=ALU.add)
            q = op(DVE, [q], nc.vector.tensor_copy, out=widx_i[:], in_=wf[:].rearrange("p j k -> p (j k)"))
            q = op(DVE, [q], nc.vector.tensor_scalar, out=bf_[:], in0=texp[:], scalar1=128.0, scalar2=prow, op0=ALU.mult, op1=ALU.add)
            q = op(DVE, [q], nc.vector.tensor_tensor, out=bf_[:], in0=bf_[:], in1=used[:], op=ALU.mult)
            q = op(DVE, [q], nc.vector.tensor_tensor, out=bf_[:], in0=bf_[:], in1=cfill[:], op=ALU.add)
            t_meta = op(DVE, [q], nc.vector.tensor_copy, out=bidx_i[:], in_=bf_[:])
            barrier()

        with ExitStack() as ph:
            hb = [sbt(ph, "hb%d" % i, [128, D], BF16) for i in range(2)]
            hb_ds = [dsem() for _ in range(2)]
            hb_free = [None] * 2
            dscat = [dsem(), dsem()]
            for t in range(NOWN):
                i = t % 2
                tl = dma(SP, [t_hx, hb_free[i]], hb_ds[i], hb[i][:], Hx[t * 128:(t + 1) * 128, :])
                POOL.wait(flat([tl, t_meta, t_zero]))
                for k in range(4):
                    nc.gpsimd.indirect_dma_start(out=Hs[:, :], out_offset=bass.IndirectOffsetOnAxis(ap=slot_i[:, t * 4 + k:t * 4 + k + 1], axis=0),
                                                 in_=hb[i][:], in_offset=None, bounds_check=bcreg_slot, oob_is_err=False).then_inc(dscat[i].sem, 16)
                    dscat[i].n += 16
                hb_free[i] = (dscat[i], dscat[i].n)
            t_scat = [(d_, d_.n) for d_ in dscat]

            wg = [sbt(ph, "wg%d" % i, [128, 8 * 2048], BF16) for i in range(3)]
            wd = [sbt(ph, "wd%d" % i, [128, 8 * 1024], BF16) for i in range(2)]
            bgt = [sbt(ph, "bgt%d" % i, [128, 16], F32) for i in range(3)]
            wg_ds = [dsem() for _ in range(3)]
            wd_ds = [dsem() for _ in range(2)]
            wg_free = [None] * 3
            wd_free = [None] * 2
            wg_tok = {}
            wd_tok = {}
            hrow = [sbt(ph, "hrow%d" % i, [128, D], BF16) for i in range(3)]
            hrow_ds = [dsem() for _ in range(3)]
            hrow_free = [None] * 3
            hT = [sbt(ph, "hTm%d" % i, [128, 8, TS], BF16) for i in range(2)]
            hT_free = [None, None]
            hT_tok = {}
            actT = [sbt(ph, "actT%d" % i, [128, 8, TS], BF16) for i in range(2)]
            actT_free = [None, None]
            tp_ps = Ring([pst(ph, "tp5_ps%d" % i, [128, 8, 128], BF16) for i in range(2)])
            g_ps = Ring([pst(ph, "g_ps%d" % i, [128, 512], F32) for i in range(2)])
            l_ps = Ring([pst(ph, "l_ps%d" % i, [128, 512], F32) for i in range(2)])
            y_ps = Ring([pst(ph, "y_ps%d" % i, [128, 512], F32) for i in range(2)])
            gc = Ring([sbt(ph, "gc%d" % i, [128, TS], F32) for i in range(2)])
            sg = Ring([sbt(ph, "sg%d" % i, [128, TS], F32) for i in range(2)])
            lc = Ring([sbt(ph, "lc%d" % i, [128, TS], F32) for i in range(2)])
            ystage = [sbt(ph, "ystage%d" % i, [128, D], F32) for i in range(2)]
            ys_free = [None, None]
            dys = [dsem(), dsem()]
            ysn = [0]
            pend = {}
            bias_tok = {}
            nrow = [0]

            ORDER = []
            lo_, hi_ = 0, NTILE - 1
            while lo_ <= hi_:
                if len(ORDER) % 3 == 2:
                    ORDER.append(hi_)
                    hi_ -= 1
                else:
                    ORDER.append(lo_)
                    lo_ += 1
            assert sorted(ORDER) == list(range(NTILE))

            def load_wg(j):
                b = j % 3
                tj = ORDER[j]
                POOL.wait(flat([wg_free[b], t_meta]))
                for k in range(8):
                    nc.gpsimd.indirect_dma_start(out=wg[b][:, k * 2048:(k + 1) * 2048], out_offset=None, in_=wgu_rows[:, :],
                                                 in_offset=bass.IndirectOffsetOnAxis(ap=widx_i[:, tj * 8 + k:tj * 8 + k + 1], axis=0),
                                                 bounds_check=bcreg_w, oob_is_err=False).then_inc(wg_ds[b].sem, 16)
                    wg_ds[b].n += 16
                nc.gpsimd.indirect_dma_start(out=bgt[b][:], out_offset=None, in_=bgu_rows[:, :],
                                             in_offset=bass.IndirectOffsetOnAxis(ap=bidx_i[:, tj:tj + 1], axis=0),
                                             bounds_check=bcreg_b, oob_is_err=False).then_inc(wg_ds[b].sem, 16)
                wg_ds[b].n += 16
                wg_tok[j] = (wg_ds[b], wg_ds[b].n)

            def load_wd(j):
                b = j % 2
                tj = ORDER[j]
                POOL.wait(flat([wd_free[b], t_meta]))
                for k in range(8):
                    nc.gpsimd.indirect_dma_start(out=wd[b][:, k * 1024:(k + 1) * 1024], out_offset=None, in_=wd_rows[:, :],
                                                 in_offset=bass.IndirectOffsetOnAxis(ap=widx_i[:, tj * 8 + k:tj * 8 + k + 1], axis=0),
                                                 bounds_check=bcreg_w, oob_is_err=False).then_inc(wd_ds[b].sem, 16)
                    wd_ds[b].n += 16
                wd_tok[j] = (wd_ds[b], wd_ds[b].n)

            def emit_rows(j):
                jb = j % 2
                tev = None
                for sidx in range(NSUB):
                    i = nrow[0] % 3
                    nrow[0] += 1
                    r0 = ORDER[j] * TS + sidx * 128
                    tl = dma(SP, [t_scat, hrow_free[i]], hrow_ds[i], hrow[i][:], Hs[r0:r0 + 128, :])
                    pi, tpp, tpfree = tp_ps.next()
                    PE.wait(flat([tl, tpfree, t_idb]))
                    for k in range(8):
                        mm = nc.tensor.transpose(tpp[:, k, :], hrow[i][:, k * 128:(k + 1) * 128], identb[:])
                    ttp = PE.fin(mm)
                    hrow_free[i] = ttp
                    tev = op(ACT, [ttp, hT_free[jb] if sidx == 0 else None], nc.scalar.activation, out=hT[jb][:, :, sidx * 128:(sidx + 1) * 128], in_=tpp[:], func=AF.Identity)
                    tp_ps.rel(pi, tev)
                hT_tok[j] = tev

            def emit_gu(j):
                b = j % 3
                jb = j % 2
                ab = j % 2
                tbias = op(DVE, [wg_tok[j]], nc.vector.tensor_scalar, out=bgt[b][:, 8:16], in0=bgt[b][:, 8:16], scalar1=1.0, scalar2=None, op0=ALU.add)
                last_tok = None
                for jc in range(8):
                    gi, gp, gfree = g_ps.next()
                    li, lp, lfree = l_ps.next()
                    PE.wait(flat([wg_tok[j], hT_tok[j], gfree, lfree]))
                    for k in range(8):
                        nc.tensor.matmul(gp[:, 0:TS], lhsT=wg[b][:, k * 2048 + jc * 128:k * 2048 + (jc + 1) * 128], rhs=hT[jb][:, k, :], start=(k == 0), stop=(k == 7))
                    for k in range(8):
                        mm = nc.tensor.matmul(lp[:, 0:TS], lhsT=wg[b][:, k * 2048 + 1024 + jc * 128:k * 2048 + 1024 + (jc + 1) * 128], rhs=hT[jb][:, k, :], start=(k == 0), stop=(k == 7))
                    tmm = PE.fin(mm)
                    bg_ap = bgt[b][:, jc:jc + 1]
                    bl_ap = bgt[b][:, 8 + jc:9 + jc]
                    ci, gct, gcfree = gc.next()
                    a1 = op(DVE, [tmm, tbias, gcfree], nc.vector.tensor_scalar, out=gct[:, 0:TS], in0=gp[:, 0:TS], scalar1=bg_ap, scalar2=7.0, op0=ALU.add, op1=ALU.min)
                    g_ps.rel(gi, a1)
                    xi, sgt, sgfree = sg.next()
                    a2 = op(ACT, [a1, sgfree], nc.scalar.activation, out=sgt[:, 0:TS], in_=gct[:, 0:TS], func=AF.Silu, scale=1.702)
                    gc.rel(ci, a2)
                    yi, lct, lcfree = lc.next()
                    a3 = op(DVE, [tmm, tbias, lcfree], nc.vector.tensor_scalar, out=lct[:, 0:TS], in0=lp[:, 0:TS], scalar1=bl_ap, scalar2=8.0, op0=ALU.add, op1=ALU.min)
                    l_ps.rel(li, a3)
                    a6 = op(DVE, [a2, a3, actT_free[ab] if jc == 0 else None], nc.vector.scalar_tensor_tensor, out=actT[ab][:, jc, :], in0=lct[:, 0:TS], scalar=-6.0, in1=sgt[:, 0:TS],
                            op0=ALU.max, op1=ALU.mult)
                    sg.rel(xi, a6)
                    lc.rel(yi, a6)
                    last_tok = a6
                hT_free[jb] = tmm
                pend[j] = last_tok
                wg_free[b] = [tmm, last_tok]
                if j + 3 < NTILE:
                    load_wg(j + 3)

            def emit_down(j):
                b = j % 2
                ab = j % 2
                tmm = None
                for sidx in range(NSUB):
                    yb = ysn[0] % 2
                    ysn[0] += 1
                    evs = []
                    for hc in range(2):
                        yi, yp, yfree = y_ps.next()
                        PE.wait(flat([pend[j], yfree, wd_tok[j]]))
                        for jc in range(8):
                            mm = nc.tensor.matmul(yp[:, :], lhsT=actT[ab][:, jc, sidx * 128:(sidx + 1) * 128], rhs=wd[b][:, jc * 1024 + hc * 512:jc * 1024 + (hc + 1) * 512],
                                                  start=(jc == 0), stop=(jc == 7))
                        tmm = PE.fin(mm)
                        ev = op(DVE, [tmm, ys_free[yb]], nc.vector.tensor_tensor, out=ystage[yb][:, hc * 512:(hc + 1) * 512], in0=yp[:, :], in1=g2bc[:, hc * 512:(hc + 1) * 512], op=ALU.mult)
                        y_ps.rel(yi, ev)
                        evs.append(ev)
                    r0 = ORDER[j] * TS + sidx * 128
                    ys_free[yb] = dma(SP, evs, dys[yb], Ys[r0:r0 + 128, :], ystage[yb][:])
                actT_free[ab] = tmm
                wd_free[b] = tmm
                del pend[j]
                if j + 2 < NTILE:
                    load_wd(j + 2)

            load_wg(0)
            load_wd(0)
            load_wg(1)
            load_wd(1)
            load_wg(2)
            emit_rows(0)
            for j in range(NTILE):
                emit_gu(j)
                if j + 1 < NTILE:
                    emit_rows(j + 1)
                emit_down(j)
            t_ys = [(d_, d_.n) for d_ in dys]
            SP.wait(t_ys)
            barrier()

        with ExitStack() as ph:
            ln2gbc = sbt(ph, "ln2gbc", [128, D], F32)
            ln2bbc = sbt(ph, "ln2bbc", [128, D], F32)
            dcc = dsem()
            dma(SP, [], dcc, ln2gbc[:], ln2_g.partition_broadcast(128))
            t_cc = dma(SP, [], dcc, ln2bbc[:], ln2_b.partition_broadcast(128))
            st = [sbt(ph, "st6_%d" % i, [128, 12], F32) for i in range(2)]
            mv = [sbt(ph, "mv6_%d" % i, [128, 2], F32) for i in range(2)]
            rs = [sbt(ph, "rs6_%d" % i, [128, 2], F32) for i in range(2)]
            ob = [sbt(ph, "ob%d" % i, [128, D], F32) for i in range(2)]
            ob_free = [None, None]
            accs = [sbt(ph, "accs%d" % i, [128, D], F32) for i in range(2)]
            accs_ds = [dsem(), dsem()]
            accs_free = [None, None]
            yk = [[sbt(ph, "yk%d_%d" % (i, k), [128, D], F32) for k in range(4)] for i in range(2)]
            yk_ds = [dsem(), dsem()]
            yk_free = [None, None]
            ssum = [sbt(ph, "ssum%d" % i, [128, D], F32) for i in range(2)]
            ssum_free = [None, None]
            dout = [dsem(), dsem()]
            tyk = {}

            def issue_gather(t):
                b = t % 2
                POOL.wait(flat([yk_free[b], t_ys]))
                for k in range(4):
                    nc.gpsimd.indirect_dma_start(out=yk[b][k][:], out_offset=None, in_=Ys[:, :],
                                                 in_offset=bass.IndirectOffsetOnAxis(ap=slot_i[:, t * 4 + k:t * 4 + k + 1], axis=0),
                                                 bounds_check=bcreg_slot, oob_is_err=False).then_inc(yk_ds[b].sem, 16)
                    yk_ds[b].n += 16
                tyk[t] = (yk_ds[b], yk_ds[b].n)

            issue_gather(0)
            issue_gather(1)
            trs_t = {}

            def stage_a(t):
                b = t % 2
                tla = dma(SP, [t_accd, accs_free[b]], accs_ds[b], accs[b][:], accd[t * 128:(t + 1) * 128, :])
                q = op(DVE, [tyk[t], tla, ssum_free[b]], nc.vector.scalar_tensor_tensor, out=ssum[b][:], in0=yk[b][0][:], scalar=gate2[:, t, 0:1], in1=accs[b][:], op0=ALU.mult, op1=ALU.add)
                accs_free[b] = q
                for k in range(1, 4):
                    q = op(DVE, [q], nc.vector.scalar_tensor_tensor, out=ssum[b][:], in0=yk[b][k][:], scalar=gate2[:, t, k:k + 1], in1=ssum[b][:], op0=ALU.mult, op1=ALU.add)
                yk_free[b] = q
                if t + 2 < NOWN:
                    issue_gather(t + 2)
                trs_t[t] = ln_stats(ssum[b], D, [q], st[b], mv[b], rs[b])

            def stage_b(t):
                b = t % 2
                trs = trs_t.pop(t)
                tn_ = op(DVE, [trs], nc.vector.scalar_tensor_tensor, out=rs[b][:, 1:2], in0=mv[b][:, 0:1], scalar=-1.0, in1=rs[b][:, 0:1], op0=ALU.mult, op1=ALU.mult)
                t1_ = op(ACT, [tn_, ob_free[b]], nc.scalar.activation, out=ob[b][:], in_=ssum[b][:], func=AF.Identity, scale=rs[b][:, 0:1], bias=rs[b][:, 1:2])
                ssum_free[b] = t1_
                t2_ = op(DVE, [t1_, t_cc], nc.vector.tensor_tensor, out=ob[b][:], in0=ob[b][:], in1=ln2gbc[:], op=ALU.mult)
                t3_ = op(DVE, [t2_], nc.vector.tensor_tensor, out=ob[b][:], in0=ob[b][:], in1=ln2bbc[:], op=ALU.add)
                ob_free[b] = dma(SP, [t3_], dout[b], out[t * 128:(t + 1) * 128, :], ob[b][:])

            stage_a(0)
            for t in range(NOWN):
                if t + 1 < NOWN:
                    stage_a(t + 1)
                stage_b(t)
            SP.wait([(d_, d_.n) for d_ in dout])
    return nc


def _bias_tables(rpb):
    c = np.arange(64)
    cs = np.clip(c - 8, 0, 48)
    kc = np.arange(64)
    colvalid = (kc[None, :] >= cs[:, None]) & (kc[None, :] < cs[:, None] + 16)
    dcidx = np.clip(kc[None, :] - c[:, None] + 15, 0, 30)

    def table(r0, lr0, a, nrows, width):
        T = np.full((2, 64, 8, width), NEG, np.float32)
        for i in range(2):
            r = r0 + lr0 + i
            rs = min(max(r - 4, 0), 120)
            for w in range(nrows):
                kr = r0 - 4 + a + w
                if kr < rs or kr >= rs + 8:
                    continue
                dr = kr - r + 7
                vals = rpb[:, dr, :][:, dcidx]
                vals = np.where(colvalid[None], vals, np.float32(NEG))
                T[i, :, :, w * 64:(w + 1) * 64] = vals.transpose(1, 0, 2)
            T[i, :, :, nrows * 64:nrows * 64 + 256] = 0.0
        return T.reshape(128, 8 * width)

    gen = table(32, 8, 8, 9, 832)
    sp = {}
    for q in range(4):
        r0 = 32 * q
        sp[q] = np.stack([table(r0, 2 * p, SPECIAL[p][0], SPECIAL[p][1], 1024) for p in SP_ORDER])
    return gen, sp


def _consts(core):
    c = np.zeros((128, CW), np.float32)
    p = np.arange(128)
    c[:, 0:128] = (p[:, None] < p[None, :]).astype(np.float32)
    c[:, 128:256] = 1.0
    e = np.arange(NE)
    r = (e - 4 * core) % NE
    c[0:NE, 256:288] = (r[:, None] < r[None, :]).astype(np.float32)
    c[:, C_J:C_J + NTILE] = np.arange(NTILE)[None, :]
    c[:, C_E:C_E + NE] = e[None, :]
    c[:, C_PK:C_PK + 8] = np.arange(8)[None, :] * 128 + p[:, None]
    c[:, C_BIG] = np.where(p == 0, 0.0, BIGIDX)
    c[:, C_P] = p
    return c


_NC_CACHE = {}


def kernel(x, c, ctx, c_ctx, ada_w, ada_b, w_in, rpb, sgu_ln_g, sgu_ln_b, sgu_w, sgu_b,
           w_out, ln1_g, ln1_b, ln2_g, ln2_b, router_w, router_b,
           exp_w_gu, exp_b_gu, exp_w_down, exp_b_down, _debug=False):
    f = lambda a: np.ascontiguousarray(np.asarray(a, dtype=np.float32))
    x, c, ctx, c_ctx = f(x), f(c), f(ctx), f(c_ctx)
    ada_w0, ada_b0 = f(ada_w)[0], f(ada_b)[0]
    gen, sp = _bias_tables(f(rpb)[0])
    shared = {
        "ada_w": ada_w0,
        "adabT": np.ascontiguousarray(ada_b0.reshape(48, 128).T),
        "ada_b": ada_b0,
        "w_in": f(w_in)[0],
        "w_out": f(w_out)[0],
        "bias_gen": gen,
        "sgu_ln_g": f(sgu_ln_g)[0],
        "sgu_ln_b": f(sgu_ln_b)[0],
        "wsT": np.ascontiguousarray(f(sgu_w)[0].transpose(2, 0, 1).reshape(128, 512)),
        "sgu_bv": np.ascontiguousarray(f(sgu_b)[0].reshape(512)),
        "ln1_g": f(ln1_g)[0], "ln1_b": f(ln1_b)[0], "ln2_g": f(ln2_g)[0], "ln2_b": f(ln2_b)[0],
        "router_w": f(router_w)[0], "router_b": f(router_b)[0],
        "exp_w_gu": f(exp_w_gu)[0],
        "bgu_rows": np.ascontiguousarray(f(exp_b_gu)[0].reshape(NE, 16, 128).transpose(0, 2, 1).reshape(NE * 128, 16)),
        "zeros": np.zeros((512, D), dtype=ml_dtypes.bfloat16),
        "exp_w_down": f(exp_w_down)[0],
        "exp_b_down": f(exp_b_down)[0],
        "ident": np.eye(128, dtype=np.float32),
    }
    in_maps = []
    for j in range(NCORES):
        b, q = j // 4, j % 4
        r0 = 32 * q
        xg = x[b].reshape(128, 64, D)
        xe = np.zeros((NT_ALL * 128, D), np.float32)
        xev = xe[:NEXT_T * 128].reshape(40, 64, D)
        lo, hi = r0 - 4, r0 + 36
        slo, shi = max(lo, 0), min(hi, 128)
        xev[slo - lo:shi - lo] = xg[slo:shi]
        xe[NEXT_T * 128:] = ctx[b]
        cc = np.stack([c[b], c_ctx], axis=1)
        cT = np.ascontiguousarray(cc.reshape(8, 128, 2).transpose(1, 0, 2).reshape(128, 16))
        m = dict(shared)
        m["xe"] = xe
        m["cT"] = cT
        m["bias_sp"] = sp[q]
        m["cst"] = _consts(j)
        in_maps.append(m)
    key = bool(_debug)
    if key not in _NC_CACHE:
        _NC_CACHE[key] = build(debug=key)
    nc = _NC_CACHE[key]
    res = run_bass_kernel_spmd(nc, in_maps, core_ids=list(range(NCORES)))
    outs = [np.asarray(r["out"], dtype=np.float32) for r in res.results]
    full = np.concatenate(outs, axis=0).reshape(2, 8192, D)
    if _debug:
        return full, res.results
    return full
```

```python
import numpy as np
import ml_dtypes
from contextlib import ExitStack
import concourse.bass as bass
import concourse.mybir as mybir
from concourse.bass_utils import run_bass_kernel_spmd

F32 = mybir.dt.float32
BF16 = mybir.dt.bfloat16
I32 = mybir.dt.int32
AF = mybir.ActivationFunctionType
ALU = mybir.AluOpType
AX = mybir.AxisListType

NCORES = 8
D = 1024
NEXT_T = 20
NT_ALL = 22
OWN0 = 2
NOWN = 16
NTOK = NOWN * 128
NEG = -30000.0
ALPHA = 2.0 ** 0.25
LN_EPS = 1e-5
NE = 32
TS = 384
NSUB = TS // 128
NTILE = (4 * 2048 + NE * (TS - 1)) // TS
NSLOT = NTILE * TS
C_J = 288
C_E = C_J + NTILE
C_PK = C_E + NE
C_BIG = C_PK + 8
C_P = C_BIG + 1
CW = 384
assert C_P < CW
BIGIDX = 4.0e6
SPECIAL = {0: (0, 12), 1: (2, 10), 14: (28, 9), 15: (28, 11)}
SP_ORDER = [0, 1, 14, 15]
PAIR_ORDER = [0, 2, 3, 4, 1, 5, 6, 7, 8, 14, 9, 10, 11, 15, 12, 13]


class Eng:
    def __init__(s, es, nc, eng, name):
        s.eng = eng
        s.name = name
        s.sem = es.enter_context(nc.semaphore("sem_" + name))
        s.n = 0
        s.seen = {}

    def wait(s, toks):
        for t in toks:
            if t is None:
                continue
            e, c = t
            if s.seen.get(e, 0) < c:
                s.eng.wait_ge(e.sem, c)
                s.seen[e] = c

    def fin(s, inst):
        inst.then_inc(s.sem, 1)
        s.n += 1
        return (s, s.n)

    def last(s):
        return (s, s.n) if s.n > 0 else None


class DSem:
    def __init__(s, es, nc, name):
        s.sem = es.enter_context(nc.semaphore("dsem_" + name))
        s.n = 0


def flat(deps):
    out = []
    for d in deps:
        if d is None:
            continue
        if isinstance(d, list):
            out.extend(flat(d))
        else:
            out.append(d)
    return out


class Ring:
    def __init__(s, bufs):
        s.bufs = bufs
        s.free = [None] * len(bufs)
        s.i = 0

    def next(s):
        i = s.i % len(s.bufs)
        s.i += 1
        return i, s.bufs[i], s.free[i]

    def rel(s, i, tok):
        s.free[i] = tok


def build(debug=False):
    nc = bass.Bass("TRN2", target_bir_lowering=False)

    def din(name, shape):
        return nc.dram_tensor(name, shape, F32, kind="ExternalInput").ap()

    xe = din("xe", [NT_ALL * 128, D])
    cT = din("cT", [128, 16])
    ada_w = din("ada_w", [D, 6 * D])
    adabT = din("adabT", [128, 48])
    ada_b = din("ada_b", [6 * D])
    w_in = din("w_in", [D, 2560])
    w_out = din("w_out", [D, D])
    bias_gen = din("bias_gen", [128, 8 * 832])
    bias_sp = din("bias_sp", [4, 128, 8 * 1024])
    sgu_ln_g = din("sgu_ln_g", [512])
    sgu_ln_b = din("sgu_ln_b", [512])
    wsT = din("wsT", [128, 512])
    sgu_bv = din("sgu_bv", [512])
    ln1_g = din("ln1_g", [D])
    ln1_b = din("ln1_b", [D])
    ln2_g = din("ln2_g", [D])
    ln2_b = din("ln2_b", [D])
    router_w = din("router_w", [D, NE])
    router_b = din("router_b", [NE])
    exp_w_gu = din("exp_w_gu", [NE, D, 2 * D])
    exp_w_down = din("exp_w_down", [NE, D, D])
    exp_b_down = din("exp_b_down", [NE, D])
    ident = din("ident", [128, 128])
    cst = din("cst", [128, CW])
    zeros = nc.dram_tensor("zeros", [512, D], BF16, kind="ExternalInput").ap()
    bgu_rows = din("bgu_rows", [NE * 128, 16])
    out = nc.dram_tensor("out", [NTOK, D], F32, kind="ExternalOutput").ap()
    Hx = nc.dram_tensor("Hx", [NTOK, D], BF16, kind="Internal").ap()
    Hs = nc.dram_tensor("Hs", [NSLOT, D], BF16, kind="Internal").ap()
    Ys = nc.dram_tensor("Ys", [NSLOT, D], F32, kind="Internal").ap()
    accd = nc.dram_tensor("accd", [NTOK, D], F32, kind="Internal").ap()
    wgu_rows = exp_w_gu.rearrange("e r n -> (e r) n")
    wd_rows = exp_w_down.rearrange("e r n -> (e r) n")
    dbg = {}
    if debug:
        for nm, shp in [("d_KT", [128, 4 * 2816]), ("d_V", [128, 22 * 512]), ("d_QT", [128, 4 * 2048]),
                        ("d_sguT", [128, 4 * 2048]), ("d_attT", [128, 4 * 2048]), ("d_acc", [128, 16 * 1024]),
                        ("d_h2T", [128, 8 * 2048]), ("d_comb", [128, 16 * 32]), ("d_ada", [128, 96]),
                        ("d_g1bc", [128, 1024])]:
            dbg[nm] = nc.dram_tensor(nm, shp, F32, kind="ExternalOutput").ap()

    with ExitStack() as es:
        PE = Eng(es, nc, nc.tensor, "pe")
        ACT = Eng(es, nc, nc.scalar, "act")
        DVE = Eng(es, nc, nc.vector, "dve")
        POOL = Eng(es, nc, nc.gpsimd, "pool")
        SP = Eng(es, nc, nc.sync, "sp")
        ENGS = [PE, ACT, DVE, POOL, SP]
        nds = [0]

        def dsem():
            nds[0] += 1
            return DSem(es, nc, "d%d" % nds[0])

        def op(E, deps, fn, *a, **k):
            E.wait(flat(deps))
            return E.fin(fn(*a, **k))

        def dma(Q, deps, ds, out_, in_):
            Q.wait(flat(deps))
            Q.eng.dma_start(out=out_, in_=in_).then_inc(ds.sem, 16)
            ds.n += 16
            return (ds, ds.n)

        def barrier():
            toks = [e.last() for e in ENGS]
            for e in ENGS:
                e.wait([t for t in toks if t is not None and t[0] is not e])

        dbg_sem = dsem()

        dummy = es.enter_context(nc.sbuf_tensor("dummy_t", [128, 8], F32))

        def dump(name, src_ap, deps, dt=F32, shape=None):
            if not debug:
                return
            W = src_ap.shape[-1]
            with ExitStack() as tmp:
                CH = 1024
                stg = tmp.enter_context(nc.sbuf_tensor("stg_" + name, [128, CH], F32))
                t2 = None
                for c0 in range(0, W, CH):
                    c1 = min(W, c0 + CH)
                    t = op(DVE, flat([deps, t2]), nc.vector.tensor_copy, out=stg[:, 0:c1 - c0], in_=src_ap[:, c0:c1])
                    t2 = dma(SP, [t], dbg_sem, dbg[name][:, c0:c1], stg[:, 0:c1 - c0])
                SP.wait([t2])
                op(DVE, [t2], nc.vector.memset, dummy[:], 0.0)

        def sbt(stack, name, shape, dt):
            return stack.enter_context(nc.sbuf_tensor(name, shape, dt))

        def pst(stack, name, shape, dt):
            return stack.enter_context(nc.psum_tensor(name, shape, dt))

        identf = sbt(es, "identf", [128, 128], F32)
        identb = sbt(es, "identb", [128, 128], BF16)
        adaT = sbt(es, "adaT", [128, 48, 2], F32)
        modv = sbt(es, "modv", [128, 6, 8], F32)
        g1bc = sbt(es, "g1bc", [128, D], F32)
        g2bc = sbt(es, "g2bc", [128, D], F32)
        m05 = sbt(es, "m05", [128, 1], F32)
        sh2bc = sbt(es, "sh2bc", [128, D], F32)
        sc2bc = sbt(es, "sc2bc", [128, D], F32)
        comb = sbt(es, "comb", [128, NOWN, NE], F32)
        slot_i = sbt(es, "slot_i", [128, NOWN * 4], I32)
        gate2 = sbt(es, "gate2", [128, NOWN, 4], F32)
        widx_i = sbt(es, "widx_i", [128, NTILE * 8], I32)
        bidx_i = sbt(es, "bidx_i", [128, NTILE], I32)
        dc0 = dsem()
        dc1 = dsem()
        t_idf = dma(SP, [], dc0, identf[:], ident[:, :])
        t_idb = dma(POOL, [], dc1, identb[:], ident[:, :])
        t_m05 = op(POOL, [], nc.gpsimd.memset, m05[:], -0.5)

        def ln_stats(src, width, deps, st_t, mv_t, rs_t):
            nchunk = width // 512
            toks = []
            for c in range(nchunk):
                toks.append(op(DVE, deps, nc.vector.bn_stats, out=st_t[:, c * 6:(c + 1) * 6], in_=src[:, c * 512:(c + 1) * 512]))
            t = op(DVE, toks, nc.vector.bn_aggr, out=mv_t[:, 0:2], in_=st_t[:, 0:6 * nchunk])
            t = op(POOL, [t, t_m05], nc.gpsimd.tensor_scalar, out=rs_t[:, 1:2], in0=mv_t[:, 1:2], scalar1=LN_EPS, scalar2=None, op0=ALU.add)
            t = op(POOL, [t], nc.gpsimd.tensor_tensor, out=rs_t[:, 0:1], in0=rs_t[:, 1:2], in1=m05[:], op=ALU.pow)
            return t

        with ExitStack() as ph:
            cT_sb = sbt(ph, "cT_sb", [128, 8, 2], F32)
            sT = sbt(ph, "sT", [128, 8, 2], F32)
            srep = sbt(ph, "srep", [128, 8, 128], F32)
            adab_sb = sbt(ph, "adab_sb", [128, 48], F32)
            wb = [sbt(ph, "adaw%d" % i, [128, 8, D], F32) for i in range(2)]
            wb_ds = [dsem(), dsem()]
            bb = [sbt(ph, "adabb%d" % i, [128, D], F32) for i in range(4)]
            ada_ps = pst(ph, "ada_ps", [128, 48, 2], F32)
            bc_ps = pst(ph, "bc_ps", [128, D], F32)
            t1 = dma(SP, [], dc0, cT_sb[:], cT.rearrange("p (k m) -> p k m", m=2))
            t2 = dma(SP, [], dc0, adab_sb[:], adabT[:, :])
            for i_, s_ in enumerate((2, 3, 4, 5)):
                dma(SP, [], dc0, bb[i_][:], ada_b[s_ * D:(s_ + 1) * D].partition_broadcast(128))
            tc_all = (dc0, dc0.n)
            t_idf = tc_all
            t_s = op(ACT, [tc_all], nc.scalar.activation, out=sT[:], in_=cT_sb[:], func=AF.Silu)
            t_rep = op(DVE, [t_s], nc.vector.tensor_copy, out=srep[:], in_=sT[:, :, 0:1].to_broadcast([128, 8, 128]))
            ada_w_v = ada_w.rearrange("(k p) n -> p k n", p=128)
            wfree = [None, None]
            t_last_mm = None
            bc_toks = {}
            for s in range(6):
                b = s % 2
                tl = dma(SP, [wfree[b]], wb_ds[b], wb[b][:], ada_w_v[:, :, s * D:(s + 1) * D])
                PE.wait([tl, t_s])
                for cc in range(8):
                    for k in range(8):
                        mm = nc.tensor.matmul(ada_ps[:, s * 8 + cc, :], lhsT=wb[b][:, k, cc * 128:(cc + 1) * 128], rhs=sT[:, k, :],
                                              start=(k == 0), stop=(k == 7))
                t_last_mm = PE.fin(mm)
                if s in (2, 3, 4, 5):
                    PE.wait([t_rep])
                    for hf in range(2):
                        for k in range(8):
                            mm = nc.tensor.matmul(bc_ps[:, hf * 512:(hf + 1) * 512], lhsT=srep[:, k, :], rhs=wb[b][:, k, hf * 512:(hf + 1) * 512],
                                                  start=(k == 0), stop=(k == 7))
                    t_last_mm = PE.fin(mm)
                    dst = {2: g1bc, 3: sh2bc, 4: sc2bc, 5: g2bc}[s]
                    if s == 4:
                        tt = op(DVE, [t_last_mm, tc_all], nc.vector.scalar_tensor_tensor, out=dst[:], in0=bc_ps[:], scalar=1.0, in1=bb[s - 2][:], op0=ALU.add, op1=ALU.add)
                    else:
                        tt = op(DVE, [t_last_mm, tc_all], nc.vector.tensor_tensor, out=dst[:], in0=bc_ps[:], in1=bb[s - 2][:], op=ALU.add)
                    bc_toks[s] = tt
                    PE.wait([tt])
                wfree[b] = t_last_mm
            t_ada = op(DVE, [t_last_mm, tc_all], nc.vector.tensor_tensor, out=adaT[:], in0=ada_ps[:],
                       in1=adab_sb[:].unsqueeze(2).to_broadcast([128, 48, 2]), op=ALU.add)
            tm = []
            tm.append(op(DVE, [t_ada], nc.vector.tensor_scalar, out=modv[:, 0, :], in0=adaT[:, 8:16, 0], scalar1=1.0, scalar2=None, op0=ALU.add))
            tm.append(op(DVE, [t_ada], nc.vector.tensor_copy, out=modv[:, 1, :], in_=adaT[:, 0:8, 0]))
            tm.append(op(DVE, [t_ada], nc.vector.tensor_scalar, out=modv[:, 2, :], in0=adaT[:, 8:16, 1], scalar1=1.0, scalar2=None, op0=ALU.add))
            tm.append(op(DVE, [t_ada], nc.vector.tensor_copy, out=modv[:, 3, :], in_=adaT[:, 0:8, 1]))
            tm.append(op(DVE, [t_ada], nc.vector.tensor_scalar, out=modv[:, 4, :], in0=adaT[:, 32:40, 0], scalar1=1.0, scalar2=None, op0=ALU.add))
            t_mod = op(DVE, [t_ada], nc.vector.tensor_copy, out=modv[:, 5, :], in_=adaT[:, 24:32, 0])
            dump("d_ada", adaT[:].rearrange("p a b -> p (a b)"), [t_mod])
            dump("d_g1bc", g1bc[:], [t_mod])
            barrier()

        lg_all = sbt(es, "lg_all", [128, NOWN, NE], F32)
        m8_all = sbt(es, "m8_all", [128, NOWN, 8], F32)
        maskall = sbt(es, "maskall", [128, NOWN, NE], F32)
        mid = ExitStack()
        mixT = sbt(mid, "mixT", [128, 8, NTOK], BF16)
        front = ExitStack()
        KT = sbt(front, "KT", [128, 4, 2816], BF16)
        V = sbt(front, "V", [128, NT_ALL, 512], BF16)
        QT = sbt(front, "QT", [128, 4, NTOK], BF16)

        with ExitStack() as ph:
            w_in_bf = sbt(ph, "w_in_bf", [128, 8, 2560], BF16)
            wsT_bf = sbt(ph, "wsT_bf", [128, 4, 128], BF16)
            lngbc = sbt(ph, "lngbc", [128, 512], F32)
            lnbbc = sbt(ph, "lnbbc", [128, 512], F32)
            bsbc = sbt(ph, "bsbc", [128, 512], F32)
            dw = dsem()
            dcc = dsem()
            w_in_v = w_in.rearrange("(k p) n -> p k n", p=128)
            for k in range(8):
                t_win = dma(POOL, [], dw, w_in_bf[:, k, :], w_in_v[:, k, :])
            t_win = dma(POOL, [], dw, wsT_bf[:], wsT.rearrange("p (g i) -> p g i", g=4))
            dma(SP, [], dcc, lngbc[:], sgu_ln_g.partition_broadcast(128))
            dma(SP, [], dcc, lnbbc[:], sgu_ln_b.partition_broadcast(128))
            t_cc = dma(SP, [], dcc, bsbc[:], sgu_bv.partition_broadcast(128))

            xt = [sbt(ph, "xt%d" % i, [128, D], F32) for i in range(2)]
            xt_ds = [dsem(), dsem()]
            xt_free = [None, None]
            xn = [sbt(ph, "xn%d" % i, [128, D], BF16) for i in range(2)]
            xn_free = [None, None]
            st = [sbt(ph, "st%d" % i, [128, 12], F32) for i in range(2)]
            mv = [sbt(ph, "mv%d" % i, [128, 2], F32) for i in range(2)]
            rs = [sbt(ph, "rs%d" % i, [128, 2], F32) for i in range(2)]
            tp_ps = Ring([pst(ph, "tp_ps%d" % i, [128, 8, 128], BF16) for i in range(2)])
            hT = [sbt(ph, "hT%d" % i, [128, 8, 512], BF16) for i in range(2)]
            hT_free = [None, None]
            acc_ps = Ring([pst(ph, "acc_ps%d" % i, [128, 512], F32) for i in range(4)])
            sg_ps = pst(ph, "sg_ps", [128, 4, 128], F32)
            sg_free = [None]
            Gg = [sbt(ph, "Gg%d" % i, [128, 512], F32) for i in range(2)]
            Gn = [sbt(ph, "Gn%d" % i, [128, 512], F32) for i in range(2)]
            Gb = [sbt(ph, "Gb%d" % i, [128, 512], BF16) for i in range(2)]
            Gfree = [None, None]
            gst = [sbt(ph, "gst%d" % i, [128, 6], F32) for i in range(2)]
            gmv = [sbt(ph, "gmv%d" % i, [128, 2], F32) for i in range(2)]
            grs = [sbt(ph, "grs%d" % i, [128, 2], F32) for i in range(2)]
            stmp = [sbt(ph, "stmp%d" % i, [128, 512], F32) for i in range(2)]
            stmp_free = [None, None]
            evq = [0]

            def evac_copy(deps, out_, in_, scale=None, func=None):
                evq[0] += 1
                if func is not None:
                    return op(ACT, deps, nc.scalar.activation, out=out_, in_=in_, func=func)
                if evq[0] % 2 == 0:
                    if scale is None:
                        return op(ACT, deps, nc.scalar.activation, out=out_, in_=in_, func=AF.Identity)
                    return op(ACT, deps, nc.scalar.activation, out=out_, in_=in_, func=AF.Identity, scale=float(scale))
                if scale is None:
                    return op(DVE, deps, nc.vector.tensor_copy, out=out_, in_=in_)
                return op(DVE, deps, nc.vector.tensor_scalar, out=out_, in0=in_, scalar1=float(scale), scalar2=None, op0=ALU.mult)

            groups = [list(range(g * 4, min(g * 4 + 4, NT_ALL))) for g in range(6)]
            ti_glob = 0
            gcount = 0
            for gi, tiles in enumerate(groups):
                hb = gi % 2
                ntok = 128 * len(tiles)
                ev_toks = []
                for sl, ti in enumerate(tiles):
                    b = ti_glob % 2
                    ti_glob += 1
                    is_ctx = ti >= NEXT_T
                    tl = dma(SP, [xt_free[b]], xt_ds[b], xt[b][:], xe[ti * 128:(ti + 1) * 128, :])
                    trs = ln_stats(xt[b], D, [tl], st[b], mv[b], rs[b])
                    tn = op(DVE, [trs, xn_free[b]], nc.vector.tensor_scalar, out=xn[b][:], in0=xt[b][:], scalar1=mv[b][:, 0:1], scalar2=rs[b][:, 0:1],
                            op0=ALU.subtract, op1=ALU.mult)
                    xt_free[b] = tn
                    pi, pt, pfree = tp_ps.next()
                    PE.wait(flat([tn, pfree, t_idb]))
                    for k in range(8):
                        mm = nc.tensor.transpose(pt[:, k, :], xn[b][:, k * 128:(k + 1) * 128], identb[:])
                    ttp = PE.fin(mm)
                    xn_free[b] = ttp
                    msc, msh = (2, 3) if is_ctx else (0, 1)
                    ACT.wait(flat([ttp, hT_free[hb], t_mod]))
                    for k in range(8):
                        a = nc.scalar.activation(out=hT[hb][:, k, sl * 128:(sl + 1) * 128], in_=pt[:, k, :], func=AF.Identity,
                                                 scale=modv[:, msc, k:k + 1], bias=modv[:, msh, k:k + 1])
                    tev = ACT.fin(a)
                    tp_ps.rel(pi, tev)
                    ev_toks.append(tev)
                hready = ev_toks[-1]
                tok0 = tiles[0] * 128
                mm_last = None
                for c in range(4):
                    ai, ap_, afree = acc_ps.next()
                    PE.wait(flat([hready, afree, t_win]))
                    for k in range(8):
                        mm = nc.tensor.matmul(ap_[:, 0:ntok], lhsT=w_in_bf[:, k, 512 + c * 128:512 + (c + 1) * 128], rhs=hT[hb][:, k, 0:ntok],
                                              start=(k == 0), stop=(k == 7))
                    tmm = PE.fin(mm)
                    te = evac_copy([tmm], KT[:, c, tok0:tok0 + ntok], ap_[:, 0:ntok])
                    acc_ps.rel(ai, te)
                for sl, ti in enumerate(tiles):
                    ai, ap_, afree = acc_ps.next()
                    PE.wait(flat([hready, afree, t_win]))
                    for k in range(8):
                        mm = nc.tensor.matmul(ap_[:, :], lhsT=hT[hb][:, k, sl * 128:(sl + 1) * 128], rhs=w_in_bf[:, k, 1024:1536],
                                              start=(k == 0), stop=(k == 7))
                    tmm = PE.fin(mm)
                    te = evac_copy([tmm], V[:, ti, :], ap_[:, :])
                    acc_ps.rel(ai, te)
                    mm_last = tmm
                own = [(sl, ti) for sl, ti in enumerate(tiles) if OWN0 <= ti < OWN0 + NOWN]
                if own:
                    s0 = own[0][0] * 128
                    nown = 128 * len(own)
                    o0 = (own[0][1] - OWN0) * 128
                    for c in range(4):
                        ai, ap_, afree = acc_ps.next()
                        PE.wait(flat([hready, afree]))
                        for k in range(8):
                            mm = nc.tensor.matmul(ap_[:, 0:nown], lhsT=w_in_bf[:, k, c * 128:(c + 1) * 128], rhs=hT[hb][:, k, s0:s0 + nown],
                                                  start=(k == 0), stop=(k == 7))
                        tmm = PE.fin(mm)
                        te = evac_copy([tmm], QT[:, c, o0:o0 + nown], ap_[:, 0:nown], scale=0.125)
                        acc_ps.rel(ai, te)
                    ut_toks = []
                    for c in range(4):
                        ai, ap_, afree = acc_ps.next()
                        PE.wait(flat([hready, afree]))
                        for k in range(8):
                            mm = nc.tensor.matmul(ap_[:, 0:nown], lhsT=w_in_bf[:, k, 1536 + c * 128:1536 + (c + 1) * 128], rhs=hT[hb][:, k, s0:s0 + nown],
                                                  start=(k == 0), stop=(k == 7))
                        tmm = PE.fin(mm)
                        te = evac_copy([tmm], mixT[:, 4 + c, o0:o0 + nown], ap_[:, 0:nown], func=AF.Gelu_apprx_tanh)
                        acc_ps.rel(ai, te)
                        ut_toks.append(te)
                    for sl, ti in own:
                        gb = gcount % 2
                        gcount += 1
                        ot = (ti - OWN0) * 128
                        ai, ap_, afree = acc_ps.next()
                        PE.wait(flat([hready, afree]))
                        for k in range(8):
                            mm = nc.tensor.matmul(ap_[:, :], lhsT=hT[hb][:, k, sl * 128:(sl + 1) * 128], rhs=w_in_bf[:, k, 2048:2560],
                                                  start=(k == 0), stop=(k == 7))
                        tmm = PE.fin(mm)
                        mm_last = tmm
                        tg = op(ACT, [tmm, Gfree[gb]], nc.scalar.activation, out=Gg[gb][:], in_=ap_[:, :], func=AF.Gelu_apprx_tanh)
                        acc_ps.rel(ai, tg)
                        trs = ln_stats(Gg[gb], 512, [tg], gst[gb], gmv[gb], grs[gb])
                        t1_ = op(DVE, [trs], nc.vector.tensor_scalar, out=Gn[gb][:], in0=Gg[gb][:], scalar1=gmv[gb][:, 0:1], scalar2=grs[gb][:, 0:1],
                                 op0=ALU.subtract, op1=ALU.mult)
                        t2_ = op(DVE, [t1_, t_cc], nc.vector.tensor_tensor, out=Gn[gb][:], in0=Gn[gb][:], in1=lngbc[:], op=ALU.mult)
                        t3_ = op(DVE, [t2_], nc.vector.tensor_tensor, out=Gb[gb][:], in0=Gn[gb][:], in1=lnbbc[:], op=ALU.add)
                        PE.wait(flat([t3_, sg_free[0], t_win]))
                        for g in range(4):
                            mm = nc.tensor.matmul(sg_ps[:, g, :], lhsT=Gb[gb][:, g * 128:(g + 1) * 128], rhs=wsT_bf[:, g, :], start=True, stop=True)
                        tsg = PE.fin(mm)
                        Gfree[gb] = tsg
                        t4_ = op(DVE, [tsg, stmp_free[gb], t_cc], nc.vector.tensor_tensor, out=stmp[gb][:], in0=sg_ps[:].rearrange("p g i -> p (g i)"), in1=bsbc[:], op=ALU.add)
                        sg_free[0] = t4_
                        t5_ = op(DVE, [t4_, ut_toks], nc.vector.tensor_tensor, out=mixT[:, 4:8, ot:ot + 128],
                                 in0=stmp[gb][:].rearrange("p (g i) -> p g i", g=4), in1=mixT[:, 4:8, ot:ot + 128], op=ALU.mult)
                        stmp_free[gb] = t5_
                hT_free[hb] = mm_last
            barrier()
            if debug:
                dump("d_KT", KT[:].rearrange("p a b -> p (a b)"), [], BF16, [128, 4 * 2816])
                dump("d_V", V[:].rearrange("p a b -> p (a b)"), [], BF16, [128, 22 * 512])
                dump("d_QT", QT[:].rearrange("p a b -> p (a b)"), [], BF16, [128, 4 * 2048])
                dump("d_sguT", mixT[:, 4:8, :].rearrange("p a b -> p (a b)"), [], BF16, [128, 4 * 2048])
                barrier()

        with ExitStack() as ph:
            bgen = sbt(ph, "bgen", [128, 8, 832], F32)
            bsp = sbt(ph, "bsp", [128, 8, 1024], F32)
            db = dsem()
            dbs = dsem()
            t_bgen = dma(SP, [], db, bgen[:], bias_gen.rearrange("p (h k) -> p h k", h=8))
            dz = dsem()
            S_ps = Ring([pst(ph, "S_ps%d" % i, [128, 1024], F32) for i in range(2)])
            PT_ps = Ring([pst(ph, "PT_ps%d" % i, [128, 8, 128], BF16) for i in range(2)])
            O_ps = pst(ph, "O_ps", [128, 8, 64], F32)
            O_free = [None]
            AT_ps = pst(ph, "AT_ps", [128, 4, 128], BF16)
            AT_free = [None]
            S_sb = Ring([sbt(ph, "S_sb%d" % i, [128, 1024], F32) for i in range(2)])
            P_sb = Ring([sbt(ph, "P_sb%d" % i, [128, 1024], BF16) for i in range(2)])
            PT_sb = Ring([sbt(ph, "PT_sb%d" % i, [128, 8, 128], BF16) for i in range(3)])
            att_sb = Ring([sbt(ph, "att_sb%d" % i, [128, 512], BF16) for i in range(2)])
            mx_all = sbt(ph, "mx_all", [128, 128], F32)
            nmx_all = sbt(ph, "nmx_all", [128, 128], F32)
            rs_all = sbt(ph, "rs_all", [128, 128], F32)
            rinv_all = sbt(ph, "rinv_all", [128, 128], F32)

            items = []
            sp_load_tok = {}
            for p in PAIR_ORDER:
                for h in range(8):
                    items.append((p, h))
            N = len(items)
            st_ = {}
            bsp_free = [None]
            sp_next = [0]

            def issue_sp_load():
                i = sp_next[0]
                if i >= len(SP_ORDER):
                    return
                p = SP_ORDER[i]
                sp_load_tok[p] = dma(SP, [bsp_free[0]], dbs, bsp[:], bias_sp[i].rearrange("p (h k) -> p h k", h=8))
                sp_next[0] += 1

            issue_sp_load()
            for i_ in range(NTILE):
                dma(SP, [], dz, Hs[i_ * TS:(i_ + 1) * TS, :], zeros[0:TS, :])
            t_zero = (dz, dz.n)

            def blocks_for(p):
                a, nrows = SPECIAL.get(p, (2 * p, 9))
                nk = nrows * 64
                bl = []
                for j in range(nk // 128):
                    bl.append((j * 128, 128, a // 2 + j))
                if nk % 128:
                    bl.append((nk - 64, 64, a // 2 + nk // 128))
                bl.append((nk, 128, NEXT_T))
                bl.append((nk + 128, 128, NEXT_T + 1))
                return a, nrows, nk, bl

            def emit_qk(i):
                p, h = items[i]
                a, nrows, nk, bl = blocks_for(p)
                nt = nk + 256
                c = h // 2
                pb = (h % 2) * 64
                si, sp_, sfree = S_ps.next()
                PE.wait(flat([sfree]))
                q_ap = QT[pb:pb + 64, c, p * 128:(p + 1) * 128]
                k0 = a * 64
                nc.tensor.matmul(sp_[:, 0:512], lhsT=q_ap, rhs=KT[pb:pb + 64, c, k0:k0 + 512], start=True, stop=True)
                nc.tensor.matmul(sp_[:, 512:nk], lhsT=q_ap, rhs=KT[pb:pb + 64, c, k0 + 512:k0 + nk], start=True, stop=True)
                tqk = PE.fin(nc.tensor.matmul(sp_[:, nk:nt], lhsT=q_ap, rhs=KT[pb:pb + 64, c, 2560:2816], start=True, stop=True))
                if p in SPECIAL:
                    btab = bsp[:, h, 0:nt]
                    bdep = sp_load_tok[p]
                else:
                    btab = bgen[:, h, 0:nt]
                    bdep = t_bgen
                bi, sb_, sbfree = S_sb.next()
                ts = op(DVE, [tqk, bdep, sbfree], nc.vector.tensor_tensor, out=sb_[:, 0:nt], in0=sp_[:, 0:nt], in1=btab, op=ALU.add)
                S_ps.rel(si, ts)
                col = i
                tm1 = op(DVE, [ts], nc.vector.tensor_reduce, out=mx_all[:, col:col + 1], in_=sb_[:, 0:nt], axis=AX.X, op=ALU.max)
                tm2 = op(DVE, [tm1], nc.vector.tensor_scalar, out=nmx_all[:, col:col + 1], in0=mx_all[:, col:col + 1], scalar1=-1.0, scalar2=None, op0=ALU.mult)
                pi, pp, pfree = P_sb.next()
                tp = op(ACT, [tm2, pfree], nc.scalar.activation, out=pp[:, 0:nt], in_=sb_[:, 0:nt], func=AF.Exp, bias=nmx_all[:, col:col + 1], scale=1.0,
                        accum_out=rs_all[:, col:col + 1])
                S_sb.rel(bi, tp)
                st_[i] = dict(pi=pi, pp=pp, tp=tp, bl=bl, last_sp=(p in SPECIAL and h == 7))
                if p in SPECIAL and h == 7:
                    bsp_free[0] = ts
                    issue_sp_load()

            def emit_tr(i):
                d = st_[i]
                ti_, tps, tfree = PT_ps.next()
                PE.wait(flat([d["tp"], tfree, t_idb]))
                for j, (off, ln, vt) in enumerate(d["bl"]):
                    mm = nc.tensor.transpose(tps[0:ln, j, :], d["pp"][:, off:off + ln], identb[:])
                ttr = PE.fin(mm)
                P_sb.rel(d["pi"], ttr)
                nb = len(d["bl"])
                qi, qsb, qfree = PT_sb.next()
                if i % 2 == 0:
                    te = op(ACT, [ttr, qfree], nc.scalar.activation, out=qsb[:, 0:nb, :], in_=tps[:, 0:nb, :], func=AF.Identity)
                else:
                    te = op(DVE, [ttr, qfree], nc.vector.tensor_copy, out=qsb[:, 0:nb, :], in_=tps[:, 0:nb, :])
                PT_ps.rel(ti_, te)
                d["qi"] = qi
                d["qsb"] = qsb
                d["te"] = te

            def emit_pv(i):
                p, h = items[i]
                d = st_[i]
                PE.wait(flat([d["te"], O_free[0] if h == 0 else None]))
                nb = len(d["bl"])
                for j, (off, ln, vt) in enumerate(d["bl"]):
                    mm = nc.tensor.matmul(O_ps[:, h, :], lhsT=d["qsb"][0:ln, j, :], rhs=V[0:ln, vt, h * 64:(h + 1) * 64], start=(j == 0), stop=(j == nb - 1))
                tpv = PE.fin(mm)
                PT_sb.rel(d["qi"], tpv)
                if h == 7:
                    c0 = i - 7
                    tr_ = op(DVE, [d["tp"]], nc.vector.reciprocal, out=rinv_all[:, c0:c0 + 8], in_=rs_all[:, c0:c0 + 8])
                    ai, asb, afree = att_sb.next()
                    ta = op(DVE, [tr_, tpv, afree], nc.vector.tensor_tensor, out=asb[:].rearrange("p (h d) -> p h d", h=8), in0=O_ps[:],
                            in1=rinv_all[:, c0:c0 + 8].unsqueeze(2).to_broadcast([128, 8, 64]), op=ALU.mult)
                    O_free[0] = ta
                    PE.wait(flat([ta, AT_free[0]]))
                    for c in range(4):
                        mm = nc.tensor.transpose(AT_ps[:, c, :], asb[:, c * 128:(c + 1) * 128], identb[:])
                    tat = PE.fin(mm)
                    att_sb.rel(ai, tat)
                    te = op(ACT, [tat], nc.scalar.activation, out=mixT[:, 0:4, p * 128:(p + 1) * 128], in_=AT_ps[:], func=AF.Identity)
                    AT_free[0] = te
                del st_[i]

            for i in range(N + 2):
                if i < N:
                    emit_qk(i)
                if 1 <= i <= N:
                    emit_tr(i - 1)
                if i >= 2:
                    emit_pv(i - 2)
            barrier()
            if debug:
                dump("d_attT", mixT[:, 0:4, :].rearrange("p a b -> p (a b)"), [], BF16, [128, 4 * 2048])
                barrier()
        front.close()

        hrb_ds = [dsem(), dsem()]
        acct_ds = [dsem(), dsem(), dsem()]
        with ExitStack() as ph:
            w_out_bf = sbt(ph, "w_out_bf", [128, 8, D], BF16)
            ln1gbc = sbt(ph, "ln1gbc", [128, D], F32)
            ln1bbc = sbt(ph, "ln1bbc", [128, D], F32)
            rw = sbt(ph, "rw", [128, 8, NE], F32)
            rbbc = sbt(ph, "rbbc", [128, NE], F32)
            bd_sb = sbt(ph, "bd_sb", [NE, D], F32)
            dw = dsem()
            dcc = dsem()
            t_wout = dma(POOL, [], dw, w_out_bf[:], w_out.rearrange("(k p) n -> p k n", p=128))
            dma(SP, [], dcc, ln1gbc[:], ln1_g.partition_broadcast(128))
            dma(SP, [], dcc, ln1bbc[:], ln1_b.partition_broadcast(128))
            dma(SP, [], dcc, rw[:], router_w.rearrange("(k p) n -> p k n", p=128))
            dma(SP, [], dcc, rbbc[:], router_b.partition_broadcast(128))
            t_cc = dma(SP, [], dcc, bd_sb[:], exp_b_down[:, :])
            xr = [sbt(ph, "xr%d" % i, [128, D], F32) for i in range(2)]
            xr_ds = [dsem(), dsem()]
            xr_free = [None, None]
            mix_ps = pst(ph, "mix_ps", [128, D], F32)
            mix_free = [None]
            tr_ps = pst(ph, "tr_ps", [128, 8, 128], F32)
            tr_free = [None]
            lg_ps = pst(ph, "lg_ps", [128, NE], F32)
            lg_free = [None]
            ct_ps = pst(ph, "ct_ps", [NE, 128], F32)
            ct_free = [None]
            bd_ps = pst(ph, "bd_ps", [128, D], F32)
            bd_free = [None]
            wk = sbt(ph, "wk", [128, D], F32)
            wk_free = [None]
            x1n = sbt(ph, "x1n", [128, D], F32)
            x1n_free = [None]
            h2f = sbt(ph, "h2f", [128, 8, 128], F32)
            h2f_free = [None]
            hrf = sbt(ph, "hrf", [128, D], F32)
            hrb = [sbt(ph, "hrb%d" % i, [128, D], BF16) for i in range(2)]
            hrb_free = [None, None]
            acct = [sbt(ph, "acct%d" % i, [128, D], F32) for i in range(3)]
            acct_free = [None, None, None]
            st = sbt(ph, "st4", [128, 12], F32)
            mv = sbt(ph, "mv4", [128, 2], F32)
            rs = sbt(ph, "rs4", [128, 2], F32)
            st2 = sbt(ph, "st5", [128, 12], F32)
            mv2 = sbt(ph, "mv5", [128, 2], F32)
            rs2 = sbt(ph, "rs5", [128, 2], F32)
            rt = sbt(ph, "rt", [128, 4], F32)
            ex = sbt(ph, "ex", [128, NE], F32)
            combT = sbt(ph, "combT", [NE, 128], F32)
            combT_free = [None]
            ex_free = [None]

            ones1 = sbt(ph, "ones1", [1, 128], F32)
            rb1 = sbt(ph, "rb1", [1, NE], F32)
            bdt = sbt(ph, "bdt", [128, D], F32)
            bdt_free = [None]
            t_ones = op(DVE, [], nc.vector.memset, ones1[:], 1.0)
            drb = dsem()
            t_rb1 = dma(SP, [], drb, rb1[:], router_b.rearrange("(o n) -> o n", o=1))
            s1 = {}

            def outproj(t):
                b = t % 2
                tok = slice(t * 128, (t + 1) * 128)
                tl = dma(SP, [xr_free[b]], xr_ds[b], xr[b][:], xe[(OWN0 + t) * 128:(OWN0 + t + 1) * 128, :])
                PE.wait(flat([mix_free[0], t_wout]))
                for hf in range(2):
                    for k in range(8):
                        mm = nc.tensor.matmul(mix_ps[:, hf * 512:(hf + 1) * 512], lhsT=mixT[:, k, tok], rhs=w_out_bf[:, k, hf * 512:(hf + 1) * 512],
                                              start=(k == 0), stop=(k == 7))
                tmm = PE.fin(mm)
                s1[t] = dict(tl=tl, tmm=tmm)

            def stage1a(t):
                b = t % 2
                b3 = t % 3
                tl = s1[t]["tl"]
                tmm = s1[t]["tmm"]
                ta = op(DVE, [tmm, wk_free[0]], nc.vector.tensor_tensor, out=wk[:], in0=mix_ps[:], in1=g1bc[:], op=ALU.mult)
                mix_free[0] = ta
                tb_ = op(DVE, [ta, tl], nc.vector.scalar_tensor_tensor, out=wk[:], in0=xr[b][:], scalar=float(ALPHA), in1=wk[:], op0=ALU.mult, op1=ALU.add)
                xr_free[b] = tb_
                trs = ln_stats(wk, D, [tb_], st, mv, rs)
                tc_ = op(DVE, [trs], nc.vector.tensor_scalar, out=wk[:], in0=wk[:], scalar1=mv[:, 0:1], scalar2=rs[:, 0:1], op0=ALU.subtract, op1=ALU.mult)
                td_ = op(DVE, [tc_, t_cc], nc.vector.tensor_tensor, out=wk[:], in0=wk[:], in1=ln1gbc[:], op=ALU.mult)
                te_ = op(DVE, [td_], nc.vector.tensor_tensor, out=wk[:], in0=wk[:], in1=ln1bbc[:], op=ALU.add)
                tf_ = op(ACT, [te_, acct_free[b3]], nc.scalar.activation, out=acct[b3][:], in_=wk[:], func=AF.Identity, scale=float(ALPHA))
                trs2 = ln_stats(wk, D, [te_], st2, mv2, rs2)
                tg_ = op(DVE, [trs2, x1n_free[0]], nc.vector.tensor_scalar, out=x1n[:], in0=wk[:], scalar1=mv2[:, 0:1], scalar2=rs2[:, 0:1],
                         op0=ALU.subtract, op1=ALU.mult)
                wk_free[0] = [tf_, tg_]
                hr1 = op(DVE, [tg_], nc.vector.tensor_tensor, out=hrf[:], in0=x1n[:], in1=sc2bc[:], op=ALU.mult)
                hr2 = op(DVE, [hr1, hrb_free[b]], nc.vector.tensor_tensor, out=hrb[b][:], in0=hrf[:], in1=sh2bc[:], op=ALU.add)
                hrb_free[b] = dma(SP, [hr2], hrb_ds[b], Hx[t * 128:(t + 1) * 128, :], hrb[b][:])
                s1[t].update(tf_=tf_, tg_=tg_, hr1=hr1)

            def stage1b(t):
                tg_ = s1[t]["tg_"]
                PE.wait(flat([tg_, tr_free[0], t_idf]))
                for k in range(8):
                    mm = nc.tensor.transpose(tr_ps[:, k, :], x1n[:, k * 128:(k + 1) * 128], identf[:])
                ttr = PE.fin(mm)
                x1n_free[0] = [ttr, s1[t]["hr1"]]
                ACT.wait(flat([ttr, h2f_free[0]]))
                for k in range(8):
                    a = nc.scalar.activation(out=h2f[:, k, :], in_=tr_ps[:, k, :], func=AF.Identity, scale=modv[:, 4, k:k + 1], bias=modv[:, 5, k:k + 1])
                th = ACT.fin(a)
                tr_free[0] = th
                PE.wait(flat([th, lg_free[0], t_cc, t_ones, t_rb1]))
                for k in range(8):
                    nc.tensor.matmul(lg_ps[:, :], lhsT=h2f[:, k, :], rhs=rw[:, k, :], start=(k == 0), stop=False)
                tlg = PE.fin(nc.tensor.matmul(lg_ps[:, :], lhsT=ones1[0:1, :], rhs=rb1[0:1, :], start=False, stop=True))
                h2f_free[0] = [tlg]
                r1 = op(ACT, [tlg], nc.scalar.activation, out=lg_all[:, t, :], in_=lg_ps[:], func=AF.Identity)
                lg_free[0] = r1
                s1[t]["r1"] = r1

            def stage2a(t):
                lg = lg_all[:, t, :]
                m8 = m8_all[:, t, :]
                msk = maskall[:, t, :]
                r1 = s1[t]["r1"]
                r2 = op(DVE, [r1], nc.vector.max, out=m8, in_=lg)
                r3 = op(DVE, [r2], nc.vector.tensor_scalar, out=rt[:, 0:1], in0=m8_all[:, t, 0:1], scalar1=-1.0, scalar2=None, op0=ALU.mult)
                r4 = op(ACT, [r3, ex_free[0]], nc.scalar.activation, out=ex[:], in_=lg, func=AF.Exp, bias=rt[:, 0:1], scale=1.0)
                r5 = op(DVE, [r2], nc.vector.tensor_scalar, out=msk, in0=lg, scalar1=m8_all[:, t, 3:4], scalar2=None, op0=ALU.is_ge)
                r6 = op(DVE, [r4, r5], nc.vector.tensor_tensor, out=ex[:], in0=ex[:], in1=msk, op=ALU.mult)
                r7 = op(DVE, [r6], nc.vector.tensor_reduce, out=rt[:, 1:2], in_=ex[:], axis=AX.X, op=ALU.add)
                r8 = op(DVE, [r7], nc.vector.reciprocal, out=rt[:, 2:3], in_=rt[:, 1:2])
                r9 = op(DVE, [r8], nc.vector.tensor_scalar, out=comb[:, t, :], in0=ex[:], scalar1=rt[:, 2:3], scalar2=None, op0=ALU.mult)
                ex_free[0] = r9
                PE.wait(flat([r9, ct_free[0]]))
                tct = PE.fin(nc.tensor.transpose(ct_ps[:, :], comb[:, t, :], identf[:]))
                tcc_ = op(ACT, [tct, combT_free[0]], nc.scalar.activation, out=combT[:], in_=ct_ps[:], func=AF.Identity)
                ct_free[0] = tcc_
                PE.wait(flat([tcc_, bd_free[0]]))
                for hf in range(2):
                    mm = nc.tensor.matmul(bd_ps[:, hf * 512:(hf + 1) * 512], lhsT=combT[:, :], rhs=bd_sb[:, hf * 512:(hf + 1) * 512], start=True, stop=True)
                tbd = PE.fin(mm)
                combT_free[0] = tbd
                s1[t]["tbd"] = tbd

            def stage2b(t):
                b3 = t % 3
                u1 = op(DVE, [s1[t]["tbd"], bdt_free[0]], nc.vector.tensor_tensor, out=bdt[:], in0=bd_ps[:], in1=g2bc[:], op=ALU.mult)
                bd_free[0] = u1
                u2 = op(DVE, [u1, s1[t]["tf_"]], nc.vector.tensor_tensor, out=acct[b3][:], in0=acct[b3][:], in1=bdt[:], op=ALU.add)
                bdt_free[0] = u2
                acct_free[b3] = dma(SP, [u2], acct_ds[b3], accd[t * 128:(t + 1) * 128, :], acct[b3][:])
                del s1[t]

            outproj(0)
            stage1a(0)
            for t in range(NOWN):
                if t + 1 < NOWN:
                    outproj(t + 1)
                stage1b(t)
                if t + 1 < NOWN:
                    stage1a(t + 1)
                if t >= 1:
                    stage2b(t - 1)
                stage2a(t)
            stage2b(NOWN - 1)
            SP.wait([(d_, d_.n) for d_ in hrb_ds + acct_ds])
            barrier()
        t_hx = [(d_, d_.n) for d_ in hrb_ds]
        t_accd = [(d_, d_.n) for d_ in acct_ds]
        mid.close()

        bcreg_slot = nc.gpsimd.alloc_register("bc_slot")
        nc.gpsimd.reg_mov(bcreg_slot, NSLOT - 1)
        bcreg_w = nc.gpsimd.alloc_register("bc_w")
        nc.gpsimd.reg_mov(bcreg_w, NE * 1024 - 1)
        bcreg_b = nc.gpsimd.alloc_register("bc_b")
        nc.gpsimd.reg_mov(bcreg_b, NE * 128 - 1)
        with ExitStack() as ph:
            cst_sb = sbt(ph, "cst_sb", [128, CW], F32)
            dcs = dsem()
            t_cst = dma(SP, [], dcs, cst_sb[:], cst[:, :])
            U128 = cst_sb[:, 0:128]
            ONES = cst_sb[:, 128:256]
            USTR = cst_sb[0:NE, 256:288]
            jrow = cst_sb[:, C_J:C_J + NTILE]
            erow = cst_sb[:, C_E:C_E + NE]
            pk = cst_sb[:, C_PK:C_PK + 8]
            bigp = cst_sb[:, C_BIG:C_BIG + 1]
            prow = cst_sb[:, C_P:C_P + 1]
            cum = sbt(ph, "cum", [128, NOWN, NE], F32)
            pos_ps = pst(ph, "pos_ps", [128, NOWN, NE], F32)
            cnt_ps = pst(ph, "cnt_ps", [128, NE], F32)
            ntT_ps = pst(ph, "ntT_ps", [NE, 128], F32)
            ts_ps = pst(ph, "ts_ps", [128, NE], F32)
            cnt_sb = sbt(ph, "cnt_sb", [128, NE], F32)
            nt = sbt(ph, "nt", [128, NE], F32)
            ntT = sbt(ph, "ntT", [NE, 128], F32)
            tstart = sbt(ph, "tstart", [128, NE], F32)
            tend = sbt(ph, "tend", [128, NE], F32)
            slotf = sbt(ph, "slotf", [128, NOWN, NE], F32)
            oh = sbt(ph, "oh", [128, NOWN, NE], F32)
            tmpm = sbt(ph, "tmpm", [128, NOWN, NE], F32)
            slotsel = sbt(ph, "slotsel", [128, NOWN, 4], F32)
            gate = sbt(ph, "gate", [128, NOWN, 4], F32)
            A3 = sbt(ph, "A3", [128, NTILE, NE], F32)
            B3 = sbt(ph, "B3", [128, NTILE, NE], F32)
            used = sbt(ph, "used", [128, NTILE], F32)
            texp = sbt(ph, "texp", [128, NTILE], F32)
            cfill = sbt(ph, "cfill", [128, NTILE], F32)
            wf = sbt(ph, "wf", [128, NTILE, 8], F32)
            bf_ = sbt(ph, "bf_", [128, NTILE], F32)

            tq = op(DVE, [], nc.vector.memset, cum[:, 0, :], 0.0)
            for t in range(1, NOWN):
                tq = op(DVE, [tq], nc.vector.tensor_tensor, out=cum[:, t, :], in0=cum[:, t - 1, :], in1=maskall[:, t - 1, :], op=ALU.add)
            PE.wait(flat([tq, t_cst]))
            for t in range(NOWN):
                nc.tensor.matmul(pos_ps[:, t, :], lhsT=U128, rhs=maskall[:, t, :], start=True, stop=False)
                mm = nc.tensor.matmul(pos_ps[:, t, :], lhsT=ONES, rhs=cum[:, t, :], start=False, stop=True)
            nc.tensor.matmul(cnt_ps[:, :], lhsT=ONES, rhs=cum[:, NOWN - 1, :], start=True, stop=False)
            tpos = PE.fin(nc.tensor.matmul(cnt_ps[:, :], lhsT=ONES, rhs=maskall[:, NOWN - 1, :], start=False, stop=True))
            q = op(DVE, [tpos], nc.vector.tensor_copy, out=cnt_sb[:], in_=cnt_ps[:])
            q = op(DVE, [q], nc.vector.tensor_scalar, out=nt[:], in0=cnt_sb[:], scalar1=0.0, scalar2=None, op0=ALU.is_gt)
            for thr in [float(TS * i_) for i_ in range(1, 2048 // TS + 1) if TS * i_ < 2048]:
                q = op(DVE, [q], nc.vector.scalar_tensor_tensor, out=nt[:], in0=cnt_sb[:], scalar=thr, in1=nt[:], op0=ALU.is_gt, op1=ALU.add)
            PE.wait(flat([q, t_idf]))
            tnt = PE.fin(nc.tensor.transpose(ntT_ps[:, :], nt[:], identf[:]))
            q2 = op(DVE, [tnt], nc.vector.tensor_copy, out=ntT[:], in_=ntT_ps[:])
            PE.wait(flat([q2]))
            tts = PE.fin(nc.tensor.matmul(ts_ps[:, :], lhsT=ntT[:, :], rhs=USTR, start=True, stop=True))
            q = op(DVE, [tts], nc.vector.tensor_copy, out=tstart[:], in_=ts_ps[:])
            q = op(DVE, [q], nc.vector.tensor_tensor, out=tend[:], in0=tstart[:], in1=nt[:], op=ALU.add)
            q = op(DVE, [q], nc.vector.scalar_tensor_tensor, out=slotf[:], in0=tstart[:].unsqueeze(1).to_broadcast([128, NOWN, NE]), scalar=float(TS), in1=pos_ps[:],
                   op0=ALU.mult, op1=ALU.add)
            for k in range(4):
                q = op(DVE, [q], nc.vector.tensor_tensor, out=oh[:], in0=lg_all[:], in1=m8_all[:, :, k:k + 1].to_broadcast([128, NOWN, NE]), op=ALU.is_equal)
                q = op(DVE, [q], nc.vector.tensor_tensor, out=tmpm[:], in0=oh[:], in1=slotf[:], op=ALU.mult)
                q = op(DVE, [q], nc.vector.tensor_reduce, out=slotsel[:, :, k], in_=tmpm[:], axis=AX.X, op=ALU.add)
                q = op(DVE, [q], nc.vector.tensor_tensor, out=tmpm[:], in0=oh[:], in1=comb[:], op=ALU.mult)
                q = op(DVE, [q], nc.vector.tensor_reduce, out=gate[:, :, k], in_=tmpm[:], axis=AX.X, op=ALU.add)
            q = op(DVE, [q], nc.vector.tensor_copy, out=slot_i[:], in_=slotsel[:].rearrange("p t k -> p (t k)"))
            q = op(DVE, [q], nc.vector.tensor_scalar, out=gate2[:], in0=gate[:], scalar1=float(1.0 / 1.702), scalar2=None, op0=ALU.mult)
            q = op(DVE, [q], nc.vector.tensor_tensor, out=A3[:], in0=tstart[:].unsqueeze(1).to_broadcast([128, NTILE, NE]),
                   in1=jrow.unsqueeze(2).to_broadcast([128, NTILE, NE]), op=ALU.is_le)
            q = op(DVE, [q], nc.vector.tensor_tensor, out=B3[:], in0=tend[:].unsqueeze(1).to_broadcast([128, NTILE, NE]),
                   in1=jrow.unsqueeze(2).to_broadcast([128, NTILE, NE]), op=ALU.is_gt)
            q = op(DVE, [q], nc.vector.tensor_tensor, out=A3[:], in0=A3[:], in1=B3[:], op=ALU.mult)
            q = op(DVE, [q], nc.vector.tensor_reduce, out=used[:], in_=A3[:], axis=AX.X, op=ALU.add)
            q = op(DVE, [q], nc.vector.tensor_tensor, out=B3[:], in0=A3[:], in1=erow.unsqueeze(1).to_broadcast([128, NTILE, NE]), op=ALU.mult)
            q = op(DVE, [q], nc.vector.tensor_reduce, out=texp[:], in_=B3[:], axis=AX.X, op=ALU.add)
            q = op(DVE, [q], nc.vector.tensor_scalar, out=cfill[:], in0=used[:], scalar1=-1.0, scalar2=1.0, op0=ALU.mult, op1=ALU.add)
            q = op(DVE, [q], nc.vector.tensor_scalar, out=cfill[:], in0=cfill[:], scalar1=bigp, scalar2=None, op0=ALU.mult)
            q = op(DVE, [q], nc.vector.tensor_scalar, out=bf_[:], in0=texp[:], scalar1=1024.0, scalar2=None, op0=ALU.mult)
            q = op(DVE, [q], nc.vector.tensor_tensor, out=wf[:], in0=bf_[:].unsqueeze(2).to_broadcast([128, NTILE, 8]),
                   in1=pk.unsqueeze(1).to_broadcast([128, NTILE, 8]), op=ALU.add)
            q = op(DVE, [q], nc.vector.tensor_tensor, out=wf[:], in0=wf[:], in1=used[:].unsqueeze(2).to_broadcast([128, NTILE, 8]), op=ALU.mult)
            q = op(DVE, [q], nc.vector.tensor_tensor, out=wf[:], in0=wf[:], in1=cfill[:].unsqueeze(2).to_broadcast([128, NTILE, 8]), op=ALU.add)
            q = op(DVE, [q], nc.vector.tensor_copy, out=widx_i[:], in_=wf[:].rearrange("p j k -> p (j k)"))
            q = op(DVE, [q], nc.vector.tensor_scalar, out=bf_[:], in0=texp[:], scalar1=128.0, scalar2=prow, op0=ALU.mult, op1=ALU.add)
            q = op(DVE, [q], nc.vector.tensor_tensor, out=bf_[:], in0=bf_[:], in1=used[:], op=ALU.mult)
            q = op(DVE, [q], nc.vector.tensor_tensor, out=bf_[:], in0=bf_[:], in1=cfill[:], op=ALU.add)
            t_meta = op(DVE, [q], nc.vector.tensor_copy, out=bidx_i[:], in_=bf_[:])
            barrier()

        with ExitStack() as ph:
            hb = [sbt(ph, "hb%d" % i, [128, D], BF16) for i in range(2)]
            hb_ds = [dsem() for _ in range(2)]
            hb_free = [None] * 2
            dscat = [dsem(), dsem()]
            for t in range(NOWN):
                i = t % 2
                tl = dma(SP, [t_hx, hb_free[i]], hb_ds[i], hb[i][:], Hx[t * 128:(t + 1) * 128, :])
                POOL.wait(flat([tl, t_meta, t_zero]))
                for k in range(4):
                    nc.gpsimd.indirect_dma_start(out=Hs[:, :], out_offset=bass.IndirectOffsetOnAxis(ap=slot_i[:, t * 4 + k:t * 4 + k + 1], axis=0),
                                                 in_=hb[i][:], in_offset=None, bounds_check=bcreg_slot, oob_is_err=False).then_inc(dscat[i].sem, 16)
                    dscat[i].n += 16
                hb_free[i] = (dscat[i], dscat[i].n)
            t_scat = [(d_, d_.n) for d_ in dscat]

            wg = [sbt(ph, "wg%d" % i, [128, 8 * 2048], BF16) for i in range(3)]
            wd = [sbt(ph, "wd%d" % i, [128, 8 * 1024], BF16) for i in range(2)]
            bgt = [sbt(ph, "bgt%d" % i, [128, 16], F32) for i in range(3)]
            wg_ds = [dsem() for _ in range(3)]
            wd_ds = [dsem() for _ in range(2)]
            wg_free = [None] * 3
            wd_free = [None] * 2
            wg_tok = {}
            wd_tok = {}
            hrow = [sbt(ph, "hrow%d" % i, [128, D], BF16) for i in range(3)]
            hrow_ds = [dsem() for _ in range(3)]
            hrow_free = [None] * 3
            hT = [sbt(ph, "hTm%d" % i, [128, 8, TS], BF16) for i in range(2)]
            hT_free = [None, None]
            hT_tok = {}
            actT = [sbt(ph, "actT%d" % i, [128, 8, TS], BF16) for i in range(2)]
            actT_free = [None, None]
            tp_ps = Ring([pst(ph, "tp5_ps%d" % i, [128, 8, 128], BF16) for i in range(2)])
            g_ps = Ring([pst(ph, "g_ps%d" % i, [128, 512], F32) for i in range(2)])
            l_ps = Ring([pst(ph, "l_ps%d" % i, [128, 512], F32) for i in range(2)])
            y_ps = Ring([pst(ph, "y_ps%d" % i, [128, 512], F32) for i in range(2)])
            gc = Ring([sbt(ph, "gc%d" % i, [128, TS], F32) for i in range(2)])
            sg = Ring([sbt(ph, "sg%d" % i, [128, TS], F32) for i in range(2)])
            lc = Ring([sbt(ph, "lc%d" % i, [128, TS], F32) for i in range(2)])
            ystage = [sbt(ph, "ystage%d" % i, [128, D], F32) for i in range(2)]
            ys_free = [None, None]
            dys = [dsem(), dsem()]
            ysn = [0]
            pend = {}
            bias_tok = {}
            nrow = [0]

            ORDER = []
            lo_, hi_ = 0, NTILE - 1
            while lo_ <= hi_:
                if len(ORDER) % 3 == 2:
                    ORDER.append(hi_)
                    hi_ -= 1
                else:
                    ORDER.append(lo_)
                    lo_ += 1
            assert sorted(ORDER) == list(range(NTILE))

            def load_wg(j):
                b = j % 3
                tj = ORDER[j]
                POOL.wait(flat([wg_free[b], t_meta]))
                for k in range(8):
                    nc.gpsimd.indirect_dma_start(out=wg[b][:, k * 2048:(k + 1) * 2048], out_offset=None, in_=wgu_rows[:, :],
                                                 in_offset=bass.IndirectOffsetOnAxis(ap=widx_i[:, tj * 8 + k:tj * 8 + k + 1], axis=0),
                                                 bounds_check=bcreg_w, oob_is_err=False).then_inc(wg_ds[b].sem, 16)
                    wg_ds[b].n += 16
                nc.gpsimd.indirect_dma_start(out=bgt[b][:], out_offset=None, in_=bgu_rows[:, :],
                                             in_offset=bass.IndirectOffsetOnAxis(ap=bidx_i[:, tj:tj + 1], axis=0),
                                             bounds_check=bcreg_b, oob_is_err=False).then_inc(wg_ds[b].sem, 16)
                wg_ds[b].n += 16
                wg_tok[j] = (wg_ds[b], wg_ds[b].n)

            def load_wd(j):
                b = j % 2
                tj = ORDER[j]
                POOL.wait(flat([wd_free[b], t_meta]))
                for k in range(8):
                    nc.gpsimd.indirect_dma_start(out=wd[b][:, k * 1024:(k + 1) * 1024], out_offset=None, in_=wd_rows[:, :],
                                                 in_offset=bass.IndirectOffsetOnAxis(ap=widx_i[:, tj * 8 + k:tj * 8 + k + 1], axis=0),
                                                 bounds_check=bcreg_w, oob_is_err=False).then_inc(wd_ds[b].sem, 16)
                    wd_ds[b].n += 16
                wd_tok[j] = (wd_ds[b], wd_ds[b].n)

            def emit_rows(j):
                jb = j % 2
                tev = None
                for sidx in range(NSUB):
                    i = nrow[0] % 3
                    nrow[0] += 1
                    r0 = ORDER[j] * TS + sidx * 128
                    tl = dma(SP, [t_scat, hrow_free[i]], hrow_ds[i], hrow[i][:], Hs[r0:r0 + 128, :])
                    pi, tpp, tpfree = tp_ps.next()
                    PE.wait(flat([tl, tpfree, t_idb]))
                    for k in range(8):
                        mm = nc.tensor.transpose(tpp[:, k, :], hrow[i][:, k * 128:(k + 1) * 128], identb[:])
                    ttp = PE.fin(mm)
                    hrow_free[i] = ttp
                    tev = op(ACT, [ttp, hT_free[jb] if sidx == 0 else None], nc.scalar.activation, out=hT[jb][:, :, sidx * 128:(sidx + 1) * 128], in_=tpp[:], func=AF.Identity)
                    tp_ps.rel(pi, tev)
                hT_tok[j] = tev

            def emit_gu(j):
                b = j % 3
                jb = j % 2
                ab = j % 2
                tbias = op(DVE, [wg_tok[j]], nc.vector.tensor_scalar, out=bgt[b][:, 8:16], in0=bgt[b][:, 8:16], scalar1=1.0, scalar2=None, op0=ALU.add)
                last_tok = None
                for jc in range(8):
                    gi, gp, gfree = g_ps.next()
                    li, lp, lfree = l_ps.next()
                    PE.wait(flat([wg_tok[j], hT_tok[j], gfree, lfree]))
                    for k in range(8):
                        nc.tensor.matmul(gp[:, 0:TS], lhsT=wg[b][:, k * 2048 + jc * 128:k * 2048 + (jc + 1) * 128], rhs=hT[jb][:, k, :], start=(k == 0), stop=(k == 7))
                    for k in range(8):
                        mm = nc.tensor.matmul(lp[:, 0:TS], lhsT=wg[b][:, k * 2048 + 1024 + jc * 128:k * 2048 + 1024 + (jc + 1) * 128], rhs=hT[jb][:, k, :], start=(k == 0), stop=(k == 7))
                    tmm = PE.fin(mm)
                    bg_ap = bgt[b][:, jc:jc + 1]
                    bl_ap = bgt[b][:, 8 + jc:9 + jc]
                    ci, gct, gcfree = gc.next()
                    a1 = op(DVE, [tmm, tbias, gcfree], nc.vector.tensor_scalar, out=gct[:, 0:TS], in0=gp[:, 0:TS], scalar1=bg_ap, scalar2=7.0, op0=ALU.add, op1=ALU.min)
                    g_ps.rel(gi, a1)
                    xi, sgt, sgfree = sg.next()
                    a2 = op(ACT, [a1, sgfree], nc.scalar.activation, out=sgt[:, 0:TS], in_=gct[:, 0:TS], func=AF.Silu, scale=1.702)
                    gc.rel(ci, a2)
                    yi, lct, lcfree = lc.next()
                    a3 = op(DVE, [tmm, tbias, lcfree], nc.vector.tensor_scalar, out=lct[:, 0:TS], in0=lp[:, 0:TS], scalar1=bl_ap, scalar2=8.0, op0=ALU.add, op1=ALU.min)
                    l_ps.rel(li, a3)
                    a6 = op(DVE, [a2, a3, actT_free[ab] if jc == 0 else None], nc.vector.scalar_tensor_tensor, out=actT[ab][:, jc, :], in0=lct[:, 0:TS], scalar=-6.0, in1=sgt[:, 0:TS],
                            op0=ALU.max, op1=ALU.mult)
                    sg.rel(xi, a6)
                    lc.rel(yi, a6)
                    last_tok = a6
                hT_free[jb] = tmm
                pend[j] = last_tok
                wg_free[b] = [tmm, last_tok]
                if j + 3 < NTILE:
                    load_wg(j + 3)

            def emit_down(j):
                b = j % 2
                ab = j % 2
                tmm = None
                for sidx in range(NSUB):
                    yb = ysn[0] % 2
                    ysn[0] += 1
                    evs = []
                    for hc in range(2):
                        yi, yp, yfree = y_ps.next()
                        PE.wait(flat([pend[j], yfree, wd_tok[j]]))
                        for jc in range(8):
                            mm = nc.tensor.matmul(yp[:, :], lhsT=actT[ab][:, jc, sidx * 128:(sidx + 1) * 128], rhs=wd[b][:, jc * 1024 + hc * 512:jc * 1024 + (hc + 1) * 512],
                                                  start=(jc == 0), stop=(jc == 7))
                        tmm = PE.fin(mm)
                        ev = op(DVE, [tmm, ys_free[yb]], nc.vector.tensor_tensor, out=ystage[yb][:, hc * 512:(hc + 1) * 512], in0=yp[:, :], in1=g2bc[:, hc * 512:(hc + 1) * 512], op=ALU.mult)
                        y_ps.rel(yi, ev)
                        evs.append(ev)
                    r0 = ORDER[j] * TS + sidx * 128
                    ys_free[yb] = dma(SP, evs, dys[yb], Ys[r0:r0 + 128, :], ystage[yb][:])
                actT_free[ab] = tmm
                wd_free[b] = tmm
                del pend[j]
                if j + 2 < NTILE:
                    load_wd(j + 2)

            load_wg(0)
            load_wd(0)
            load_wg(1)
            load_wd(1)
            load_wg(2)
            emit_rows(0)
            for j in range(NTILE):
                emit_gu(j)
                if j + 1 < NTILE:
                    emit_rows(j + 1)
                emit_down(j)
            t_ys = [(d_, d_.n) for d_ in dys]
            SP.wait(t_ys)
            barrier()

        with ExitStack() as ph:
            ln2gbc = sbt(ph, "ln2gbc", [128, D], F32)
            ln2bbc = sbt(ph, "ln2bbc", [128, D], F32)
            dcc = dsem()
            dma(SP, [], dcc, ln2gbc[:], ln2_g.partition_broadcast(128))
            t_cc = dma(SP, [], dcc, ln2bbc[:], ln2_b.partition_broadcast(128))
            st = [sbt(ph, "st6_%d" % i, [128, 12], F32) for i in range(2)]
            mv = [sbt(ph, "mv6_%d" % i, [128, 2], F32) for i in range(2)]
            rs = [sbt(ph, "rs6_%d" % i, [128, 2], F32) for i in range(2)]
            ob = [sbt(ph, "ob%d" % i, [128, D], F32) for i in range(2)]
            ob_free = [None, None]
            accs = [sbt(ph, "accs%d" % i, [128, D], F32) for i in range(2)]
            accs_ds = [dsem(), dsem()]
            accs_free = [None, None]
            yk = [[sbt(ph, "yk%d_%d" % (i, k), [128, D], F32) for k in range(4)] for i in range(2)]
            yk_ds = [dsem(), dsem()]
            yk_free = [None, None]
            ssum = [sbt(ph, "ssum%d" % i, [128, D], F32) for i in range(2)]
            ssum_free = [None, None]
            dout = [dsem(), dsem()]
            tyk = {}

            def issue_gather(t):
                b = t % 2
                POOL.wait(flat([yk_free[b], t_ys]))
                for k in range(4):
                    nc.gpsimd.indirect_dma_start(out=yk[b][k][:], out_offset=None, in_=Ys[:, :],
                                                 in_offset=bass.IndirectOffsetOnAxis(ap=slot_i[:, t * 4 + k:t * 4 + k + 1], axis=0),
                                                 bounds_check=bcreg_slot, oob_is_err=False).then_inc(yk_ds[b].sem, 16)
                    yk_ds[b].n += 16
                tyk[t] = (yk_ds[b], yk_ds[b].n)

            issue_gather(0)
            issue_gather(1)
            trs_t = {}

            def stage_a(t):
                b = t % 2
                tla = dma(SP, [t_accd, accs_free[b]], accs_ds[b], accs[b][:], accd[t * 128:(t + 1) * 128, :])
                q = op(DVE, [tyk[t], tla, ssum_free[b]], nc.vector.scalar_tensor_tensor, out=ssum[b][:], in0=yk[b][0][:], scalar=gate2[:, t, 0:1], in1=accs[b][:], op0=ALU.mult, op1=ALU.add)
                accs_free[b] = q
                for k in range(1, 4):
                    q = op(DVE, [q], nc.vector.scalar_tensor_tensor, out=ssum[b][:], in0=yk[b][k][:], scalar=gate2[:, t, k:k + 1], in1=ssum[b][:], op0=ALU.mult, op1=ALU.add)
                yk_free[b] = q
                if t + 2 < NOWN:
                    issue_gather(t + 2)
                trs_t[t] = ln_stats(ssum[b], D, [q], st[b], mv[b], rs[b])

            def stage_b(t):
                b = t % 2
                trs = trs_t.pop(t)
                tn_ = op(DVE, [trs], nc.vector.scalar_tensor_tensor, out=rs[b][:, 1:2], in0=mv[b][:, 0:1], scalar=-1.0, in1=rs[b][:, 0:1], op0=ALU.mult, op1=ALU.mult)
                t1_ = op(ACT, [tn_, ob_free[b]], nc.scalar.activation, out=ob[b][:], in_=ssum[b][:], func=AF.Identity, scale=rs[b][:, 0:1], bias=rs[b][:, 1:2])
                ssum_free[b] = t1_
                t2_ = op(DVE, [t1_, t_cc], nc.vector.tensor_tensor, out=ob[b][:], in0=ob[b][:], in1=ln2gbc[:], op=ALU.mult)
                t3_ = op(DVE, [t2_], nc.vector.tensor_tensor, out=ob[b][:], in0=ob[b][:], in1=ln2bbc[:], op=ALU.add)
                ob_free[b] = dma(SP, [t3_], dout[b], out[t * 128:(t + 1) * 128, :], ob[b][:])

            stage_a(0)
            for t in range(NOWN):
                if t + 1 < NOWN:
                    stage_a(t + 1)
                stage_b(t)
            SP.wait([(d_, d_.n) for d_ in dout])
    return nc


def _bias_tables(rpb):
    c = np.arange(64)
    cs = np.clip(c - 8, 0, 48)
    kc = np.arange(64)
    colvalid = (kc[None, :] >= cs[:, None]) & (kc[None, :] < cs[:, None] + 16)
    dcidx = np.clip(kc[None, :] - c[:, None] + 15, 0, 30)

    def table(r0, lr0, a, nrows, width):
        T = np.full((2, 64, 8, width), NEG, np.float32)
        for i in range(2):
            r = r0 + lr0 + i
            rs = min(max(r - 4, 0), 120)
            for w in range(nrows):
                kr = r0 - 4 + a + w
                if kr < rs or kr >= rs + 8:
                    continue
                dr = kr - r + 7
                vals = rpb[:, dr, :][:, dcidx]
                vals = np.where(colvalid[None], vals, np.float32(NEG))
                T[i, :, :, w * 64:(w + 1) * 64] = vals.transpose(1, 0, 2)
            T[i, :, :, nrows * 64:nrows * 64 + 256] = 0.0
        return T.reshape(128, 8 * width)

    gen = table(32, 8, 8, 9, 832)
    sp = {}
    for q in range(4):
        r0 = 32 * q
        sp[q] = np.stack([table(r0, 2 * p, SPECIAL[p][0], SPECIAL[p][1], 1024) for p in SP_ORDER])
    return gen, sp


def _consts(core):
    c = np.zeros((128, CW), np.float32)
    p = np.arange(128)
    c[:, 0:128] = (p[:, None] < p[None, :]).astype(np.float32)
    c[:, 128:256] = 1.0
    e = np.arange(NE)
    r = (e - 4 * core) % NE
    c[0:NE, 256:288] = (r[:, None] < r[None, :]).astype(np.float32)
    c[:, C_J:C_J + NTILE] = np.arange(NTILE)[None, :]
    c[:, C_E:C_E + NE] = e[None, :]
    c[:, C_PK:C_PK + 8] = np.arange(8)[None, :] * 128 + p[:, None]
    c[:, C_BIG] = np.where(p == 0, 0.0, BIGIDX)
    c[:, C_P] = p
    return c


_NC_CACHE = {}


def kernel(x, c, ctx, c_ctx, ada_w, ada_b, w_in, rpb, sgu_ln_g, sgu_ln_b, sgu_w, sgu_b,
           w_out, ln1_g, ln1_b, ln2_g, ln2_b, router_w, router_b,
           exp_w_gu, exp_b_gu, exp_w_down, exp_b_down, _debug=False):
    f = lambda a: np.ascontiguousarray(np.asarray(a, dtype=np.float32))
    x, c, ctx, c_ctx = f(x), f(c), f(ctx), f(c_ctx)
    ada_w0, ada_b0 = f(ada_w)[0], f(ada_b)[0]
    gen, sp = _bias_tables(f(rpb)[0])
    shared = {
        "ada_w": ada_w0,
        "adabT": np.ascontiguousarray(ada_b0.reshape(48, 128).T),
        "ada_b": ada_b0,
        "w_in": f(w_in)[0],
        "w_out": f(w_out)[0],
        "bias_gen": gen,
        "sgu_ln_g": f(sgu_ln_g)[0],
        "sgu_ln_b": f(sgu_ln_b)[0],
        "wsT": np.ascontiguousarray(f(sgu_w)[0].transpose(2, 0, 1).reshape(128, 512)),
        "sgu_bv": np.ascontiguousarray(f(sgu_b)[0].reshape(512)),
        "ln1_g": f(ln1_g)[0], "ln1_b": f(ln1_b)[0], "ln2_g": f(ln2_g)[0], "ln2_b": f(ln2_b)[0],
        "router_w": f(router_w)[0], "router_b": f(router_b)[0],
        "exp_w_gu": f(exp_w_gu)[0],
        "bgu_rows": np.ascontiguousarray(f(exp_b_gu)[0].reshape(NE, 16, 128).transpose(0, 2, 1).reshape(NE * 128, 16)),
        "zeros": np.zeros((512, D), dtype=ml_dtypes.bfloat16),
        "exp_w_down": f(exp_w_down)[0],
        "exp_b_down": f(exp_b_down)[0],
        "ident": np.eye(128, dtype=np.float32),
    }
    in_maps = []
    for j in range(NCORES):
        b, q = j // 4, j % 4
        r0 = 32 * q
        xg = x[b].reshape(128, 64, D)
        xe = np.zeros((NT_ALL * 128, D), np.float32)
        xev = xe[:NEXT_T * 128].reshape(40, 64, D)
        lo, hi = r0 - 4, r0 + 36
        slo, shi = max(lo, 0), min(hi, 128)
        xev[slo - lo:shi - lo] = xg[slo:shi]
        xe[NEXT_T * 128:] = ctx[b]
        cc = np.stack([c[b], c_ctx], axis=1)
        cT = np.ascontiguousarray(cc.reshape(8, 128, 2).transpose(1, 0, 2).reshape(128, 16))
        m = dict(shared)
        m["xe"] = xe
        m["cT"] = cT
        m["bias_sp"] = sp[q]
        m["cst"] = _consts(j)
        in_maps.append(m)
    key = bool(_debug)
    if key not in _NC_CACHE:
        _NC_CACHE[key] = build(debug=key)
    nc = _NC_CACHE[key]
    res = run_bass_kernel_spmd(nc, in_maps, core_ids=list(range(NCORES)))
    outs = [np.asarray(r["out"], dtype=np.float32) for r in res.results]
    full = np.concatenate(outs, axis=0).reshape(2, 8192, D)
    if _debug:
        return full, res.results
    return full
```

```python
import numpy as np
import ml_dtypes
from contextlib import ExitStack
import concourse.bass as bass
import concourse.mybir as mybir
from concourse.bass_utils import run_bass_kernel_spmd

F32 = mybir.dt.float32
BF16 = mybir.dt.bfloat16
I32 = mybir.dt.int32
AF = mybir.ActivationFunctionType
ALU = mybir.AluOpType
AX = mybir.AxisListType

NCORES = 8
D = 1024
NEXT_T = 20
NT_ALL = 22
OWN0 = 2
NOWN = 16
NTOK = NOWN * 128
NEG = -30000.0
ALPHA = 2.0 ** 0.25
LN_EPS = 1e-5
NE = 32
TS = 384
NSUB = TS // 128
NTILE = (4 * 2048 + NE * (TS - 1)) // TS
NSLOT = NTILE * TS
C_J = 288
C_E = C_J + NTILE
C_PK = C_E + NE
C_BIG = C_PK + 8
C_P = C_BIG + 1
CW = 384
assert C_P < CW
BIGIDX = 4.0e6
SPECIAL = {0: (0, 12), 1: (2, 10), 14: (28, 9), 15: (28, 11)}
SP_ORDER = [0, 1, 14, 15]
PAIR_ORDER = [0, 2, 3, 4, 1, 5, 6, 7, 8, 14, 9, 10, 11, 15, 12, 13]


class Eng:
    def __init__(s, es, nc, eng, name):
        s.eng = eng
        s.name = name
        s.sem = es.enter_context(nc.semaphore("sem_" + name))
        s.n = 0
        s.seen = {}

    def wait(s, toks):
        for t in toks:
            if t is None:
                continue
            e, c = t
            if s.seen.get(e, 0) < c:
                s.eng.wait_ge(e.sem, c)
                s.seen[e] = c

    def fin(s, inst):
        inst.then_inc(s.sem, 1)
        s.n += 1
        return (s, s.n)

    def last(s):
        return (s, s.n) if s.n > 0 else None


class DSem:
    def __init__(s, es, nc, name):
        s.sem = es.enter_context(nc.semaphore("dsem_" + name))
        s.n = 0


def flat(deps):
    out = []
    for d in deps:
        if d is None:
            continue
        if isinstance(d, list):
            out.extend(flat(d))
        else:
            out.append(d)
    return out


class Ring:
    def __init__(s, bufs):
        s.bufs = bufs
        s.free = [None] * len(bufs)
        s.i = 0

    def next(s):
        i = s.i % len(s.bufs)
        s.i += 1
        return i, s.bufs[i], s.free[i]

    def rel(s, i, tok):
        s.free[i] = tok


def build(debug=False):
    nc = bass.Bass("TRN2", target_bir_lowering=False)

    def din(name, shape):
        return nc.dram_tensor(name, shape, F32, kind="ExternalInput").ap()

    xe = din("xe", [NT_ALL * 128, D])
    cT = din("cT", [128, 16])
    ada_w = din("ada_w", [D, 6 * D])
    adabT = din("adabT", [128, 48])
    ada_b = din("ada_b", [6 * D])
    w_in = din("w_in", [D, 2560])
    w_out = din("w_out", [D, D])
    bias_gen = din("bias_gen", [128, 8 * 832])
    bias_sp = din("bias_sp", [4, 128, 8 * 1024])
    sgu_ln_g = din("sgu_ln_g", [512])
    sgu_ln_b = din("sgu_ln_b", [512])
    wsT = din("wsT", [128, 512])
    sgu_bv = din("sgu_bv", [512])
    ln1_g = din("ln1_g", [D])
    ln1_b = din("ln1_b", [D])
    ln2_g = din("ln2_g", [D])
    ln2_b = din("ln2_b", [D])
    router_w = din("router_w", [D, NE])
    router_b = din("router_b", [NE])
    exp_w_gu = din("exp_w_gu", [NE, D, 2 * D])
    exp_w_down = din("exp_w_down", [NE, D, D])
    exp_b_down = din("exp_b_down", [NE, D])
    ident = din("ident", [128, 128])
    cst = din("cst", [128, CW])
    zeros = nc.dram_tensor("zeros", [512, D], BF16, kind="ExternalInput").ap()
    bgu_rows = din("bgu_rows", [NE * 128, 16])
    out = nc.dram_tensor("out", [NTOK, D], F32, kind="ExternalOutput").ap()
    Hx = nc.dram_tensor("Hx", [NTOK, D], BF16, kind="Internal").ap()
    Hs = nc.dram_tensor("Hs", [NSLOT, D], BF16, kind="Internal").ap()
    Ys = nc.dram_tensor("Ys", [NSLOT, D], F32, kind="Internal").ap()
    accd = nc.dram_tensor("accd", [NTOK, D], F32, kind="Internal").ap()
    wgu_rows = exp_w_gu.rearrange("e r n -> (e r) n")
    wd_rows = exp_w_down.rearrange("e r n -> (e r) n")
    dbg = {}
    if debug:
        for nm, shp in [("d_KT", [128, 4 * 2816]), ("d_V", [128, 22 * 512]), ("d_QT", [128, 4 * 2048]),
                        ("d_sguT", [128, 4 * 2048]), ("d_attT", [128, 4 * 2048]), ("d_acc", [128, 16 * 1024]),
                        ("d_h2T", [128, 8 * 2048]), ("d_comb", [128, 16 * 32]), ("d_ada", [128, 96]),
                        ("d_g1bc", [128, 1024])]:
            dbg[nm] = nc.dram_tensor(nm, shp, F32, kind="ExternalOutput").ap()

    with ExitStack() as es:
        PE = Eng(es, nc, nc.tensor, "pe")
        ACT = Eng(es, nc, nc.scalar, "act")
        DVE = Eng(es, nc, nc.vector, "dve")
        POOL = Eng(es, nc, nc.gpsimd, "pool")
        SP = Eng(es, nc, nc.sync, "sp")
        ENGS = [PE, ACT, DVE, POOL, SP]
        nds = [0]

        def dsem():
            nds[0] += 1
            return DSem(es, nc, "d%d" % nds[0])

        def op(E, deps, fn, *a, **k):
            E.wait(flat(deps))
            return E.fin(fn(*a, **k))

        def dma(Q, deps, ds, out_, in_):
            Q.wait(flat(deps))
            Q.eng.dma_start(out=out_, in_=in_).then_inc(ds.sem, 16)
            ds.n += 16
            return (ds, ds.n)

        def barrier():
            toks = [e.last() for e in ENGS]
            for e in ENGS:
                e.wait([t for t in toks if t is not None and t[0] is not e])

        dbg_sem = dsem()

        dummy = es.enter_context(nc.sbuf_tensor("dummy_t", [128, 8], F32))

        def dump(name, src_ap, deps, dt=F32, shape=None):
            if not debug:
                return
            W = src_ap.shape[-1]
            with ExitStack() as tmp:
                CH = 1024
                stg = tmp.enter_context(nc.sbuf_tensor("stg_" + name, [128, CH], F32))
                t2 = None
                for c0 in range(0, W, CH):
                    c1 = min(W, c0 + CH)
                    t = op(DVE, flat([deps, t2]), nc.vector.tensor_copy, out=stg[:, 0:c1 - c0], in_=src_ap[:, c0:c1])
                    t2 = dma(SP, [t], dbg_sem, dbg[name][:, c0:c1], stg[:, 0:c1 - c0])
                SP.wait([t2])
                op(DVE, [t2], nc.vector.memset, dummy[:], 0.0)

        def sbt(stack, name, shape, dt):
            return stack.enter_context(nc.sbuf_tensor(name, shape, dt))

        def pst(stack, name, shape, dt):
            return stack.enter_context(nc.psum_tensor(name, shape, dt))

        identf = sbt(es, "identf", [128, 128], F32)
        identb = sbt(es, "identb", [128, 128], BF16)
        adaT = sbt(es, "adaT", [128, 48, 2], F32)
        modv = sbt(es, "modv", [128, 6, 8], F32)
        g1bc = sbt(es, "g1bc", [128, D], F32)
        g2bc = sbt(es, "g2bc", [128, D], F32)
        m05 = sbt(es, "m05", [128, 1], F32)
        sh2bc = sbt(es, "sh2bc", [128, D], F32)
        sc2bc = sbt(es, "sc2bc", [128, D], F32)
        comb = sbt(es, "comb", [128, NOWN, NE], F32)
        slot_i = sbt(es, "slot_i", [128, NOWN * 4], I32)
        gate2 = sbt(es, "gate2", [128, NOWN, 4], F32)
        widx_i = sbt(es, "widx_i", [128, NTILE * 8], I32)
        bidx_i = sbt(es, "bidx_i", [128, NTILE], I32)
        dc0 = dsem()
        dc1 = dsem()
        t_idf = dma(SP, [], dc0, identf[:], ident[:, :])
        t_idb = dma(POOL, [], dc1, identb[:], ident[:, :])
        t_m05 = op(POOL, [], nc.gpsimd.memset, m05[:], -0.5)

        def ln_stats(src, width, deps, st_t, mv_t, rs_t):
            nchunk = width // 512
            toks = []
            for c in range(nchunk):
                toks.append(op(DVE, deps, nc.vector.bn_stats, out=st_t[:, c * 6:(c + 1) * 6], in_=src[:, c * 512:(c + 1) * 512]))
            t = op(DVE, toks, nc.vector.bn_aggr, out=mv_t[:, 0:2], in_=st_t[:, 0:6 * nchunk])
            t = op(POOL, [t, t_m05], nc.gpsimd.tensor_scalar, out=rs_t[:, 1:2], in0=mv_t[:, 1:2], scalar1=LN_EPS, scalar2=None, op0=ALU.add)
            t = op(POOL, [t], nc.gpsimd.tensor_tensor, out=rs_t[:, 0:1], in0=rs_t[:, 1:2], in1=m05[:], op=ALU.pow)
            return t

        with ExitStack() as ph:
            cT_sb = sbt(ph, "cT_sb", [128, 8, 2], F32)
            sT = sbt(ph, "sT", [128, 8, 2], F32)
            srep = sbt(ph, "srep", [128, 8, 128], F32)
            adab_sb = sbt(ph, "adab_sb", [128, 48], F32)
            wb = [sbt(ph, "adaw%d" % i, [128, 8, D], F32) for i in range(2)]
            wb_ds = [dsem(), dsem()]
            bb = [sbt(ph, "adabb%d" % i, [128, D], F32) for i in range(4)]
            ada_ps = pst(ph, "ada_ps", [128, 48, 2], F32)
            bc_ps = pst(ph, "bc_ps", [128, D], F32)
            t1 = dma(SP, [], dc0, cT_sb[:], cT.rearrange("p (k m) -> p k m", m=2))
            t2 = dma(SP, [], dc0, adab_sb[:], adabT[:, :])
            for i_, s_ in enumerate((2, 3, 4, 5)):
                dma(SP, [], dc0, bb[i_][:], ada_b[s_ * D:(s_ + 1) * D].partition_broadcast(128))
            tc_all = (dc0, dc0.n)
            t_idf = tc_all
            t_s = op(ACT, [tc_all], nc.scalar.activation, out=sT[:], in_=cT_sb[:], func=AF.Silu)
            t_rep = op(DVE, [t_s], nc.vector.tensor_copy, out=srep[:], in_=sT[:, :, 0:1].to_broadcast([128, 8, 128]))
            ada_w_v = ada_w.rearrange("(k p) n -> p k n", p=128)
            wfree = [None, None]
            t_last_mm = None
            bc_toks = {}
            for s in range(6):
                b = s % 2
                tl = dma(SP, [wfree[b]], wb_ds[b], wb[b][:], ada_w_v[:, :, s * D:(s + 1) * D])
                PE.wait([tl, t_s])
                for cc in range(8):
                    for k in range(8):
                        mm = nc.tensor.matmul(ada_ps[:, s * 8 + cc, :], lhsT=wb[b][:, k, cc * 128:(cc + 1) * 128], rhs=sT[:, k, :],
                                              start=(k == 0), stop=(k == 7))
                t_last_mm = PE.fin(mm)
                if s in (2, 3, 4, 5):
                    PE.wait([t_rep])
                    for hf in range(2):
                        for k in range(8):
                            mm = nc.tensor.matmul(bc_ps[:, hf * 512:(hf + 1) * 512], lhsT=srep[:, k, :], rhs=wb[b][:, k, hf * 512:(hf + 1) * 512],
                                                  start=(k == 0), stop=(k == 7))
                    t_last_mm = PE.fin(mm)
                    dst = {2: g1bc, 3: sh2bc, 4: sc2bc, 5: g2bc}[s]
                    if s == 4:
                        tt = op(DVE, [t_last_mm, tc_all], nc.vector.scalar_tensor_tensor, out=dst[:], in0=bc_ps[:], scalar=1.0, in1=bb[s - 2][:], op0=ALU.add, op1=ALU.add)
                    else:
                        tt = op(DVE, [t_last_mm, tc_all], nc.vector.tensor_tensor, out=dst[:], in0=bc_ps[:], in1=bb[s - 2][:], op=ALU.add)
                    bc_toks[s] = tt
                    PE.wait([tt])
                wfree[b] = t_last_mm
            t_ada = op(DVE, [t_last_mm, tc_all], nc.vector.tensor_tensor, out=adaT[:], in0=ada_ps[:],
                       in1=adab_sb[:].unsqueeze(2).to_broadcast([128, 48, 2]), op=ALU.add)
            tm = []
            tm.append(op(DVE, [t_ada], nc.vector.tensor_scalar, out=modv[:, 0, :], in0=adaT[:, 8:16, 0], scalar1=1.0, scalar2=None, op0=ALU.add))
            tm.append(op(DVE, [t_ada], nc.vector.tensor_copy, out=modv[:, 1, :], in_=adaT[:, 0:8, 0]))
            tm.append(op(DVE, [t_ada], nc.vector.tensor_scalar, out=modv[:, 2, :], in0=adaT[:, 8:16, 1], scalar1=1.0, scalar2=None, op0=ALU.add))
            tm.append(op(DVE, [t_ada], nc.vector.tensor_copy, out=modv[:, 3, :], in_=adaT[:, 0:8, 1]))
            tm.append(op(DVE, [t_ada], nc.vector.tensor_scalar, out=modv[:, 4, :], in0=adaT[:, 32:40, 0], scalar1=1.0, scalar2=None, op0=ALU.add))
            t_mod = op(DVE, [t_ada], nc.vector.tensor_copy, out=modv[:, 5, :], in_=adaT[:, 24:32, 0])
            dump("d_ada", adaT[:].rearrange("p a b -> p (a b)"), [t_mod])
            dump("d_g1bc", g1bc[:], [t_mod])
            barrier()

        lg_all = sbt(es, "lg_all", [128, NOWN, NE], F32)
        m8_all = sbt(es, "m8_all", [128, NOWN, 8], F32)
        maskall = sbt(es, "maskall", [128, NOWN, NE], F32)
        mid = ExitStack()
        mixT = sbt(mid, "mixT", [128, 8, NTOK], BF16)
        front = ExitStack()
        KT = sbt(front, "KT", [128, 4, 2816], BF16)
        V = sbt(front, "V", [128, NT_ALL, 512], BF16)
        QT = sbt(front, "QT", [128, 4, NTOK], BF16)

        with ExitStack() as ph:
            w_in_bf = sbt(ph, "w_in_bf", [128, 8, 2560], BF16)
            wsT_bf = sbt(ph, "wsT_bf", [128, 4, 128], BF16)
            lngbc = sbt(ph, "lngbc", [128, 512], F32)
            lnbbc = sbt(ph, "lnbbc", [128, 512], F32)
            bsbc = sbt(ph, "bsbc", [128, 512], F32)
            dw = dsem()
            dcc = dsem()
            w_in_v = w_in.rearrange("(k p) n -> p k n", p=128)
            for k in range(8):
                t_win = dma(POOL, [], dw, w_in_bf[:, k, :], w_in_v[:, k, :])
            t_win = dma(POOL, [], dw, wsT_bf[:], wsT.rearrange("p (g i) -> p g i", g=4))
            dma(SP, [], dcc, lngbc[:], sgu_ln_g.partition_broadcast(128))
            dma(SP, [], dcc, lnbbc[:], sgu_ln_b.partition_broadcast(128))
            t_cc = dma(SP, [], dcc, bsbc[:], sgu_bv.partition_broadcast(128))

            xt = [sbt(ph, "xt%d" % i, [128, D], F32) for i in range(2)]
            xt_ds = [dsem(), dsem()]
            xt_free = [None, None]
            xn = [sbt(ph, "xn%d" % i, [128, D], BF16) for i in range(2)]
            xn_free = [None, None]
            st = [sbt(ph, "st%d" % i, [128, 12], F32) for i in range(2)]
            mv = [sbt(ph, "mv%d" % i, [128, 2], F32) for i in range(2)]
            rs = [sbt(ph, "rs%d" % i, [128, 2], F32) for i in range(2)]
            tp_ps = Ring([pst(ph, "tp_ps%d" % i, [128, 8, 128], BF16) for i in range(2)])
            hT = [sbt(ph, "hT%d" % i, [128, 8, 512], BF16) for i in range(2)]
            hT_free = [None, None]
            acc_ps = Ring([pst(ph, "acc_ps%d" % i, [128, 512], F32) for i in range(4)])
            sg_ps = pst(ph, "sg_ps", [128, 4, 128], F32)
            sg_free = [None]
            Gg = [sbt(ph, "Gg%d" % i, [128, 512], F32) for i in range(2)]
            Gn = [sbt(ph, "Gn%d" % i, [128, 512], F32) for i in range(2)]
            Gb = [sbt(ph, "Gb%d" % i, [128, 512], BF16) for i in range(2)]
            Gfree = [None, None]
            gst = [sbt(ph, "gst%d" % i, [128, 6], F32) for i in range(2)]
            gmv = [sbt(ph, "gmv%d" % i, [128, 2], F32) for i in range(2)]
            grs = [sbt(ph, "grs%d" % i, [128, 2], F32) for i in range(2)]
            stmp = [sbt(ph, "stmp%d" % i, [128, 512], F32) for i in range(2)]
            stmp_free = [None, None]
            evq = [0]

            def evac_copy(deps, out_, in_, scale=None, func=None):
                evq[0] += 1
                if func is not None:
                    return op(ACT, deps, nc.scalar.activation, out=out_, in_=in_, func=func)
                if evq[0] % 2 == 0:
                    if scale is None:
                        return op(ACT, deps, nc.scalar.activation, out=out_, in_=in_, func=AF.Identity)
                    return op(ACT, deps, nc.scalar.activation, out=out_, in_=in_, func=AF.Identity, scale=float(scale))
                if scale is None:
                    return op(DVE, deps, nc.vector.tensor_copy, out=out_, in_=in_)
                return op(DVE, deps, nc.vector.tensor_scalar, out=out_, in0=in_, scalar1=float(scale), scalar2=None, op0=ALU.mult)

            groups = [list(range(g * 4, min(g * 4 + 4, NT_ALL))) for g in range(6)]
            ti_glob = 0
            gcount = 0
            for gi, tiles in enumerate(groups):
                hb = gi % 2
                ntok = 128 * len(tiles)
                ev_toks = []
                for sl, ti in enumerate(tiles):
                    b = ti_glob % 2
                    ti_glob += 1
                    is_ctx = ti >= NEXT_T
                    tl = dma(SP, [xt_free[b]], xt_ds[b], xt[b][:], xe[ti * 128:(ti + 1) * 128, :])
                    trs = ln_stats(xt[b], D, [tl], st[b], mv[b], rs[b])
                    tn = op(DVE, [trs, xn_free[b]], nc.vector.tensor_scalar, out=xn[b][:], in0=xt[b][:], scalar1=mv[b][:, 0:1], scalar2=rs[b][:, 0:1],
                            op0=ALU.subtract, op1=ALU.mult)
                    xt_free[b] = tn
                    pi, pt, pfree = tp_ps.next()
                    PE.wait(flat([tn, pfree, t_idb]))
                    for k in range(8):
                        mm = nc.tensor.transpose(pt[:, k, :], xn[b][:, k * 128:(k + 1) * 128], identb[:])
                    ttp = PE.fin(mm)
                    xn_free[b] = ttp
                    msc, msh = (2, 3) if is_ctx else (0, 1)
                    ACT.wait(flat([ttp, hT_free[hb], t_mod]))
                    for k in range(8):
                        a = nc.scalar.activation(out=hT[hb][:, k, sl * 128:(sl + 1) * 128], in_=pt[:, k, :], func=AF.Identity,
                                                 scale=modv[:, msc, k:k + 1], bias=modv[:, msh, k:k + 1])
                    tev = ACT.fin(a)
                    tp_ps.rel(pi, tev)
                    ev_toks.append(tev)
                hready = ev_toks[-1]
                tok0 = tiles[0] * 128
                mm_last = None
                for c in range(4):
                    ai, ap_, afree = acc_ps.next()
                    PE.wait(flat([hready, afree, t_win]))
                    for k in range(8):
                        mm = nc.tensor.matmul(ap_[:, 0:ntok], lhsT=w_in_bf[:, k, 512 + c * 128:512 + (c + 1) * 128], rhs=hT[hb][:, k, 0:ntok],
                                              start=(k == 0), stop=(k == 7))
                    tmm = PE.fin(mm)
                    te = evac_copy([tmm], KT[:, c, tok0:tok0 + ntok], ap_[:, 0:ntok])
                    acc_ps.rel(ai, te)
                for sl, ti in enumerate(tiles):
                    ai, ap_, afree = acc_ps.next()
                    PE.wait(flat([hready, afree, t_win]))
                    for k in range(8):
                        mm = nc.tensor.matmul(ap_[:, :], lhsT=hT[hb][:, k, sl * 128:(sl + 1) * 128], rhs=w_in_bf[:, k, 1024:1536],
                                              start=(k == 0), stop=(k == 7))
                    tmm = PE.fin(mm)
                    te = evac_copy([tmm], V[:, ti, :], ap_[:, :])
                    acc_ps.rel(ai, te)
                    mm_last = tmm
                own = [(sl, ti) for sl, ti in enumerate(tiles) if OWN0 <= ti < OWN0 + NOWN]
                if own:
                    s0 = own[0][0] * 128
                    nown = 128 * len(own)
                    o0 = (own[0][1] - OWN0) * 128
                    for c in range(4):
                        ai, ap_, afree = acc_ps.next()
                        PE.wait(flat([hready, afree]))
                        for k in range(8):
                            mm = nc.tensor.matmul(ap_[:, 0:nown], lhsT=w_in_bf[:, k, c * 128:(c + 1) * 128], rhs=hT[hb][:, k, s0:s0 + nown],
                                                  start=(k == 0), stop=(k == 7))
                        tmm = PE.fin(mm)
                        te = evac_copy([tmm], QT[:, c, o0:o0 + nown], ap_[:, 0:nown], scale=0.125)
                        acc_ps.rel(ai, te)
                    ut_toks = []
                    for c in range(4):
                        ai, ap_, afree = acc_ps.next()
                        PE.wait(flat([hready, afree]))
                        for k in range(8):
                            mm = nc.tensor.matmul(ap_[:, 0:nown], lhsT=w_in_bf[:, k, 1536 + c * 128:1536 + (c + 1) * 128], rhs=hT[hb][:, k, s0:s0 + nown],
                                                  start=(k == 0), stop=(k == 7))
                        tmm = PE.fin(mm)
                        te = evac_copy([tmm], mixT[:, 4 + c, o0:o0 + nown], ap_[:, 0:nown], func=AF.Gelu_apprx_tanh)
                        acc_ps.rel(ai, te)
                        ut_toks.append(te)
                    for sl, ti in own:
                        gb = gcount % 2
                        gcount += 1
                        ot = (ti - OWN0) * 128
                        ai, ap_, afree = acc_ps.next()
                        PE.wait(flat([hready, afree]))
                        for k in range(8):
                            mm = nc.tensor.matmul(ap_[:, :], lhsT=hT[hb][:, k, sl * 128:(sl + 1) * 128], rhs=w_in_bf[:, k, 2048:2560],
                                                  start=(k == 0), stop=(k == 7))
                        tmm = PE.fin(mm)
                        mm_last = tmm
                        tg = op(ACT, [tmm, Gfree[gb]], nc.scalar.activation, out=Gg[gb][:], in_=ap_[:, :], func=AF.Gelu_apprx_tanh)
                        acc_ps.rel(ai, tg)
                        trs = ln_stats(Gg[gb], 512, [tg], gst[gb], gmv[gb], grs[gb])
                        t1_ = op(DVE, [trs], nc.vector.tensor_scalar, out=Gn[gb][:], in0=Gg[gb][:], scalar1=gmv[gb][:, 0:1], scalar2=grs[gb][:, 0:1],
                                 op0=ALU.subtract, op1=ALU.mult)
                        t2_ = op(DVE, [t1_, t_cc], nc.vector.tensor_tensor, out=Gn[gb][:], in0=Gn[gb][:], in1=lngbc[:], op=ALU.mult)
                        t3_ = op(DVE, [t2_], nc.vector.tensor_tensor, out=Gb[gb][:], in0=Gn[gb][:], in1=lnbbc[:], op=ALU.add)
                        PE.wait(flat([t3_, sg_free[0], t_win]))
                        for g in range(4):
                            mm = nc.tensor.matmul(sg_ps[:, g, :], lhsT=Gb[gb][:, g * 128:(g + 1) * 128], rhs=wsT_bf[:, g, :], start=True, stop=True)
                        tsg = PE.fin(mm)
                        Gfree[gb] = tsg
                        t4_ = op(DVE, [tsg, stmp_free[gb], t_cc], nc.vector.tensor_tensor, out=stmp[gb][:], in0=sg_ps[:].rearrange("p g i -> p (g i)"), in1=bsbc[:], op=ALU.add)
                        sg_free[0] = t4_
                        t5_ = op(DVE, [t4_, ut_toks], nc.vector.tensor_tensor, out=mixT[:, 4:8, ot:ot + 128],
                                 in0=stmp[gb][:].rearrange("p (g i) -> p g i", g=4), in1=mixT[:, 4:8, ot:ot + 128], op=ALU.mult)
                        stmp_free[gb] = t5_
                hT_free[hb] = mm_last
            barrier()
            if debug:
                dump("d_KT", KT[:].rearrange("p a b -> p (a b)"), [], BF16, [128, 4 * 2816])
                dump("d_V", V[:].rearrange("p a b -> p (a b)"), [], BF16, [128, 22 * 512])
                dump("d_QT", QT[:].rearrange("p a b -> p (a b)"), [], BF16, [128, 4 * 2048])
                dump("d_sguT", mixT[:, 4:8, :].rearrange("p a b -> p (a b)"), [], BF16, [128, 4 * 2048])
                barrier()

        with ExitStack() as ph:
            bgen = sbt(ph, "bgen", [128, 8, 832], F32)
            bsp = sbt(ph, "bsp", [128, 8, 1024], F32)
            db = dsem()
            dbs = dsem()
            t_bgen = dma(SP, [], db, bgen[:], bias_gen.rearrange("p (h k) -> p h k", h=8))
            dz = dsem()
            S_ps = Ring([pst(ph, "S_ps%d" % i, [128, 1024], F32) for i in range(2)])
            PT_ps = Ring([pst(ph, "PT_ps%d" % i, [128, 8, 128], BF16) for i in range(2)])
            O_ps = pst(ph, "O_ps", [128, 8, 64], F32)
            O_free = [None]
            AT_ps = pst(ph, "AT_ps", [128, 4, 128], BF16)
            AT_free = [None]
            S_sb = Ring([sbt(ph, "S_sb%d" % i, [128, 1024], F32) for i in range(2)])
            P_sb = Ring([sbt(ph, "P_sb%d" % i, [128, 1024], BF16) for i in range(2)])
            PT_sb = Ring([sbt(ph, "PT_sb%d" % i, [128, 8, 128], BF16) for i in range(3)])
            att_sb = Ring([sbt(ph, "att_sb%d" % i, [128, 512], BF16) for i in range(2)])
            mx_all = sbt(ph, "mx_all", [128, 128], F32)
            nmx_all = sbt(ph, "nmx_all", [128, 128], F32)
            rs_all = sbt(ph, "rs_all", [128, 128], F32)
            rinv_all = sbt(ph, "rinv_all", [128, 128], F32)

            items = []
            sp_load_tok = {}
            for p in PAIR_ORDER:
                for h in range(8):
                    items.append((p, h))
            N = len(items)
            st_ = {}
            bsp_free = [None]
            sp_next = [0]

            def issue_sp_load():
                i = sp_next[0]
                if i >= len(SP_ORDER):
                    return
                p = SP_ORDER[i]
                sp_load_tok[p] = dma(SP, [bsp_free[0]], dbs, bsp[:], bias_sp[i].rearrange("p (h k) -> p h k", h=8))
                sp_next[0] += 1

            issue_sp_load()
            for i_ in range(NTILE):
                dma(POOL, [], dz, Hs[i_ * TS:(i_ + 1) * TS, :], zeros[0:TS, :])
            t_zero = (dz, dz.n)

            def blocks_for(p):
                a, nrows = SPECIAL.get(p, (2 * p, 9))
                nk = nrows * 64
                bl = []
                for j in range(nk // 128):
                    bl.append((j * 128, 128, a // 2 + j))
                if nk % 128:
                    bl.append((nk - 64, 64, a // 2 + nk // 128))
                bl.append((nk, 128, NEXT_T))
                bl.append((nk + 128, 128, NEXT_T + 1))
                return a, nrows, nk, bl

            def emit_qk(i):
                p, h = items[i]
                a, nrows, nk, bl = blocks_for(p)
                nt = nk + 256
                c = h // 2
                pb = (h % 2) * 64
                si, sp_, sfree = S_ps.next()
                PE.wait(flat([sfree]))
                q_ap = QT[pb:pb + 64, c, p * 128:(p + 1) * 128]
                k0 = a * 64
                nc.tensor.matmul(sp_[:, 0:512], lhsT=q_ap, rhs=KT[pb:pb + 64, c, k0:k0 + 512], start=True, stop=True)
                nc.tensor.matmul(sp_[:, 512:nk], lhsT=q_ap, rhs=KT[pb:pb + 64, c, k0 + 512:k0 + nk], start=True, stop=True)
                tqk = PE.fin(nc.tensor.matmul(sp_[:, nk:nt], lhsT=q_ap, rhs=KT[pb:pb + 64, c, 2560:2816], start=True, stop=True))
                if p in SPECIAL:
                    btab = bsp[:, h, 0:nt]
                    bdep = sp_load_tok[p]
                else:
                    btab = bgen[:, h, 0:nt]
                    bdep = t_bgen
                bi, sb_, sbfree = S_sb.next()
                ts = op(DVE, [tqk, bdep, sbfree], nc.vector.tensor_tensor, out=sb_[:, 0:nt], in0=sp_[:, 0:nt], in1=btab, op=ALU.add)
                S_ps.rel(si, ts)
                col = i
                tm1 = op(DVE, [ts], nc.vector.tensor_reduce, out=mx_all[:, col:col + 1], in_=sb_[:, 0:nt], axis=AX.X, op=ALU.max)
                tm2 = op(DVE, [tm1], nc.vector.tensor_scalar, out=nmx_all[:, col:col + 1], in0=mx_all[:, col:col + 1], scalar1=-1.0, scalar2=None, op0=ALU.mult)
                pi, pp, pfree = P_sb.next()
                tp = op(ACT, [tm2, pfree], nc.scalar.activation, out=pp[:, 0:nt], in_=sb_[:, 0:nt], func=AF.Exp, bias=nmx_all[:, col:col + 1], scale=1.0,
                        accum_out=rs_all[:, col:col + 1])
                S_sb.rel(bi, tp)
                st_[i] = dict(pi=pi, pp=pp, tp=tp, bl=bl, last_sp=(p in SPECIAL and h == 7))
                if p in SPECIAL and h == 7:
                    bsp_free[0] = ts
                    issue_sp_load()

            def emit_tr(i):
                d = st_[i]
                ti_, tps, tfree = PT_ps.next()
                PE.wait(flat([d["tp"], tfree, t_idb]))
                for j, (off, ln, vt) in enumerate(d["bl"]):
                    mm = nc.tensor.transpose(tps[0:ln, j, :], d["pp"][:, off:off + ln], identb[:])
                ttr = PE.fin(mm)
                P_sb.rel(d["pi"], ttr)
                nb = len(d["bl"])
                qi, qsb, qfree = PT_sb.next()
                if i % 2 == 0:
                    te = op(ACT, [ttr, qfree], nc.scalar.activation, out=qsb[:, 0:nb, :], in_=tps[:, 0:nb, :], func=AF.Identity)
                else:
                    te = op(DVE, [ttr, qfree], nc.vector.tensor_copy, out=qsb[:, 0:nb, :], in_=tps[:, 0:nb, :])
                PT_ps.rel(ti_, te)
                d["qi"] = qi
                d["qsb"] = qsb
                d["te"] = te

            def emit_pv(i):
                p, h = items[i]
                d = st_[i]
                PE.wait(flat([d["te"], O_free[0] if h == 0 else None]))
                nb = len(d["bl"])
                for j, (off, ln, vt) in enumerate(d["bl"]):
                    mm = nc.tensor.matmul(O_ps[:, h, :], lhsT=d["qsb"][0:ln, j, :], rhs=V[0:ln, vt, h * 64:(h + 1) * 64], start=(j == 0), stop=(j == nb - 1))
                tpv = PE.fin(mm)
                PT_sb.rel(d["qi"], tpv)
                if h == 7:
                    c0 = i - 7
                    tr_ = op(DVE, [d["tp"]], nc.vector.reciprocal, out=rinv_all[:, c0:c0 + 8], in_=rs_all[:, c0:c0 + 8])
                    ai, asb, afree = att_sb.next()
                    ta = op(DVE, [tr_, tpv, afree], nc.vector.tensor_tensor, out=asb[:].rearrange("p (h d) -> p h d", h=8), in0=O_ps[:],
                            in1=rinv_all[:, c0:c0 + 8].unsqueeze(2).to_broadcast([128, 8, 64]), op=ALU.mult)
                    O_free[0] = ta
                    PE.wait(flat([ta, AT_free[0]]))
                    for c in range(4):
                        mm = nc.tensor.transpose(AT_ps[:, c, :], asb[:, c * 128:(c + 1) * 128], identb[:])
                    tat = PE.fin(mm)
                    att_sb.rel(ai, tat)
                    te = op(ACT, [tat], nc.scalar.activation, out=mixT[:, 0:4, p * 128:(p + 1) * 128], in_=AT_ps[:], func=AF.Identity)
                    AT_free[0] = te
                del st_[i]

            for i in range(N + 2):
                if i < N:
                    emit_qk(i)
                if 1 <= i <= N:
                    emit_tr(i - 1)
                if i >= 2:
                    emit_pv(i - 2)
            barrier()
            if debug:
                dump("d_attT", mixT[:, 0:4, :].rearrange("p a b -> p (a b)"), [], BF16, [128, 4 * 2048])
                barrier()
        front.close()

        hrb_ds = [dsem(), dsem()]
        acct_ds = [dsem(), dsem(), dsem()]
        with ExitStack() as ph:
            w_out_bf = sbt(ph, "w_out_bf", [128, 8, D], BF16)
            ln1gbc = sbt(ph, "ln1gbc", [128, D], F32)
            ln1bbc = sbt(ph, "ln1bbc", [128, D], F32)
            rw = sbt(ph, "rw", [128, 8, NE], F32)
            rbbc = sbt(ph, "rbbc", [128, NE], F32)
            bd_sb = sbt(ph, "bd_sb", [NE, D], F32)
            dw = dsem()
            dcc = dsem()
            t_wout = dma(POOL, [], dw, w_out_bf[:], w_out.rearrange("(k p) n -> p k n", p=128))
            dma(SP, [], dcc, ln1gbc[:], ln1_g.partition_broadcast(128))
            dma(SP, [], dcc, ln1bbc[:], ln1_b.partition_broadcast(128))
            dma(SP, [], dcc, rw[:], router_w.rearrange("(k p) n -> p k n", p=128))
            dma(SP, [], dcc, rbbc[:], router_b.partition_broadcast(128))
            t_cc = dma(SP, [], dcc, bd_sb[:], exp_b_down[:, :])
            xr = [sbt(ph, "xr%d" % i, [128, D], F32) for i in range(2)]
            xr_ds = [dsem(), dsem()]
            xr_free = [None, None]
            mix_ps = pst(ph, "mix_ps", [128, D], F32)
            mix_free = [None]
            tr_ps = pst(ph, "tr_ps", [128, 8, 128], F32)
            tr_free = [None]
            lg_ps = pst(ph, "lg_ps", [128, NE], F32)
            lg_free = [None]
            ct_ps = pst(ph, "ct_ps", [NE, 128], F32)
            ct_free = [None]
            bd_ps = pst(ph, "bd_ps", [128, D], F32)
            bd_free = [None]
            wk = sbt(ph, "wk", [128, D], F32)
            wk_free = [None]
            x1n = sbt(ph, "x1n", [128, D], F32)
            x1n_free = [None]
            h2f = sbt(ph, "h2f", [128, 8, 128], F32)
            h2f_free = [None]
            hrf = sbt(ph, "hrf", [128, D], F32)
            hrb = [sbt(ph, "hrb%d" % i, [128, D], BF16) for i in range(2)]
            hrb_free = [None, None]
            acct = [sbt(ph, "acct%d" % i, [128, D], F32) for i in range(3)]
            acct_free = [None, None, None]
            st = sbt(ph, "st4", [128, 12], F32)
            mv = sbt(ph, "mv4", [128, 2], F32)
            rs = sbt(ph, "rs4", [128, 2], F32)
            st2 = sbt(ph, "st5", [128, 12], F32)
            mv2 = sbt(ph, "mv5", [128, 2], F32)
            rs2 = sbt(ph, "rs5", [128, 2], F32)
            rt = sbt(ph, "rt", [128, 4], F32)
            ex = sbt(ph, "ex", [128, NE], F32)
            combT = sbt(ph, "combT", [NE, 128], F32)
            combT_free = [None]
            ex_free = [None]

            ones1 = sbt(ph, "ones1", [1, 128], F32)
            rb1 = sbt(ph, "rb1", [1, NE], F32)
            bdt = sbt(ph, "bdt", [128, D], F32)
            bdt_free = [None]
            t_ones = op(DVE, [], nc.vector.memset, ones1[:], 1.0)
            drb = dsem()
            t_rb1 = dma(SP, [], drb, rb1[:], router_b.rearrange("(o n) -> o n", o=1))
            s1 = {}

            def outproj(t):
                b = t % 2
                tok = slice(t * 128, (t + 1) * 128)
                tl = dma(SP, [xr_free[b]], xr_ds[b], xr[b][:], xe[(OWN0 + t) * 128:(OWN0 + t + 1) * 128, :])
                PE.wait(flat([mix_free[0], t_wout]))
                for hf in range(2):
                    for k in range(8):
                        mm = nc.tensor.matmul(mix_ps[:, hf * 512:(hf + 1) * 512], lhsT=mixT[:, k, tok], rhs=w_out_bf[:, k, hf * 512:(hf + 1) * 512],
                                              start=(k == 0), stop=(k == 7))
                tmm = PE.fin(mm)
                s1[t] = dict(tl=tl, tmm=tmm)

            def stage1a(t):
                b = t % 2
                b3 = t % 3
                tl = s1[t]["tl"]
                tmm = s1[t]["tmm"]
                ta = op(DVE, [tmm, wk_free[0]], nc.vector.tensor_tensor, out=wk[:], in0=mix_ps[:], in1=g1bc[:], op=ALU.mult)
                mix_free[0] = ta
                tb_ = op(DVE, [ta, tl], nc.vector.scalar_tensor_tensor, out=wk[:], in0=xr[b][:], scalar=float(ALPHA), in1=wk[:], op0=ALU.mult, op1=ALU.add)
                xr_free[b] = tb_
                trs = ln_stats(wk, D, [tb_], st, mv, rs)
                tc_ = op(DVE, [trs], nc.vector.tensor_scalar, out=wk[:], in0=wk[:], scalar1=mv[:, 0:1], scalar2=rs[:, 0:1], op0=ALU.subtract, op1=ALU.mult)
                td_ = op(DVE, [tc_, t_cc], nc.vector.tensor_tensor, out=wk[:], in0=wk[:], in1=ln1gbc[:], op=ALU.mult)
                te_ = op(DVE, [td_], nc.vector.tensor_tensor, out=wk[:], in0=wk[:], in1=ln1bbc[:], op=ALU.add)
                tf_ = op(ACT, [te_, acct_free[b3]], nc.scalar.activation, out=acct[b3][:], in_=wk[:], func=AF.Identity, scale=float(ALPHA))
                trs2 = ln_stats(wk, D, [te_], st2, mv2, rs2)
                tg_ = op(DVE, [trs2, x1n_free[0]], nc.vector.tensor_scalar, out=x1n[:], in0=wk[:], scalar1=mv2[:, 0:1], scalar2=rs2[:, 0:1],
                         op0=ALU.subtract, op1=ALU.mult)
                wk_free[0] = [tf_, tg_]
                hr1 = op(DVE, [tg_], nc.vector.tensor_tensor, out=hrf[:], in0=x1n[:], in1=sc2bc[:], op=ALU.mult)
                hr2 = op(DVE, [hr1, hrb_free[b]], nc.vector.tensor_tensor, out=hrb[b][:], in0=hrf[:], in1=sh2bc[:], op=ALU.add)
                hrb_free[b] = dma(SP, [hr2], hrb_ds[b], Hx[t * 128:(t + 1) * 128, :], hrb[b][:])
                s1[t].update(tf_=tf_, tg_=tg_, hr1=hr1)

            def stage1b(t):
                tg_ = s1[t]["tg_"]
                PE.wait(flat([tg_, tr_free[0], t_idf]))
                for k in range(8):
                    mm = nc.tensor.transpose(tr_ps[:, k, :], x1n[:, k * 128:(k + 1) * 128], identf[:])
                ttr = PE.fin(mm)
                x1n_free[0] = [ttr, s1[t]["hr1"]]
                ACT.wait(flat([ttr, h2f_free[0]]))
                for k in range(8):
                    a = nc.scalar.activation(out=h2f[:, k, :], in_=tr_ps[:, k, :], func=AF.Identity, scale=modv[:, 4, k:k + 1], bias=modv[:, 5, k:k + 1])
                th = ACT.fin(a)
                tr_free[0] = th
                PE.wait(flat([th, lg_free[0], t_cc, t_ones, t_rb1]))
                for k in range(8):
                    nc.tensor.matmul(lg_ps[:, :], lhsT=h2f[:, k, :], rhs=rw[:, k, :], start=(k == 0), stop=False)
                tlg = PE.fin(nc.tensor.matmul(lg_ps[:, :], lhsT=ones1[0:1, :], rhs=rb1[0:1, :], start=False, stop=True))
                h2f_free[0] = [tlg]
                r1 = op(ACT, [tlg], nc.scalar.activation, out=lg_all[:, t, :], in_=lg_ps[:], func=AF.Identity)
                lg_free[0] = r1
                s1[t]["r1"] = r1

            def stage2a(t):
                lg = lg_all[:, t, :]
                m8 = m8_all[:, t, :]
                msk = maskall[:, t, :]
                r1 = s1[t]["r1"]
                r2 = op(DVE, [r1], nc.vector.max, out=m8, in_=lg)
                r3 = op(DVE, [r2], nc.vector.tensor_scalar, out=rt[:, 0:1], in0=m8_all[:, t, 0:1], scalar1=-1.0, scalar2=None, op0=ALU.mult)
                r4 = op(ACT, [r3, ex_free[0]], nc.scalar.activation, out=ex[:], in_=lg, func=AF.Exp, bias=rt[:, 0:1], scale=1.0)
                r5 = op(DVE, [r2], nc.vector.tensor_scalar, out=msk, in0=lg, scalar1=m8_all[:, t, 3:4], scalar2=None, op0=ALU.is_ge)
                r6 = op(DVE, [r4, r5], nc.vector.tensor_tensor, out=ex[:], in0=ex[:], in1=msk, op=ALU.mult)
                r7 = op(DVE, [r6], nc.vector.tensor_reduce, out=rt[:, 1:2], in_=ex[:], axis=AX.X, op=ALU.add)
                r8 = op(DVE, [r7], nc.vector.reciprocal, out=rt[:, 2:3], in_=rt[:, 1:2])
                r9 = op(DVE, [r8], nc.vector.tensor_scalar, out=comb[:, t, :], in0=ex[:], scalar1=rt[:, 2:3], scalar2=None, op0=ALU.mult)
                ex_free[0] = r9
                PE.wait(flat([r9, ct_free[0]]))
                tct = PE.fin(nc.tensor.transpose(ct_ps[:, :], comb[:, t, :], identf[:]))
                tcc_ = op(ACT, [tct, combT_free[0]], nc.scalar.activation, out=combT[:], in_=ct_ps[:], func=AF.Identity)
                ct_free[0] = tcc_
                PE.wait(flat([tcc_, bd_free[0]]))
                for hf in range(2):
                    mm = nc.tensor.matmul(bd_ps[:, hf * 512:(hf + 1) * 512], lhsT=combT[:, :], rhs=bd_sb[:, hf * 512:(hf + 1) * 512], start=True, stop=True)
                tbd = PE.fin(mm)
                combT_free[0] = tbd
                s1[t]["tbd"] = tbd

            def stage2b(t):
                b3 = t % 3
                u1 = op(DVE, [s1[t]["tbd"], bdt_free[0]], nc.vector.tensor_tensor, out=bdt[:], in0=bd_ps[:], in1=g2bc[:], op=ALU.mult)
                bd_free[0] = u1
                u2 = op(DVE, [u1, s1[t]["tf_"]], nc.vector.tensor_tensor, out=acct[b3][:], in0=acct[b3][:], in1=bdt[:], op=ALU.add)
                bdt_free[0] = u2
                acct_free[b3] = dma(SP, [u2], acct_ds[b3], accd[t * 128:(t + 1) * 128, :], acct[b3][:])
                del s1[t]

            outproj(0)
            stage1a(0)
            for t in range(NOWN):
                if t + 1 < NOWN:
                    outproj(t + 1)
                stage1b(t)
                if t + 1 < NOWN:
                    stage1a(t + 1)
                if t >= 1:
                    stage2b(t - 1)
                stage2a(t)
            stage2b(NOWN - 1)
            SP.wait([(d_, d_.n) for d_ in hrb_ds + acct_ds])
            barrier()
        t_hx = [(d_, d_.n) for d_ in hrb_ds]
        t_accd = [(d_, d_.n) for d_ in acct_ds]
        mid.close()

        bcreg_slot = nc.gpsimd.alloc_register("bc_slot")
        nc.gpsimd.reg_mov(bcreg_slot, NSLOT - 1)
        bcreg_w = nc.gpsimd.alloc_register("bc_w")
        nc.gpsimd.reg_mov(bcreg_w, NE * 1024 - 1)
        bcreg_b = nc.gpsimd.alloc_register("bc_b")
        nc.gpsimd.reg_mov(bcreg_b, NE * 128 - 1)
        with ExitStack() as ph:
            cst_sb = sbt(ph, "cst_sb", [128, CW], F32)
            dcs = dsem()
            t_cst = dma(SP, [], dcs, cst_sb[:], cst[:, :])
            U128 = cst_sb[:, 0:128]
            ONES = cst_sb[:, 128:256]
            USTR = cst_sb[0:NE, 256:288]
            jrow = cst_sb[:, C_J:C_J + NTILE]
            erow = cst_sb[:, C_E:C_E + NE]
            pk = cst_sb[:, C_PK:C_PK + 8]
            bigp = cst_sb[:, C_BIG:C_BIG + 1]
            prow = cst_sb[:, C_P:C_P + 1]
            cum = sbt(ph, "cum", [128, NOWN, NE], F32)
            pos_ps = pst(ph, "pos_ps", [128, NOWN, NE], F32)
            cnt_ps = pst(ph, "cnt_ps", [128, NE], F32)
            ntT_ps = pst(ph, "ntT_ps", [NE, 128], F32)
            ts_ps = pst(ph, "ts_ps", [128, NE], F32)
            cnt_sb = sbt(ph, "cnt_sb", [128, NE], F32)
            nt = sbt(ph, "nt", [128, NE], F32)
            ntT = sbt(ph, "ntT", [NE, 128], F32)
            tstart = sbt(ph, "tstart", [128, NE], F32)
            tend = sbt(ph, "tend", [128, NE], F32)
            slotf = sbt(ph, "slotf", [128, NOWN, NE], F32)
            oh = sbt(ph, "oh", [128, NOWN, NE], F32)
            tmpm = sbt(ph, "tmpm", [128, NOWN, NE], F32)
            slotsel = sbt(ph, "slotsel", [128, NOWN, 4], F32)
            gate = sbt(ph, "gate", [128, NOWN, 4], F32)
            A3 = sbt(ph, "A3", [128, NTILE, NE], F32)
            B3 = sbt(ph, "B3", [128, NTILE, NE], F32)
            used = sbt(ph, "used", [128, NTILE], F32)
            texp = sbt(ph, "texp", [128, NTILE], F32)
            cfill = sbt(ph, "cfill", [128, NTILE], F32)
            wf = sbt(ph, "wf", [128, NTILE, 8], F32)
            bf_ = sbt(ph, "bf_", [128, NTILE], F32)

            tq = op(DVE, [], nc.vector.memset, cum[:, 0, :], 0.0)
            for t in range(1, NOWN):
                tq = op(DVE, [tq], nc.vector.tensor_tensor, out=cum[:, t, :], in0=cum[:, t - 1, :], in1=maskall[:, t - 1, :], op=ALU.add)
            PE.wait(flat([tq, t_cst]))
            for t in range(NOWN):
                nc.tensor.matmul(pos_ps[:, t, :], lhsT=U128, rhs=maskall[:, t, :], start=True, stop=False)
                mm = nc.tensor.matmul(pos_ps[:, t, :], lhsT=ONES, rhs=cum[:, t, :], start=False, stop=True)
            nc.tensor.matmul(cnt_ps[:, :], lhsT=ONES, rhs=cum[:, NOWN - 1, :], start=True, stop=False)
            tpos = PE.fin(nc.tensor.matmul(cnt_ps[:, :], lhsT=ONES, rhs=maskall[:, NOWN - 1, :], start=False, stop=True))
            q = op(DVE, [tpos], nc.vector.tensor_copy, out=cnt_sb[:], in_=cnt_ps[:])
            q = op(DVE, [q], nc.vector.tensor_scalar, out=nt[:], in0=cnt_sb[:], scalar1=0.0, scalar2=None, op0=ALU.is_gt)
            for thr in [float(TS * i_) for i_ in range(1, 2048 // TS + 1) if TS * i_ < 2048]:
                q = op(DVE, [q], nc.vector.scalar_tensor_tensor, out=nt[:], in0=cnt_sb[:], scalar=thr, in1=nt[:], op0=ALU.is_gt, op1=ALU.add)
            PE.wait(flat([q, t_idf]))
            tnt = PE.fin(nc.tensor.transpose(ntT_ps[:, :], nt[:], identf[:]))
            q2 = op(DVE, [tnt], nc.vector.tensor_copy, out=ntT[:], in_=ntT_ps[:])
            PE.wait(flat([q2]))
            tts = PE.fin(nc.tensor.matmul(ts_ps[:, :], lhsT=ntT[:, :], rhs=USTR, start=True, stop=True))
            q = op(DVE, [tts], nc.vector.tensor_copy, out=tstart[:], in_=ts_ps[:])
            q = op(DVE, [q], nc.vector.tensor_tensor, out=tend[:], in0=tstart[:], in1=nt[:], op=ALU.add)
            q = op(DVE, [q], nc.vector.scalar_tensor_tensor, out=slotf[:], in0=tstart[:].unsqueeze(1).to_broadcast([128, NOWN, NE]), scalar=float(TS), in1=pos_ps[:],
                   op0=ALU.mult, op1=ALU.add)
            for k in range(4):
                q = op(DVE, [q], nc.vector.tensor_tensor, out=oh[:], in0=lg_all[:], in1=m8_all[:, :, k:k + 1].to_broadcast([128, NOWN, NE]), op=ALU.is_equal)
                q = op(DVE, [q], nc.vector.tensor_tensor, out=tmpm[:], in0=oh[:], in1=slotf[:], op=ALU.mult)
                q = op(DVE, [q], nc.vector.tensor_reduce, out=slotsel[:, :, k], in_=tmpm[:], axis=AX.X, op=ALU.add)
                q = op(DVE, [q], nc.vector.tensor_tensor, out=tmpm[:], in0=oh[:], in1=comb[:], op=ALU.mult)
                q = op(DVE, [q], nc.vector.tensor_reduce, out=gate[:, :, k], in_=tmpm[:], axis=AX.X, op=ALU.add)
            q = op(DVE, [q], nc.vector.tensor_copy, out=slot_i[:], in_=slotsel[:].rearrange("p t k -> p (t k)"))
            q = op(DVE, [q], nc.vector.tensor_scalar, out=gate2[:], in0=gate[:], scalar1=float(1.0 / 1.702), scalar2=None, op0=ALU.mult)
            q = op(DVE, [q], nc.vector.tensor_tensor, out=A3[:], in0=tstart[:].unsqueeze(1).to_broadcast([128, NTILE, NE]),
                   in1=jrow.unsqueeze(2).to_broadcast([128, NTILE, NE]), op=ALU.is_le)
            q = op(DVE, [q], nc.vector.tensor_tensor, out=B3[:], in0=tend[:].unsqueeze(1).to_broadcast([128, NTILE, NE]),
                   in1=jrow.unsqueeze(2).to_broadcast([128, NTILE, NE]), op=ALU.is_gt)
            q = op(DVE, [q], nc.vector.tensor_tensor, out=A3[:], in0=A3[:], in1=B3[:], op=ALU.mult)
            q = op(DVE, [q], nc.vector.tensor_reduce, out=used[:], in_=A3[:], axis=AX.X, op=ALU.add)
            q = op(DVE, [q], nc.vector.tensor_tensor, out=B3[:], in0=A3[:], in1=erow.unsqueeze(1).to_broadcast([128, NTILE, NE]), op=ALU.mult)
            q = op(DVE, [q], nc.vector.tensor_reduce, out=texp[:], in_=B3[:], axis=AX.X, op=ALU.add)
            q = op(DVE, [q], nc.vector.tensor_scalar, out=cfill[:], in0=used[:], scalar1=-1.0, scalar2=1.0, op0=ALU.mult, op1=ALU.add)
            q = op(DVE, [q], nc.vector.tensor_scalar, out=cfill[:], in0=cfill[:], scalar1=bigp, scalar2=None, op0=ALU.mult)
            q = op(DVE, [q], nc.vector.tensor_scalar, out=bf_[:], in0=texp[:], scalar1=1024.0, scalar2=None, op0=ALU.mult)
            q = op(DVE, [q], nc.vector.tensor_tensor, out=wf[:], in0=bf_[:].unsqueeze(2).to_broadcast([128, NTILE, 8]),
                   in1=pk.unsqueeze(1).to_broadcast([128, NTILE, 8]), op=ALU.add)
            q = op(DVE, [q], nc.vector.tensor_tensor, out=wf[:], in0=wf[:], in1=used[:].unsqueeze(2).to_broadcast([128, NTILE, 8]), op=ALU.mult)
            q = op(DVE, [q], nc.vector.tensor_tensor, out=wf[:], in0=wf[:], in1=cfill[:].unsqueeze(2).to_broadcast([128, NTILE, 8]), op=ALU.add)
            q = op(DVE, [q], nc.vector.tensor_copy, out=widx_i[:], in_=wf[:].rearrange("p j k -> p (j k)"))
            q = op(DVE, [q], nc.vector.tensor_scalar, out=bf_[:], in0=texp[:], scalar1=128.0, scalar2=prow, op0=ALU.mult, op1=ALU.add)
            q = op(DVE, [q], nc.vector.tensor_tensor, out=bf_[:], in0=bf_[:], in1=used[:], op=ALU.mult)
            q = op(DVE, [q], nc.vector.tensor_tensor, out=bf_[:], in0=bf_[:], in1=cfill[:], op=ALU.add)
            t_meta = op(DVE, [q], nc.vector.tensor_copy, out=bidx_i[:], in_=bf_[:])
            barrier()

        with ExitStack() as ph:
            hb = [sbt(ph, "hb%d" % i, [128, D], BF16) for i in range(2)]
            hb_ds = [dsem() for _ in range(2)]
            hb_free = [None] * 2
            dscat = [dsem(), dsem()]
            for t in range(NOWN):
                i = t % 2
                tl = dma(SP, [t_hx, hb_free[i]], hb_ds[i], hb[i][:], Hx[t * 128:(t + 1) * 128, :])
                POOL.wait(flat([tl, t_meta, t_zero]))
                for k in range(4):
                    nc.gpsimd.indirect_dma_start(out=Hs[:, :], out_offset=bass.IndirectOffsetOnAxis(ap=slot_i[:, t * 4 + k:t * 4 + k + 1], axis=0),
                                                 in_=hb[i][:], in_offset=None, bounds_check=bcreg_slot, oob_is_err=False).then_inc(dscat[i].sem, 16)
                    dscat[i].n += 16
                hb_free[i] = (dscat[i], dscat[i].n)
            t_scat = [(d_, d_.n) for d_ in dscat]

            wg = [sbt(ph, "wg%d" % i, [128, 8 * 2048], BF16) for i in range(3)]
            wd = [sbt(ph, "wd%d" % i, [128, 8 * 1024], BF16) for i in range(2)]
            bgt = [sbt(ph, "bgt%d" % i, [128, 16], F32) for i in range(3)]
            wg_ds = [dsem() for _ in range(3)]
            wd_ds = [dsem() for _ in range(2)]
            wg_free = [None] * 3
            wd_free = [None] * 2
            wg_tok = {}
            wd_tok = {}
            hrow = [sbt(ph, "hrow%d" % i, [128, D], BF16) for i in range(3)]
            hrow_ds = [dsem() for _ in range(3)]
            hrow_free = [None] * 3
            hT = [sbt(ph, "hTm%d" % i, [128, 8, TS], BF16) for i in range(2)]
            hT_free = [None, None]
            hT_tok = {}
            actT = [sbt(ph, "actT%d" % i, [128, 8, TS], BF16) for i in range(2)]
            actT_free = [None, None]
            tp_ps = Ring([pst(ph, "tp5_ps%d" % i, [128, 8, 128], BF16) for i in range(2)])
            g_ps = Ring([pst(ph, "g_ps%d" % i, [128, 512], F32) for i in range(2)])
            l_ps = Ring([pst(ph, "l_ps%d" % i, [128, 512], F32) for i in range(2)])
            y_ps = Ring([pst(ph, "y_ps%d" % i, [128, 512], F32) for i in range(2)])
            gc = Ring([sbt(ph, "gc%d" % i, [128, TS], F32) for i in range(2)])
            sg = Ring([sbt(ph, "sg%d" % i, [128, TS], F32) for i in range(2)])
            lc = Ring([sbt(ph, "lc%d" % i, [128, TS], F32) for i in range(2)])
            ystage = [sbt(ph, "ystage%d" % i, [128, D], F32) for i in range(2)]
            ys_free = [None, None]
            dys = [dsem(), dsem()]
            ysn = [0]
            pend = {}
            bias_tok = {}
            nrow = [0]

            ORDER = []
            lo_, hi_ = 0, NTILE - 1
            while lo_ <= hi_:
                if len(ORDER) % 3 == 2:
                    ORDER.append(hi_)
                    hi_ -= 1
                else:
                    ORDER.append(lo_)
                    lo_ += 1
            assert sorted(ORDER) == list(range(NTILE))

            def load_wg(j):
                b = j % 3
                tj = ORDER[j]
                POOL.wait(flat([wg_free[b], t_meta]))
                for k in range(8):
                    nc.gpsimd.indirect_dma_start(out=wg[b][:, k * 2048:(k + 1) * 2048], out_offset=None, in_=wgu_rows[:, :],
                                                 in_offset=bass.IndirectOffsetOnAxis(ap=widx_i[:, tj * 8 + k:tj * 8 + k + 1], axis=0),
                                                 bounds_check=bcreg_w, oob_is_err=False).then_inc(wg_ds[b].sem, 16)
                    wg_ds[b].n += 16
                nc.gpsimd.indirect_dma_start(out=bgt[b][:], out_offset=None, in_=bgu_rows[:, :],
                                             in_offset=bass.IndirectOffsetOnAxis(ap=bidx_i[:, tj:tj + 1], axis=0),
                                             bounds_check=bcreg_b, oob_is_err=False).then_inc(wg_ds[b].sem, 16)
                wg_ds[b].n += 16
                wg_tok[j] = (wg_ds[b], wg_ds[b].n)

            def load_wd(j):
                b = j % 2
                tj = ORDER[j]
                POOL.wait(flat([wd_free[b], t_meta]))
                for k in range(8):
                    nc.gpsimd.indirect_dma_start(out=wd[b][:, k * 1024:(k + 1) * 1024], out_offset=None, in_=wd_rows[:, :],
                                                 in_offset=bass.IndirectOffsetOnAxis(ap=widx_i[:, tj * 8 + k:tj * 8 + k + 1], axis=0),
                                                 bounds_check=bcreg_w, oob_is_err=False).then_inc(wd_ds[b].sem, 16)
                    wd_ds[b].n += 16
                wd_tok[j] = (wd_ds[b], wd_ds[b].n)

            def emit_rows(j):
                jb = j % 2
                tev = None
                for sidx in range(NSUB):
                    i = nrow[0] % 3
                    nrow[0] += 1
                    r0 = ORDER[j] * TS + sidx * 128
                    tl = dma(SP, [t_scat, hrow_free[i]], hrow_ds[i], hrow[i][:], Hs[r0:r0 + 128, :])
                    pi, tpp, tpfree = tp_ps.next()
                    PE.wait(flat([tl, tpfree, t_idb]))
                    for k in range(8):
                        mm = nc.tensor.transpose(tpp[:, k, :], hrow[i][:, k * 128:(k + 1) * 128], identb[:])
                    ttp = PE.fin(mm)
                    hrow_free[i] = ttp
                    tev = op(ACT, [ttp, hT_free[jb] if sidx == 0 else None], nc.scalar.activation, out=hT[jb][:, :, sidx * 128:(sidx + 1) * 128], in_=tpp[:], func=AF.Identity)
                    tp_ps.rel(pi, tev)
                hT_tok[j] = tev

            def emit_gu(j):
                b = j % 3
                jb = j % 2
                ab = j % 2
                tbias = op(DVE, [wg_tok[j]], nc.vector.tensor_scalar, out=bgt[b][:, 8:16], in0=bgt[b][:, 8:16], scalar1=1.0, scalar2=None, op0=ALU.add)
                last_tok = None
                for jc in range(8):
                    gi, gp, gfree = g_ps.next()
                    li, lp, lfree = l_ps.next()
                    PE.wait(flat([wg_tok[j], hT_tok[j], gfree, lfree]))
                    for k in range(8):
                        nc.tensor.matmul(gp[:, 0:TS], lhsT=wg[b][:, k * 2048 + jc * 128:k * 2048 + (jc + 1) * 128], rhs=hT[jb][:, k, :], start=(k == 0), stop=(k == 7))
                    for k in range(8):
                        mm = nc.tensor.matmul(lp[:, 0:TS], lhsT=wg[b][:, k * 2048 + 1024 + jc * 128:k * 2048 + 1024 + (jc + 1) * 128], rhs=hT[jb][:, k, :], start=(k == 0), stop=(k == 7))
                    tmm = PE.fin(mm)
                    bg_ap = bgt[b][:, jc:jc + 1]
                    bl_ap = bgt[b][:, 8 + jc:9 + jc]
                    ci, gct, gcfree = gc.next()
                    a1 = op(DVE, [tmm, tbias, gcfree], nc.vector.tensor_scalar, out=gct[:, 0:TS], in0=gp[:, 0:TS], scalar1=bg_ap, scalar2=7.0, op0=ALU.add, op1=ALU.min)
                    g_ps.rel(gi, a1)
                    xi, sgt, sgfree = sg.next()
                    a2 = op(ACT, [a1, sgfree], nc.scalar.activation, out=sgt[:, 0:TS], in_=gct[:, 0:TS], func=AF.Silu, scale=1.702)
                    gc.rel(ci, a2)
                    yi, lct, lcfree = lc.next()
                    a3 = op(DVE, [tmm, tbias, lcfree], nc.vector.tensor_scalar, out=lct[:, 0:TS], in0=lp[:, 0:TS], scalar1=bl_ap, scalar2=8.0, op0=ALU.add, op1=ALU.min)
                    l_ps.rel(li, a3)
                    a6 = op(DVE, [a2, a3, actT_free[ab] if jc == 0 else None], nc.vector.scalar_tensor_tensor, out=actT[ab][:, jc, :], in0=lct[:, 0:TS], scalar=-6.0, in1=sgt[:, 0:TS],
                            op0=ALU.max, op1=ALU.mult)
                    sg.rel(xi, a6)
                    lc.rel(yi, a6)
                    last_tok = a6
                hT_free[jb] = tmm
                pend[j] = last_tok
                wg_free[b] = [tmm, last_tok]
                if j + 3 < NTILE:
                    load_wg(j + 3)

            def emit_down(j):
                b = j % 2
                ab = j % 2
                tmm = None
                for sidx in range(NSUB):
                    yb = ysn[0] % 2
                    ysn[0] += 1
                    evs = []
                    for hc in range(2):
                        yi, yp, yfree = y_ps.next()
                        PE.wait(flat([pend[j], yfree, wd_tok[j]]))
                        for jc in range(8):
                            mm = nc.tensor.matmul(yp[:, :], lhsT=actT[ab][:, jc, sidx * 128:(sidx + 1) * 128], rhs=wd[b][:, jc * 1024 + hc * 512:jc * 1024 + (hc + 1) * 512],
                                                  start=(jc == 0), stop=(jc == 7))
                        tmm = PE.fin(mm)
                        ev = op(DVE, [tmm, ys_free[yb]], nc.vector.tensor_tensor, out=ystage[yb][:, hc * 512:(hc + 1) * 512], in0=yp[:, :], in1=g2bc[:, hc * 512:(hc + 1) * 512], op=ALU.mult)
                        y_ps.rel(yi, ev)
                        evs.append(ev)
                    r0 = ORDER[j] * TS + sidx * 128
                    ys_free[yb] = dma(SP, evs, dys[yb], Ys[r0:r0 + 128, :], ystage[yb][:])
                actT_free[ab] = tmm
                wd_free[b] = tmm
                del pend[j]
                if j + 2 < NTILE:
                    load_wd(j + 2)

            load_wg(0)
            load_wd(0)
            load_wg(1)
            load_wd(1)
            load_wg(2)
            emit_rows(0)
            for j in range(NTILE):
                emit_gu(j)
                if j + 1 < NTILE:
                    emit_rows(j + 1)
                emit_down(j)
            t_ys = [(d_, d_.n) for d_ in dys]
            SP.wait(t_ys)
            barrier()

        with ExitStack() as ph:
            ln2gbc = sbt(ph, "ln2gbc", [128, D], F32)
            ln2bbc = sbt(ph, "ln2bbc", [128, D], F32)
            dcc = dsem()
            dma(SP, [], dcc, ln2gbc[:], ln2_g.partition_broadcast(128))
            t_cc = dma(SP, [], dcc, ln2bbc[:], ln2_b.partition_broadcast(128))
            st = [sbt(ph, "st6_%d" % i, [128, 12], F32) for i in range(2)]
            mv = [sbt(ph, "mv6_%d" % i, [128, 2], F32) for i in range(2)]
            rs = [sbt(ph, "rs6_%d" % i, [128, 2], F32) for i in range(2)]
            ob = [sbt(ph, "ob%d" % i, [128, D], F32) for i in range(2)]
            ob_free = [None, None]
            accs = [sbt(ph, "accs%d" % i, [128, D], F32) for i in range(2)]
            accs_ds = [dsem(), dsem()]
            accs_free = [None, None]
            yk = [[sbt(ph, "yk%d_%d" % (i, k), [128, D], F32) for k in range(4)] for i in range(2)]
            yk_ds = [dsem(), dsem()]
            yk_free = [None, None]
            ssum = [sbt(ph, "ssum%d" % i, [128, D], F32) for i in range(2)]
            ssum_free = [None, None]
            dout = [dsem(), dsem()]
            tyk = {}

            def issue_gather(t):
                b = t % 2
                POOL.wait(flat([yk_free[b], t_ys]))
                for k in range(4):
                    nc.gpsimd.indirect_dma_start(out=yk[b][k][:], out_offset=None, in_=Ys[:, :],
                                                 in_offset=bass.IndirectOffsetOnAxis(ap=slot_i[:, t * 4 + k:t * 4 + k + 1], axis=0),
                                                 bounds_check=bcreg_slot, oob_is_err=False).then_inc(yk_ds[b].sem, 16)
                    yk_ds[b].n += 16
                tyk[t] = (yk_ds[b], yk_ds[b].n)

            issue_gather(0)
            issue_gather(1)
            trs_t = {}

            def stage_a(t):
                b = t % 2
                tla = dma(SP, [t_accd, accs_free[b]], accs_ds[b], accs[b][:], accd[t * 128:(t + 1) * 128, :])
                q = op(DVE, [tyk[t], tla, ssum_free[b]], nc.vector.scalar_tensor_tensor, out=ssum[b][:], in0=yk[b][0][:], scalar=gate2[:, t, 0:1], in1=accs[b][:], op0=ALU.mult, op1=ALU.add)
                accs_free[b] = q
                for k in range(1, 4):
                    q = op(DVE, [q], nc.vector.scalar_tensor_tensor, out=ssum[b][:], in0=yk[b][k][:], scalar=gate2[:, t, k:k + 1], in1=ssum[b][:], op0=ALU.mult, op1=ALU.add)
                yk_free[b] = q
                if t + 2 < NOWN:
                    issue_gather(t + 2)
                trs_t[t] = ln_stats(ssum[b], D, [q], st[b], mv[b], rs[b])

            def stage_b(t):
                b = t % 2
                trs = trs_t.pop(t)
                tn_ = op(DVE, [trs], nc.vector.scalar_tensor_tensor, out=rs[b][:, 1:2], in0=mv[b][:, 0:1], scalar=-1.0, in1=rs[b][:, 0:1], op0=ALU.mult, op1=ALU.mult)
                t1_ = op(ACT, [tn_, ob_free[b]], nc.scalar.activation, out=ob[b][:], in_=ssum[b][:], func=AF.Identity, scale=rs[b][:, 0:1], bias=rs[b][:, 1:2])
                ssum_free[b] = t1_
                t2_ = op(DVE, [t1_, t_cc], nc.vector.tensor_tensor, out=ob[b][:], in0=ob[b][:], in1=ln2gbc[:], op=ALU.mult)
                t3_ = op(DVE, [t2_], nc.vector.tensor_tensor, out=ob[b][:], in0=ob[b][:], in1=ln2bbc[:], op=ALU.add)
                ob_free[b] = dma(SP, [t3_], dout[b], out[t * 128:(t + 1) * 128, :], ob[b][:])

            stage_a(0)
            for t in range(NOWN):
                if t + 1 < NOWN:
                    stage_a(t + 1)
                stage_b(t)
            SP.wait([(d_, d_.n) for d_ in dout])
    return nc


def _bias_tables(rpb):
    c = np.arange(64)
    cs = np.clip(c - 8, 0, 48)
    kc = np.arange(64)
    colvalid = (kc[None, :] >= cs[:, None]) & (kc[None, :] < cs[:, None] + 16)
    dcidx = np.clip(kc[None, :] - c[:, None] + 15, 0, 30)

    def table(r0, lr0, a, nrows, width):
        T = np.full((2, 64, 8, width), NEG, np.float32)
        for i in range(2):
            r = r0 + lr0 + i
            rs = min(max(r - 4, 0), 120)
            for w in range(nrows):
                kr = r0 - 4 + a + w
                if kr < rs or kr >= rs + 8:
                    continue
                dr = kr - r + 7
                vals = rpb[:, dr, :][:, dcidx]
                vals = np.where(colvalid[None], vals, np.float32(NEG))
                T[i, :, :, w * 64:(w + 1) * 64] = vals.transpose(1, 0, 2)
            T[i, :, :, nrows * 64:nrows * 64 + 256] = 0.0
        return T.reshape(128, 8 * width)

    gen = table(32, 8, 8, 9, 832)
    sp = {}
    for q in range(4):
        r0 = 32 * q
        sp[q] = np.stack([table(r0, 2 * p, SPECIAL[p][0], SPECIAL[p][1], 1024) for p in SP_ORDER])
    return gen, sp


def _consts(core):
    c = np.zeros((128, CW), np.float32)
    p = np.arange(128)
    c[:, 0:128] = (p[:, None] < p[None, :]).astype(np.float32)
    c[:, 128:256] = 1.0
    e = np.arange(NE)
    r = (e - 4 * core) % NE
    c[0:NE, 256:288] = (r[:, None] < r[None, :]).astype(np.float32)
    c[:, C_J:C_J + NTILE] = np.arange(NTILE)[None, :]
    c[:, C_E:C_E + NE] = e[None, :]
    c[:, C_PK:C_PK + 8] = np.arange(8)[None, :] * 128 + p[:, None]
    c[:, C_BIG] = np.where(p == 0, 0.0, BIGIDX)
    c[:, C_P] = p
    return c


_NC_CACHE = {}


def kernel(x, c, ctx, c_ctx, ada_w, ada_b, w_in, rpb, sgu_ln_g, sgu_ln_b, sgu_w, sgu_b,
           w_out, ln1_g, ln1_b, ln2_g, ln2_b, router_w, router_b,
           exp_w_gu, exp_b_gu, exp_w_down, exp_b_down, _debug=False):
    f = lambda a: np.ascontiguousarray(np.asarray(a, dtype=np.float32))
    x, c, ctx, c_ctx = f(x), f(c), f(ctx), f(c_ctx)
    ada_w0, ada_b0 = f(ada_w)[0], f(ada_b)[0]
    gen, sp = _bias_tables(f(rpb)[0])
    shared = {
        "ada_w": ada_w0,
        "adabT": np.ascontiguousarray(ada_b0.reshape(48, 128).T),
        "ada_b": ada_b0,
        "w_in": f(w_in)[0],
        "w_out": f(w_out)[0],
        "bias_gen": gen,
        "sgu_ln_g": f(sgu_ln_g)[0],
        "sgu_ln_b": f(sgu_ln_b)[0],
        "wsT": np.ascontiguousarray(f(sgu_w)[0].transpose(2, 0, 1).reshape(128, 512)),
        "sgu_bv": np.ascontiguousarray(f(sgu_b)[0].reshape(512)),
        "ln1_g": f(ln1_g)[0], "ln1_b": f(ln1_b)[0], "ln2_g": f(ln2_g)[0], "ln2_b": f(ln2_b)[0],
        "router_w": f(router_w)[0], "router_b": f(router_b)[0],
        "exp_w_gu": f(exp_w_gu)[0],
        "bgu_rows": np.ascontiguousarray(f(exp_b_gu)[0].reshape(NE, 16, 128).transpose(0, 2, 1).reshape(NE * 128, 16)),
        "zeros": np.zeros((512, D), dtype=ml_dtypes.bfloat16),
        "exp_w_down": f(exp_w_down)[0],
        "exp_b_down": f(exp_b_down)[0],
        "ident": np.eye(128, dtype=np.float32),
    }
    in_maps = []
    for j in range(NCORES):
        b, q = j // 4, j % 4
        r0 = 32 * q
        xg = x[b].reshape(128, 64, D)
        xe = np.zeros((NT_ALL * 128, D), np.float32)
        xev = xe[:NEXT_T * 128].reshape(40, 64, D)
        lo, hi = r0 - 4, r0 + 36
        slo, shi = max(lo, 0), min(hi, 128)
        xev[slo - lo:shi - lo] = xg[slo:shi]
        xe[NEXT_T * 128:] = ctx[b]
        cc = np.stack([c[b], c_ctx], axis=1)
        cT = np.ascontiguousarray(cc.reshape(8, 128, 2).transpose(1, 0, 2).reshape(128, 16))
        m = dict(shared)
        m["xe"] = xe
        m["cT"] = cT
        m["bias_sp"] = sp[q]
        m["cst"] = _consts(j)
        in_maps.append(m)
    key = bool(_debug)
    if key not in _NC_CACHE:
        _NC_CACHE[key] = build(debug=key)
    nc = _NC_CACHE[key]
    res = run_bass_kernel_spmd(nc, in_maps, core_ids=list(range(NCORES)))
    outs = [np.asarray(r["out"], dtype=np.float32) for r in res.results]
    full = np.concatenate(outs, axis=0).reshape(2, 8192, D)
    if _debug:
        return full, res.results
    return full
```

```python
import numpy as np
import ml_dtypes
from contextlib import ExitStack
import concourse.bass as bass
import concourse.mybir as mybir
from concourse.bass_utils import run_bass_kernel_spmd

F32 = mybir.dt.float32
BF16 = mybir.dt.bfloat16
I32 = mybir.dt.int32
AF = mybir.ActivationFunctionType
ALU = mybir.AluOpType
AX = mybir.AxisListType

NCORES = 8
D = 1024
NEXT_T = 20
NT_ALL = 22
OWN0 = 2
NOWN = 16
NTOK = NOWN * 128
NEG = -30000.0
ALPHA = 2.0 ** 0.25
LN_EPS = 1e-5
NE = 32
TS = 384
NSUB = TS // 128
NTILE = (4 * 2048 + NE * (TS - 1)) // TS
NSLOT = NTILE * TS
C_J = 288
C_E = C_J + NTILE
C_PK = C_E + NE
C_BIG = C_PK + 8
C_P = C_BIG + 1
CW = 384
assert C_P < CW
BIGIDX = 40000.0
SPECIAL = {0: (0, 12), 1: (2, 10), 14: (28, 9), 15: (28, 11)}
SP_ORDER = [0, 1, 14, 15]
PAIR_ORDER = [0, 2, 3, 4, 1, 5, 6, 7, 8, 14, 9, 10, 11, 15, 12, 13]


class Eng:
    def __init__(s, es, nc, eng, name):
        s.eng = eng
        s.name = name
        s.sem = es.enter_context(nc.semaphore("sem_" + name))
        s.n = 0
        s.seen = {}

    def wait(s, toks):
        for t in toks:
            if t is None:
                continue
            e, c = t
            if s.seen.get(e, 0) < c:
                s.eng.wait_ge(e.sem, c)
                s.seen[e] = c

    def fin(s, inst):
        inst.then_inc(s.sem, 1)
        s.n += 1
        return (s, s.n)

    def last(s):
        return (s, s.n) if s.n > 0 else None


class DSem:
    def __init__(s, es, nc, name):
        s.sem = es.enter_context(nc.semaphore("dsem_" + name))
        s.n = 0


def flat(deps):
    out = []
    for d in deps:
        if d is None:
            continue
        if isinstance(d, list):
            out.extend(flat(d))
        else:
            out.append(d)
    return out


class Ring:
    def __init__(s, bufs):
        s.bufs = bufs
        s.free = [None] * len(bufs)
        s.i = 0

    def next(s):
        i = s.i % len(s.bufs)
        s.i += 1
        return i, s.bufs[i], s.free[i]

    def rel(s, i, tok):
        s.free[i] = tok


def build(debug=False):
    nc = bass.Bass("TRN2", target_bir_lowering=False)

    def din(name, shape):
        return nc.dram_tensor(name, shape, F32, kind="ExternalInput").ap()

    xe = din("xe", [NT_ALL * 128, D])
    cT = din("cT", [128, 16])
    ada_w = din("ada_w", [D, 6 * D])
    adabT = din("adabT", [128, 48])
    ada_b = din("ada_b", [6 * D])
    w_in = din("w_in", [D, 2560])
    w_out = din("w_out", [D, D])
    bias_gen = din("bias_gen", [128, 8 * 832])
    bias_sp = din("bias_sp", [4, 128, 8 * 1024])
    sgu_ln_g = din("sgu_ln_g", [512])
    sgu_ln_b = din("sgu_ln_b", [512])
    wsT = din("wsT", [128, 512])
    sgu_bv = din("sgu_bv", [512])
    ln1_g = din("ln1_g", [D])
    ln1_b = din("ln1_b", [D])
    ln2_g = din("ln2_g", [D])
    ln2_b = din("ln2_b", [D])
    router_w = din("router_w", [D, NE])
    router_b = din("router_b", [NE])
    exp_w_gu = din("exp_w_gu", [NE, D, 2 * D])
    exp_w_down = din("exp_w_down", [NE, D, D])
    exp_b_down = din("exp_b_down", [NE, D])
    ident = din("ident", [128, 128])
    cst = din("cst", [128, CW])
    zeros = nc.dram_tensor("zeros", [512, D], BF16, kind="ExternalInput").ap()
    bgu_rows = din("bgu_rows", [NE * 128, 16])
    out = nc.dram_tensor("out", [NTOK, D], F32, kind="ExternalOutput").ap()
    Hx = nc.dram_tensor("Hx", [NTOK, D], BF16, kind="Internal").ap()
    Hs = nc.dram_tensor("Hs", [NSLOT, D], BF16, kind="Internal").ap()
    Ys = nc.dram_tensor("Ys", [NSLOT, D], F32, kind="Internal").ap()
    accd = nc.dram_tensor("accd", [NTOK, D], F32, kind="Internal").ap()
    wgu_rows = exp_w_gu.rearrange("e r n -> (e r) n")
    wd_rows = exp_w_down.rearrange("e r n -> (e r) n")
    dbg = {}
    if debug:
        for nm, shp in [("d_KT", [128, 4 * 2816]), ("d_V", [128, 22 * 512]), ("d_QT", [128, 4 * 2048]),
                        ("d_sguT", [128, 4 * 2048]), ("d_attT", [128, 4 * 2048]), ("d_acc", [128, 16 * 1024]),
                        ("d_h2T", [128, 8 * 2048]), ("d_comb", [128, 16 * 32]), ("d_ada", [128, 96]),
                        ("d_g1bc", [128, 1024])]:
            dbg[nm] = nc.dram_tensor(nm, shp, F32, kind="ExternalOutput").ap()

    with ExitStack() as es:
        PE = Eng(es, nc, nc.tensor, "pe")
        ACT = Eng(es, nc, nc.scalar, "act")
        DVE = Eng(es, nc, nc.vector, "dve")
        POOL = Eng(es, nc, nc.gpsimd, "pool")
        SP = Eng(es, nc, nc.sync, "sp")
        ENGS = [PE, ACT, DVE, POOL, SP]
        nds = [0]

        def dsem():
            nds[0] += 1
            return DSem(es, nc, "d%d" % nds[0])

        def op(E, deps, fn, *a, **k):
            E.wait(flat(deps))
            return E.fin(fn(*a, **k))

        def dma(Q, deps, ds, out_, in_):
            Q.wait(flat(deps))
            Q.eng.dma_start(out=out_, in_=in_).then_inc(ds.sem, 16)
            ds.n += 16
            return (ds, ds.n)

        def barrier():
            toks = [e.last() for e in ENGS]
            for e in ENGS:
                e.wait([t for t in toks if t is not None and t[0] is not e])

        dbg_sem = dsem()

        dummy = es.enter_context(nc.sbuf_tensor("dummy_t", [128, 8], F32))

        def dump(name, src_ap, deps, dt=F32, shape=None):
            if not debug:
                return
            W = src_ap.shape[-1]
            with ExitStack() as tmp:
                CH = 1024
                stg = tmp.enter_context(nc.sbuf_tensor("stg_" + name, [128, CH], F32))
                t2 = None
                for c0 in range(0, W, CH):
                    c1 = min(W, c0 + CH)
                    t = op(DVE, flat([deps, t2]), nc.vector.tensor_copy, out=stg[:, 0:c1 - c0], in_=src_ap[:, c0:c1])
                    t2 = dma(SP, [t], dbg_sem, dbg[name][:, c0:c1], stg[:, 0:c1 - c0])
                SP.wait([t2])
                op(DVE, [t2], nc.vector.memset, dummy[:], 0.0)

        def sbt(stack, name, shape, dt):
            return stack.enter_context(nc.sbuf_tensor(name, shape, dt))

        def pst(stack, name, shape, dt):
            return stack.enter_context(nc.psum_tensor(name, shape, dt))

        identf = sbt(es, "identf", [128, 128], F32)
        identb = sbt(es, "identb", [128, 128], BF16)
        adaT = sbt(es, "adaT", [128, 48, 2], F32)
        modv = sbt(es, "modv", [128, 6, 8], F32)
        g1bc = sbt(es, "g1bc", [128, D], F32)
        g2bc = sbt(es, "g2bc", [128, D], F32)
        m05 = sbt(es, "m05", [128, 1], F32)
        sh2bc = sbt(es, "sh2bc", [128, D], F32)
        sc2bc = sbt(es, "sc2bc", [128, D], F32)
        comb = sbt(es, "comb", [128, NOWN, NE], F32)
        slot_i = sbt(es, "slot_i", [128, NOWN * 4], I32)
        gate2 = sbt(es, "gate2", [128, NOWN, 4], F32)
        widx_i = sbt(es, "widx_i", [128, NTILE * 8], I32)
        bidx_i = sbt(es, "bidx_i", [128, NTILE], I32)
        dc0 = dsem()
        dc1 = dsem()
        t_idf = dma(SP, [], dc0, identf[:], ident[:, :])
        t_idb = dma(POOL, [], dc1, identb[:], ident[:, :])
        t_m05 = op(POOL, [], nc.gpsimd.memset, m05[:], -0.5)

        def ln_stats(src, width, deps, st_t, mv_t, rs_t):
            nchunk = width // 512
            toks = []
            for c in range(nchunk):
                toks.append(op(DVE, deps, nc.vector.bn_stats, out=st_t[:, c * 6:(c + 1) * 6], in_=src[:, c * 512:(c + 1) * 512]))
            t = op(DVE, toks, nc.vector.bn_aggr, out=mv_t[:, 0:2], in_=st_t[:, 0:6 * nchunk])
            t = op(POOL, [t, t_m05], nc.gpsimd.tensor_scalar, out=rs_t[:, 1:2], in0=mv_t[:, 1:2], scalar1=LN_EPS, scalar2=None, op0=ALU.add)
            t = op(POOL, [t], nc.gpsimd.tensor_tensor, out=rs_t[:, 0:1], in0=rs_t[:, 1:2], in1=m05[:], op=ALU.pow)
            return t

        with ExitStack() as ph:
            cT_sb = sbt(ph, "cT_sb", [128, 8, 2], F32)
            sT = sbt(ph, "sT", [128, 8, 2], F32)
            srep = sbt(ph, "srep", [128, 8, 128], F32)
            adab_sb = sbt(ph, "adab_sb", [128, 48], F32)
            wb = [sbt(ph, "adaw%d" % i, [128, 8, D], F32) for i in range(2)]
            wb_ds = [dsem(), dsem()]
            bb = [sbt(ph, "adabb%d" % i, [128, D], F32) for i in range(4)]
            ada_ps = pst(ph, "ada_ps", [128, 48, 2], F32)
            bc_ps = pst(ph, "bc_ps", [128, D], F32)
            t1 = dma(SP, [], dc0, cT_sb[:], cT.rearrange("p (k m) -> p k m", m=2))
            t2 = dma(SP, [], dc0, adab_sb[:], adabT[:, :])
            for i_, s_ in enumerate((2, 3, 4, 5)):
                dma(SP, [], dc0, bb[i_][:], ada_b[s_ * D:(s_ + 1) * D].partition_broadcast(128))
            tc_all = (dc0, dc0.n)
            t_idf = tc_all
            t_s = op(ACT, [tc_all], nc.scalar.activation, out=sT[:], in_=cT_sb[:], func=AF.Silu)
            t_rep = op(DVE, [t_s], nc.vector.tensor_copy, out=srep[:], in_=sT[:, :, 0:1].to_broadcast([128, 8, 128]))
            ada_w_v = ada_w.rearrange("(k p) n -> p k n", p=128)
            wfree = [None, None]
            t_last_mm = None
            bc_toks = {}
            for s in range(6):
                b = s % 2
                tl = dma(SP, [wfree[b]], wb_ds[b], wb[b][:], ada_w_v[:, :, s * D:(s + 1) * D])
                PE.wait([tl, t_s])
                for cc in range(8):
                    for k in range(8):
                        mm = nc.tensor.matmul(ada_ps[:, s * 8 + cc, :], lhsT=wb[b][:, k, cc * 128:(cc + 1) * 128], rhs=sT[:, k, :],
                                              start=(k == 0), stop=(k == 7))
                t_last_mm = PE.fin(mm)
                if s in (2, 3, 4, 5):
                    PE.wait([t_rep])
                    for hf in range(2):
                        for k in range(8):
                            mm = nc.tensor.matmul(bc_ps[:, hf * 512:(hf + 1) * 512], lhsT=srep[:, k, :], rhs=wb[b][:, k, hf * 512:(hf + 1) * 512],
                                                  start=(k == 0), stop=(k == 7))
                    t_last_mm = PE.fin(mm)
                    dst = {2: g1bc, 3: sh2bc, 4: sc2bc, 5: g2bc}[s]
                    if s == 4:
                        tt = op(DVE, [t_last_mm, tc_all], nc.vector.scalar_tensor_tensor, out=dst[:], in0=bc_ps[:], scalar=1.0, in1=bb[s - 2][:], op0=ALU.add, op1=ALU.add)
                    else:
                        tt = op(DVE, [t_last_mm, tc_all], nc.vector.tensor_tensor, out=dst[:], in0=bc_ps[:], in1=bb[s - 2][:], op=ALU.add)
                    bc_toks[s] = tt
                    PE.wait([tt])
                wfree[b] = t_last_mm
            t_ada = op(DVE, [t_last_mm, tc_all], nc.vector.tensor_tensor, out=adaT[:], in0=ada_ps[:],
                       in1=adab_sb[:].unsqueeze(2).to_broadcast([128, 48, 2]), op=ALU.add)
            tm = []
            tm.append(op(DVE, [t_ada], nc.vector.tensor_scalar, out=modv[:, 0, :], in0=adaT[:, 8:16, 0], scalar1=1.0, scalar2=None, op0=ALU.add))
            tm.append(op(DVE, [t_ada], nc.vector.tensor_copy, out=modv[:, 1, :], in_=adaT[:, 0:8, 0]))
            tm.append(op(DVE, [t_ada], nc.vector.tensor_scalar, out=modv[:, 2, :], in0=adaT[:, 8:16, 1], scalar1=1.0, scalar2=None, op0=ALU.add))
            tm.append(op(DVE, [t_ada], nc.vector.tensor_copy, out=modv[:, 3, :], in_=adaT[:, 0:8, 1]))
            tm.append(op(DVE, [t_ada], nc.vector.tensor_scalar, out=modv[:, 4, :], in0=adaT[:, 32:40, 0], scalar1=1.0, scalar2=None, op0=ALU.add))
            t_mod = op(DVE, [t_ada], nc.vector.tensor_copy, out=modv[:, 5, :], in_=adaT[:, 24:32, 0])
            dump("d_ada", adaT[:].rearrange("p a b -> p (a b)"), [t_mod])
            dump("d_g1bc", g1bc[:], [t_mod])
            barrier()

        lg_all = sbt(es, "lg_all", [128, NOWN, NE], F32)
        m8_all = sbt(es, "m8_all", [128, NOWN, 8], F32)
        maskall = sbt(es, "maskall", [128, NOWN, NE], F32)
        mid = ExitStack()
        mixT = sbt(mid, "mixT", [128, 8, NTOK], BF16)
        front = ExitStack()
        KT = sbt(front, "KT", [128, 4, 2816], BF16)
        V = sbt(front, "V", [128, NT_ALL, 512], BF16)
        QT = sbt(front, "QT", [128, 4, NTOK], BF16)

        with ExitStack() as ph:
            w_in_bf = sbt(ph, "w_in_bf", [128, 8, 2560], BF16)
            wsT_bf = sbt(ph, "wsT_bf", [128, 4, 128], BF16)
            lngbc = sbt(ph, "lngbc", [128, 512], F32)
            lnbbc = sbt(ph, "lnbbc", [128, 512], F32)
            bsbc = sbt(ph, "bsbc", [128, 512], F32)
            dw = dsem()
            dcc = dsem()
            w_in_v = w_in.rearrange("(k p) n -> p k n", p=128)
            for k in range(8):
                t_win = dma(POOL, [], dw, w_in_bf[:, k, :], w_in_v[:, k, :])
            t_win = dma(POOL, [], dw, wsT_bf[:], wsT.rearrange("p (g i) -> p g i", g=4))
            dma(SP, [], dcc, lngbc[:], sgu_ln_g.partition_broadcast(128))
            dma(SP, [], dcc, lnbbc[:], sgu_ln_b.partition_broadcast(128))
            t_cc = dma(SP, [], dcc, bsbc[:], sgu_bv.partition_broadcast(128))

            xt = [sbt(ph, "xt%d" % i, [128, D], F32) for i in range(2)]
            xt_ds = [dsem(), dsem()]
            xt_free = [None, None]
            xn = [sbt(ph, "xn%d" % i, [128, D], BF16) for i in range(2)]
            xn_free = [None, None]
            st = [sbt(ph, "st%d" % i, [128, 12], F32) for i in range(2)]
            mv = [sbt(ph, "mv%d" % i, [128, 2], F32) for i in range(2)]
            rs = [sbt(ph, "rs%d" % i, [128, 2], F32) for i in range(2)]
            tp_ps = Ring([pst(ph, "tp_ps%d" % i, [128, 8, 128], BF16) for i in range(2)])
            hT = [sbt(ph, "hT%d" % i, [128, 8, 512], BF16) for i in range(2)]
            hT_free = [None, None]
            acc_ps = Ring([pst(ph, "acc_ps%d" % i, [128, 512], F32) for i in range(4)])
            sg_ps = pst(ph, "sg_ps", [128, 4, 128], F32)
            sg_free = [None]
            Gg = [sbt(ph, "Gg%d" % i, [128, 512], F32) for i in range(2)]
            Gn = [sbt(ph, "Gn%d" % i, [128, 512], F32) for i in range(2)]
            Gb = [sbt(ph, "Gb%d" % i, [128, 512], BF16) for i in range(2)]
            Gfree = [None, None]
            gst = [sbt(ph, "gst%d" % i, [128, 6], F32) for i in range(2)]
            gmv = [sbt(ph, "gmv%d" % i, [128, 2], F32) for i in range(2)]
            grs = [sbt(ph, "grs%d" % i, [128, 2], F32) for i in range(2)]
            stmp = [sbt(ph, "stmp%d" % i, [128, 512], F32) for i in range(2)]
            stmp_free = [None, None]
            evq = [0]

            def evac_copy(deps, out_, in_, scale=None, func=None):
                evq[0] += 1
                if func is not None:
                    return op(ACT, deps, nc.scalar.activation, out=out_, in_=in_, func=func)
                if evq[0] % 2 == 0:
                    if scale is None:
                        return op(ACT, deps, nc.scalar.activation, out=out_, in_=in_, func=AF.Identity)
                    return op(ACT, deps, nc.scalar.activation, out=out_, in_=in_, func=AF.Identity, scale=float(scale))
                if scale is None:
                    return op(DVE, deps, nc.vector.tensor_copy, out=out_, in_=in_)
                return op(DVE, deps, nc.vector.tensor_scalar, out=out_, in0=in_, scalar1=float(scale), scalar2=None, op0=ALU.mult)

            groups = [list(range(g * 4, min(g * 4 + 4, NT_ALL))) for g in range(6)]
            ti_glob = 0
            gcount = 0
            for gi, tiles in enumerate(groups):
                hb = gi % 2
                ntok = 128 * len(tiles)
                ev_toks = []
                for sl, ti in enumerate(tiles):
                    b = ti_glob % 2
                    ti_glob += 1
                    is_ctx = ti >= NEXT_T
                    tl = dma(SP, [xt_free[b]], xt_ds[b], xt[b][:], xe[ti * 128:(ti + 1) * 128, :])
                    trs = ln_stats(xt[b], D, [tl], st[b], mv[b], rs[b])
                    tn = op(DVE, [trs, xn_free[b]], nc.vector.tensor_scalar, out=xn[b][:], in0=xt[b][:], scalar1=mv[b][:, 0:1], scalar2=rs[b][:, 0:1],
                            op0=ALU.subtract, op1=ALU.mult)
                    xt_free[b] = tn
                    pi, pt, pfree = tp_ps.next()
                    PE.wait(flat([tn, pfree, t_idb]))
                    for k in range(8):
                        mm = nc.tensor.transpose(pt[:, k, :], xn[b][:, k * 128:(k + 1) * 128], identb[:])
                    ttp = PE.fin(mm)
                    xn_free[b] = ttp
                    msc, msh = (2, 3) if is_ctx else (0, 1)
                    ACT.wait(flat([ttp, hT_free[hb], t_mod]))
                    for k in range(8):
                        a = nc.scalar.activation(out=hT[hb][:, k, sl * 128:(sl + 1) * 128], in_=pt[:, k, :], func=AF.Identity,
                                                 scale=modv[:, msc, k:k + 1], bias=modv[:, msh, k:k + 1])
                    tev = ACT.fin(a)
                    tp_ps.rel(pi, tev)
                    ev_toks.append(tev)
                hready = ev_toks[-1]
                tok0 = tiles[0] * 128
                mm_last = None
                for c in range(4):
                    ai, ap_, afree = acc_ps.next()
                    PE.wait(flat([hready, afree, t_win]))
                    for k in range(8):
                        mm = nc.tensor.matmul(ap_[:, 0:ntok], lhsT=w_in_bf[:, k, 512 + c * 128:512 + (c + 1) * 128], rhs=hT[hb][:, k, 0:ntok],
                                              start=(k == 0), stop=(k == 7))
                    tmm = PE.fin(mm)
                    te = evac_copy([tmm], KT[:, c, tok0:tok0 + ntok], ap_[:, 0:ntok])
                    acc_ps.rel(ai, te)
                for sl, ti in enumerate(tiles):
                    ai, ap_, afree = acc_ps.next()
                    PE.wait(flat([hready, afree, t_win]))
                    for k in range(8):
                        mm = nc.tensor.matmul(ap_[:, :], lhsT=hT[hb][:, k, sl * 128:(sl + 1) * 128], rhs=w_in_bf[:, k, 1024:1536],
                                              start=(k == 0), stop=(k == 7))
                    tmm = PE.fin(mm)
                    te = evac_copy([tmm], V[:, ti, :], ap_[:, :])
                    acc_ps.rel(ai, te)
                    mm_last = tmm
                own = [(sl, ti) for sl, ti in enumerate(tiles) if OWN0 <= ti < OWN0 + NOWN]
                if own:
                    s0 = own[0][0] * 128
                    nown = 128 * len(own)
                    o0 = (own[0][1] - OWN0) * 128
                    for c in range(4):
                        ai, ap_, afree = acc_ps.next()
                        PE.wait(flat([hready, afree]))
                        for k in range(8):
                            mm = nc.tensor.matmul(ap_[:, 0:nown], lhsT=w_in_bf[:, k, c * 128:(c + 1) * 128], rhs=hT[hb][:, k, s0:s0 + nown],
                                                  start=(k == 0), stop=(k == 7))
                        tmm = PE.fin(mm)
                        te = evac_copy([tmm], QT[:, c, o0:o0 + nown], ap_[:, 0:nown], scale=0.125)
                        acc_ps.rel(ai, te)
                    ut_toks = []
                    for c in range(4):
                        ai, ap_, afree = acc_ps.next()
                        PE.wait(flat([hready, afree]))
                        for k in range(8):
                            mm = nc.tensor.matmul(ap_[:, 0:nown], lhsT=w_in_bf[:, k, 1536 + c * 128:1536 + (c + 1) * 128], rhs=hT[hb][:, k, s0:s0 + nown],
                                                  start=(k == 0), stop=(k == 7))
                        tmm = PE.fin(mm)
                        te = evac_copy([tmm], mixT[:, 4 + c, o0:o0 + nown], ap_[:, 0:nown], func=AF.Gelu_apprx_tanh)
                        acc_ps.rel(ai, te)
                        ut_toks.append(te)
                    for sl, ti in own:
                        gb = gcount % 2
                        gcount += 1
                        ot = (ti - OWN0) * 128
                        ai, ap_, afree = acc_ps.next()
                        PE.wait(flat([hready, afree]))
                        for k in range(8):
                            mm = nc.tensor.matmul(ap_[:, :], lhsT=hT[hb][:, k, sl * 128:(sl + 1) * 128], rhs=w_in_bf[:, k, 2048:2560],
                                                  start=(k == 0), stop=(k == 7))
                        tmm = PE.fin(mm)
                        mm_last = tmm
                        tg = op(ACT, [tmm, Gfree[gb]], nc.scalar.activation, out=Gg[gb][:], in_=ap_[:, :], func=AF.Gelu_apprx_tanh)
                        acc_ps.rel(ai, tg)
                        trs = ln_stats(Gg[gb], 512, [tg], gst[gb], gmv[gb], grs[gb])
                        t1_ = op(DVE, [trs], nc.vector.tensor_scalar, out=Gn[gb][:], in0=Gg[gb][:], scalar1=gmv[gb][:, 0:1], scalar2=grs[gb][:, 0:1],
                                 op0=ALU.subtract, op1=ALU.mult)
                        t2_ = op(DVE, [t1_, t_cc], nc.vector.tensor_tensor, out=Gn[gb][:], in0=Gn[gb][:], in1=lngbc[:], op=ALU.mult)
                        t3_ = op(DVE, [t2_], nc.vector.tensor_tensor, out=Gb[gb][:], in0=Gn[gb][:], in1=lnbbc[:], op=ALU.add)
                        PE.wait(flat([t3_, sg_free[0], t_win]))
                        for g in range(4):
                            mm = nc.tensor.matmul(sg_ps[:, g, :], lhsT=Gb[gb][:, g * 128:(g + 1) * 128], rhs=wsT_bf[:, g, :], start=True, stop=True)
                        tsg = PE.fin(mm)
                        Gfree[gb] = tsg
                        t4_ = op(DVE, [tsg, stmp_free[gb], t_cc], nc.vector.tensor_tensor, out=stmp[gb][:], in0=sg_ps[:].rearrange("p g i -> p (g i)"), in1=bsbc[:], op=ALU.add)
                        sg_free[0] = t4_
                        t5_ = op(DVE, [t4_, ut_toks], nc.vector.tensor_tensor, out=mixT[:, 4:8, ot:ot + 128],
                                 in0=stmp[gb][:].rearrange("p (g i) -> p g i", g=4), in1=mixT[:, 4:8, ot:ot + 128], op=ALU.mult)
                        stmp_free[gb] = t5_
                hT_free[hb] = mm_last
            barrier()
            if debug:
                dump("d_KT", KT[:].rearrange("p a b -> p (a b)"), [], BF16, [128, 4 * 2816])
                dump("d_V", V[:].rearrange("p a b -> p (a b)"), [], BF16, [128, 22 * 512])
                dump("d_QT", QT[:].rearrange("p a b -> p (a b)"), [], BF16, [128, 4 * 2048])
                dump("d_sguT", mixT[:, 4:8, :].rearrange("p a b -> p (a b)"), [], BF16, [128, 4 * 2048])
                barrier()

        with ExitStack() as ph:
            bgen = sbt(ph, "bgen", [128, 8, 832], F32)
            bsp = sbt(ph, "bsp", [128, 8, 1024], F32)
            db = dsem()
            dbs = dsem()
            t_bgen = dma(SP, [], db, bgen[:], bias_gen.rearrange("p (h k) -> p h k", h=8))
            dz = dsem()
            S_ps = Ring([pst(ph, "S_ps%d" % i, [128, 1024], F32) for i in range(2)])
            PT_ps = Ring([pst(ph, "PT_ps%d" % i, [128, 8, 128], BF16) for i in range(2)])
            O_ps = pst(ph, "O_ps", [128, 8, 64], F32)
            O_free = [None]
            AT_ps = pst(ph, "AT_ps", [128, 4, 128], BF16)
            AT_free = [None]
            S_sb = Ring([sbt(ph, "S_sb%d" % i, [128, 1024], F32) for i in range(2)])
            P_sb = Ring([sbt(ph, "P_sb%d" % i, [128, 1024], BF16) for i in range(2)])
            PT_sb = Ring([sbt(ph, "PT_sb%d" % i, [128, 8, 128], BF16) for i in range(3)])
            att_sb = Ring([sbt(ph, "att_sb%d" % i, [128, 512], BF16) for i in range(2)])
            mx_all = sbt(ph, "mx_all", [128, 128], F32)
            nmx_all = sbt(ph, "nmx_all", [128, 128], F32)
            rs_all = sbt(ph, "rs_all", [128, 128], F32)
            rinv_all = sbt(ph, "rinv_all", [128, 128], F32)

            items = []
            sp_load_tok = {}
            for p in PAIR_ORDER:
                for h in range(8):
                    items.append((p, h))
            N = len(items)
            st_ = {}
            bsp_free = [None]
            sp_next = [0]

            def issue_sp_load():
                i = sp_next[0]
                if i >= len(SP_ORDER):
                    return
                p = SP_ORDER[i]
                sp_load_tok[p] = dma(SP, [bsp_free[0]], dbs, bsp[:], bias_sp[i].rearrange("p (h k) -> p h k", h=8))
                sp_next[0] += 1

            issue_sp_load()
            for i_ in range(NTILE):
                dma(POOL, [], dz, Hs[i_ * TS:(i_ + 1) * TS, :], zeros[0:TS, :])
            t_zero = (dz, dz.n)

            def blocks_for(p):
                a, nrows = SPECIAL.get(p, (2 * p, 9))
                nk = nrows * 64
                bl = []
                for j in range(nk // 128):
                    bl.append((j * 128, 128, a // 2 + j))
                if nk % 128:
                    bl.append((nk - 64, 64, a // 2 + nk // 128))
                bl.append((nk, 128, NEXT_T))
                bl.append((nk + 128, 128, NEXT_T + 1))
                return a, nrows, nk, bl

            def emit_qk(i):
                p, h = items[i]
                a, nrows, nk, bl = blocks_for(p)
                nt = nk + 256
                c = h // 2
                pb = (h % 2) * 64
                si, sp_, sfree = S_ps.next()
                PE.wait(flat([sfree]))
                q_ap = QT[pb:pb + 64, c, p * 128:(p + 1) * 128]
                k0 = a * 64
                nc.tensor.matmul(sp_[:, 0:512], lhsT=q_ap, rhs=KT[pb:pb + 64, c, k0:k0 + 512], start=True, stop=True)
                nc.tensor.matmul(sp_[:, 512:nk], lhsT=q_ap, rhs=KT[pb:pb + 64, c, k0 + 512:k0 + nk], start=True, stop=True)
                tqk = PE.fin(nc.tensor.matmul(sp_[:, nk:nt], lhsT=q_ap, rhs=KT[pb:pb + 64, c, 2560:2816], start=True, stop=True))
                if p in SPECIAL:
                    btab = bsp[:, h, 0:nt]
                    bdep = sp_load_tok[p]
                else:
                    btab = bgen[:, h, 0:nt]
                    bdep = t_bgen
                bi, sb_, sbfree = S_sb.next()
                ts = op(DVE, [tqk, bdep, sbfree], nc.vector.tensor_tensor, out=sb_[:, 0:nt], in0=sp_[:, 0:nt], in1=btab, op=ALU.add)
                S_ps.rel(si, ts)
                col = i
                tm1 = op(DVE, [ts], nc.vector.tensor_reduce, out=mx_all[:, col:col + 1], in_=sb_[:, 0:nt], axis=AX.X, op=ALU.max)
                tm2 = op(DVE, [tm1], nc.vector.tensor_scalar, out=nmx_all[:, col:col + 1], in0=mx_all[:, col:col + 1], scalar1=-1.0, scalar2=None, op0=ALU.mult)
                pi, pp, pfree = P_sb.next()
                tp = op(ACT, [tm2, pfree], nc.scalar.activation, out=pp[:, 0:nt], in_=sb_[:, 0:nt], func=AF.Exp, bias=nmx_all[:, col:col + 1], scale=1.0,
                        accum_out=rs_all[:, col:col + 1])
                S_sb.rel(bi, tp)
                st_[i] = dict(pi=pi, pp=pp, tp=tp, bl=bl, last_sp=(p in SPECIAL and h == 7))
                if p in SPECIAL and h == 7:
                    bsp_free[0] = ts
                    issue_sp_load()

            def emit_tr(i):
                d = st_[i]
                ti_, tps, tfree = PT_ps.next()
                PE.wait(flat([d["tp"], tfree, t_idb]))
                for j, (off, ln, vt) in enumerate(d["bl"]):
                    mm = nc.tensor.transpose(tps[0:ln, j, :], d["pp"][:, off:off + ln], identb[:])
                ttr = PE.fin(mm)
                P_sb.rel(d["pi"], ttr)
                nb = len(d["bl"])
                qi, qsb, qfree = PT_sb.next()
                if i % 2 == 0:
                    te = op(ACT, [ttr, qfree], nc.scalar.activation, out=qsb[:, 0:nb, :], in_=tps[:, 0:nb, :], func=AF.Identity)
                else:
                    te = op(DVE, [ttr, qfree], nc.vector.tensor_copy, out=qsb[:, 0:nb, :], in_=tps[:, 0:nb, :])
                PT_ps.rel(ti_, te)
                d["qi"] = qi
                d["qsb"] = qsb
                d["te"] = te

            def emit_pv(i):
                p, h = items[i]
                d = st_[i]
                PE.wait(flat([d["te"], O_free[0] if h == 0 else None]))
                nb = len(d["bl"])
                for j, (off, ln, vt) in enumerate(d["bl"]):
                    mm = nc.tensor.matmul(O_ps[:, h, :], lhsT=d["qsb"][0:ln, j, :], rhs=V[0:ln, vt, h * 64:(h + 1) * 64], start=(j == 0), stop=(j == nb - 1))
                tpv = PE.fin(mm)
                PT_sb.rel(d["qi"], tpv)
                if h == 7:
                    c0 = i - 7
                    tr_ = op(DVE, [d["tp"]], nc.vector.reciprocal, out=rinv_all[:, c0:c0 + 8], in_=rs_all[:, c0:c0 + 8])
                    ai, asb, afree = att_sb.next()
                    ta = op(DVE, [tr_, tpv, afree], nc.vector.tensor_tensor, out=asb[:].rearrange("p (h d) -> p h d", h=8), in0=O_ps[:],
                            in1=rinv_all[:, c0:c0 + 8].unsqueeze(2).to_broadcast([128, 8, 64]), op=ALU.mult)
                    O_free[0] = ta
                    PE.wait(flat([ta, AT_free[0]]))
                    for c in range(4):
                        mm = nc.tensor.transpose(AT_ps[:, c, :], asb[:, c * 128:(c + 1) * 128], identb[:])
                    tat = PE.fin(mm)
                    att_sb.rel(ai, tat)
                    te = op(ACT, [tat], nc.scalar.activation, out=mixT[:, 0:4, p * 128:(p + 1) * 128], in_=AT_ps[:], func=AF.Identity)
                    AT_free[0] = te
                del st_[i]

            for i in range(N + 2):
                if i < N:
                    emit_qk(i)
                if 1 <= i <= N:
                    emit_tr(i - 1)
                if i >= 2:
                    emit_pv(i - 2)
            barrier()
            if debug:
                dump("d_attT", mixT[:, 0:4, :].rearrange("p a b -> p (a b)"), [], BF16, [128, 4 * 2048])
                barrier()
        front.close()

        hrb_ds = [dsem(), dsem()]
        acct_ds = [dsem(), dsem(), dsem()]
        with ExitStack() as ph:
            w_out_bf = sbt(ph, "w_out_bf", [128, 8, D], BF16)
            ln1gbc = sbt(ph, "ln1gbc", [128, D], F32)
            ln1bbc = sbt(ph, "ln1bbc", [128, D], F32)
            rw = sbt(ph, "rw", [128, 8, NE], F32)
            rbbc = sbt(ph, "rbbc", [128, NE], F32)
            bd_sb = sbt(ph, "bd_sb", [NE, D], F32)
            dw = dsem()
            dcc = dsem()
            t_wout = dma(POOL, [], dw, w_out_bf[:], w_out.rearrange("(k p) n -> p k n", p=128))
            dma(SP, [], dcc, ln1gbc[:], ln1_g.partition_broadcast(128))
            dma(SP, [], dcc, ln1bbc[:], ln1_b.partition_broadcast(128))
            dma(SP, [], dcc, rw[:], router_w.rearrange("(k p) n -> p k n", p=128))
            dma(SP, [], dcc, rbbc[:], router_b.partition_broadcast(128))
            t_cc = dma(SP, [], dcc, bd_sb[:], exp_b_down[:, :])
            xr = [sbt(ph, "xr%d" % i, [128, D], F32) for i in range(2)]
            xr_ds = [dsem(), dsem()]
            xr_free = [None, None]
            mix_ps = pst(ph, "mix_ps", [128, D], F32)
            mix_free = [None]
            tr_ps = pst(ph, "tr_ps", [128, 8, 128], F32)
            tr_free = [None]
            lg_ps = pst(ph, "lg_ps", [128, NE], F32)
            lg_free = [None]
            ct_ps = pst(ph, "ct_ps", [NE, 128], F32)
            ct_free = [None]
            bd_ps = pst(ph, "bd_ps", [128, D], F32)
            bd_free = [None]
            wk = sbt(ph, "wk", [128, D], F32)
            wk_free = [None]
            x1n = sbt(ph, "x1n", [128, D], F32)
            x1n_free = [None]
            h2f = sbt(ph, "h2f", [128, 8, 128], F32)
            h2f_free = [None]
            hrf = sbt(ph, "hrf", [128, D], F32)
            hrb = [sbt(ph, "hrb%d" % i, [128, D], BF16) for i in range(2)]
            hrb_free = [None, None]
            acct = [sbt(ph, "acct%d" % i, [128, D], F32) for i in range(3)]
            acct_free = [None, None, None]
            st = sbt(ph, "st4", [128, 12], F32)
            mv = sbt(ph, "mv4", [128, 2], F32)
            rs = sbt(ph, "rs4", [128, 2], F32)
            st2 = sbt(ph, "st5", [128, 12], F32)
            mv2 = sbt(ph, "mv5", [128, 2], F32)
            rs2 = sbt(ph, "rs5", [128, 2], F32)
            rt = sbt(ph, "rt", [128, 4], F32)
            ex = sbt(ph, "ex", [128, NE], F32)
            combT = sbt(ph, "combT", [NE, 128], F32)
            combT_free = [None]
            ex_free = [None]

            ones1 = sbt(ph, "ones1", [1, 128], F32)
            rb1 = sbt(ph, "rb1", [1, NE], F32)
            bdt = sbt(ph, "bdt", [128, D], F32)
            bdt_free = [None]
            t_ones = op(DVE, [], nc.vector.memset, ones1[:], 1.0)
            drb = dsem()
            t_rb1 = dma(SP, [], drb, rb1[:], router_b.rearrange("(o n) -> o n", o=1))
            s1 = {}

            def outproj(t):
                b = t % 2
                tok = slice(t * 128, (t + 1) * 128)
                tl = dma(SP, [xr_free[b]], xr_ds[b], xr[b][:], xe[(OWN0 + t) * 128:(OWN0 + t + 1) * 128, :])
                PE.wait(flat([mix_free[0], t_wout]))
                for hf in range(2):
                    for k in range(8):
                        mm = nc.tensor.matmul(mix_ps[:, hf * 512:(hf + 1) * 512], lhsT=mixT[:, k, tok], rhs=w_out_bf[:, k, hf * 512:(hf + 1) * 512],
                                              start=(k == 0), stop=(k == 7))
                tmm = PE.fin(mm)
                s1[t] = dict(tl=tl, tmm=tmm)

            def stage1a(t):
                b = t % 2
                b3 = t % 3
                tl = s1[t]["tl"]
                tmm = s1[t]["tmm"]
                ta = op(DVE, [tmm, wk_free[0]], nc.vector.tensor_tensor, out=wk[:], in0=mix_ps[:], in1=g1bc[:], op=ALU.mult)
                mix_free[0] = ta
                tb_ = op(DVE, [ta, tl], nc.vector.scalar_tensor_tensor, out=wk[:], in0=xr[b][:], scalar=float(ALPHA), in1=wk[:], op0=ALU.mult, op1=ALU.add)
                xr_free[b] = tb_
                trs = ln_stats(wk, D, [tb_], st, mv, rs)
                tc_ = op(DVE, [trs], nc.vector.tensor_scalar, out=wk[:], in0=wk[:], scalar1=mv[:, 0:1], scalar2=rs[:, 0:1], op0=ALU.subtract, op1=ALU.mult)
                td_ = op(DVE, [tc_, t_cc], nc.vector.tensor_tensor, out=wk[:], in0=wk[:], in1=ln1gbc[:], op=ALU.mult)
                te_ = op(DVE, [td_], nc.vector.tensor_tensor, out=wk[:], in0=wk[:], in1=ln1bbc[:], op=ALU.add)
                tf_ = op(ACT, [te_, acct_free[b3]], nc.scalar.activation, out=acct[b3][:], in_=wk[:], func=AF.Identity, scale=float(ALPHA))
                trs2 = ln_stats(wk, D, [te_], st2, mv2, rs2)
                tg_ = op(DVE, [trs2, x1n_free[0]], nc.vector.tensor_scalar, out=x1n[:], in0=wk[:], scalar1=mv2[:, 0:1], scalar2=rs2[:, 0:1],
                         op0=ALU.subtract, op1=ALU.mult)
                wk_free[0] = [tf_, tg_]
                hr1 = op(DVE, [tg_], nc.vector.tensor_tensor, out=hrf[:], in0=x1n[:], in1=sc2bc[:], op=ALU.mult)
                hr2 = op(DVE, [hr1, hrb_free[b]], nc.vector.tensor_tensor, out=hrb[b][:], in0=hrf[:], in1=sh2bc[:], op=ALU.add)
                hrb_free[b] = dma(SP, [hr2], hrb_ds[b], Hx[t * 128:(t + 1) * 128, :], hrb[b][:])
                s1[t].update(tf_=tf_, tg_=tg_, hr1=hr1)

            def stage1b(t):
                tg_ = s1[t]["tg_"]
                PE.wait(flat([tg_, tr_free[0], t_idf]))
                for k in range(8):
                    mm = nc.tensor.transpose(tr_ps[:, k, :], x1n[:, k * 128:(k + 1) * 128], identf[:])
                ttr = PE.fin(mm)
                x1n_free[0] = [ttr, s1[t]["hr1"]]
                ACT.wait(flat([ttr, h2f_free[0]]))
                for k in range(8):
                    a = nc.scalar.activation(out=h2f[:, k, :], in_=tr_ps[:, k, :], func=AF.Identity, scale=modv[:, 4, k:k + 1], bias=modv[:, 5, k:k + 1])
                th = ACT.fin(a)
                tr_free[0] = th
                PE.wait(flat([th, lg_free[0], t_cc, t_ones, t_rb1]))
                for k in range(8):
                    nc.tensor.matmul(lg_ps[:, :], lhsT=h2f[:, k, :], rhs=rw[:, k, :], start=(k == 0), stop=False)
                tlg = PE.fin(nc.tensor.matmul(lg_ps[:, :], lhsT=ones1[0:1, :], rhs=rb1[0:1, :], start=False, stop=True))
                h2f_free[0] = [tlg]
                r1 = op(ACT, [tlg], nc.scalar.activation, out=lg_all[:, t, :], in_=lg_ps[:], func=AF.Identity)
                lg_free[0] = r1
                s1[t]["r1"] = r1

            def stage2a(t):
                lg = lg_all[:, t, :]
                m8 = m8_all[:, t, :]
                msk = maskall[:, t, :]
                r1 = s1[t]["r1"]
                r2 = op(DVE, [r1], nc.vector.max, out=m8, in_=lg)
                r3 = op(DVE, [r2], nc.vector.tensor_scalar, out=rt[:, 0:1], in0=m8_all[:, t, 0:1], scalar1=-1.0, scalar2=None, op0=ALU.mult)
                r4 = op(ACT, [r3, ex_free[0]], nc.scalar.activation, out=ex[:], in_=lg, func=AF.Exp, bias=rt[:, 0:1], scale=1.0)
                r5 = op(DVE, [r2], nc.vector.tensor_scalar, out=msk, in0=lg, scalar1=m8_all[:, t, 3:4], scalar2=None, op0=ALU.is_ge)
                r6 = op(DVE, [r4, r5], nc.vector.tensor_tensor, out=ex[:], in0=ex[:], in1=msk, op=ALU.mult)
                r7 = op(DVE, [r6], nc.vector.tensor_reduce, out=rt[:, 1:2], in_=ex[:], axis=AX.X, op=ALU.add)
                r8 = op(DVE, [r7], nc.vector.reciprocal, out=rt[:, 2:3], in_=rt[:, 1:2])
                r9 = op(DVE, [r8], nc.vector.tensor_scalar, out=comb[:, t, :], in0=ex[:], scalar1=rt[:, 2:3], scalar2=None, op0=ALU.mult)
                ex_free[0] = r9
                PE.wait(flat([r9, ct_free[0]]))
                tct = PE.fin(nc.tensor.transpose(ct_ps[:, :], comb[:, t, :], identf[:]))
                tcc_ = op(ACT, [tct, combT_free[0]], nc.scalar.activation, out=combT[:], in_=ct_ps[:], func=AF.Identity)
                ct_free[0] = tcc_
                PE.wait(flat([tcc_, bd_free[0]]))
                for hf in range(2):
                    mm = nc.tensor.matmul(bd_ps[:, hf * 512:(hf + 1) * 512], lhsT=combT[:, :], rhs=bd_sb[:, hf * 512:(hf + 1) * 512], start=True, stop=True)
                tbd = PE.fin(mm)
                combT_free[0] = tbd
                s1[t]["tbd"] = tbd

            def stage2b(t):
                b3 = t % 3
                u1 = op(DVE, [s1[t]["tbd"], bdt_free[0]], nc.vector.tensor_tensor, out=bdt[:], in0=bd_ps[:], in1=g2bc[:], op=ALU.mult)
                bd_free[0] = u1
                u2 = op(DVE, [u1, s1[t]["tf_"]], nc.vector.tensor_tensor, out=acct[b3][:], in0=acct[b3][:], in1=bdt[:], op=ALU.add)
                bdt_free[0] = u2
                acct_free[b3] = dma(SP, [u2], acct_ds[b3], accd[t * 128:(t + 1) * 128, :], acct[b3][:])
                del s1[t]

            outproj(0)
            stage1a(0)
            outproj(1)
            for t in range(NOWN):
                stage1b(t)
                if t + 1 < NOWN:
                    stage1a(t + 1)
                if t + 2 < NOWN:
                    outproj(t + 2)
                if t >= 1:
                    stage2b(t - 1)
                stage2a(t)
            stage2b(NOWN - 1)
            SP.wait([(d_, d_.n) for d_ in hrb_ds + acct_ds])
            barrier()
        t_hx = [(d_, d_.n) for d_ in hrb_ds]
        t_accd = [(d_, d_.n) for d_ in acct_ds]
        mid.close()

        bcreg_slot = nc.gpsimd.alloc_register("bc_slot")
        nc.gpsimd.reg_mov(bcreg_slot, NSLOT - 1)
        bcreg_w = nc.gpsimd.alloc_register("bc_w")
        nc.gpsimd.reg_mov(bcreg_w, NE * 1024 - 1)
        bcreg_b = nc.gpsimd.alloc_register("bc_b")
        nc.gpsimd.reg_mov(bcreg_b, NE * 128 - 1)
        with ExitStack() as ph:
            cst_sb = sbt(ph, "cst_sb", [128, CW], F32)
            dcs = dsem()
            t_cst = dma(SP, [], dcs, cst_sb[:], cst[:, :])
            U128 = cst_sb[:, 0:128]
            ONES = cst_sb[:, 128:256]
            USTR = cst_sb[0:NE, 256:288]
            jrow = cst_sb[:, C_J:C_J + NTILE]
            erow = cst_sb[:, C_E:C_E + NE]
            pk = cst_sb[:, C_PK:C_PK + 8]
            bigp = cst_sb[:, C_BIG:C_BIG + 1]
            prow = cst_sb[:, C_P:C_P + 1]
            cum = sbt(ph, "cum", [128, NOWN, NE], F32)
            pos_ps = pst(ph, "pos_ps", [128, NOWN, NE], F32)
            cnt_ps = pst(ph, "cnt_ps", [128, NE], F32)
            ntT_ps = pst(ph, "ntT_ps", [NE, 128], F32)
            ts_ps = pst(ph, "ts_ps", [128, NE], F32)
            cnt_sb = sbt(ph, "cnt_sb", [128, NE], F32)
            nt = sbt(ph, "nt", [128, NE], F32)
            ntT = sbt(ph, "ntT", [NE, 128], F32)
            tstart = sbt(ph, "tstart", [128, NE], F32)
            tend = sbt(ph, "tend", [128, NE], F32)
            slotf = sbt(ph, "slotf", [128, NOWN, NE], F32)
            oh = sbt(ph, "oh", [128, NOWN, NE], F32)
            tmpm = sbt(ph, "tmpm", [128, NOWN, NE], F32)
            slotsel = sbt(ph, "slotsel", [128, NOWN, 4], F32)
            gate = sbt(ph, "gate", [128, NOWN, 4], F32)
            A3 = sbt(ph, "A3", [128, NTILE, NE], F32)
            B3 = sbt(ph, "B3", [128, NTILE, NE], F32)
            used = sbt(ph, "used", [128, NTILE], F32)
            texp = sbt(ph, "texp", [128, NTILE], F32)
            cfill = sbt(ph, "cfill", [128, NTILE], F32)
            wf = sbt(ph, "wf", [128, NTILE, 8], F32)
            bf_ = sbt(ph, "bf_", [128, NTILE], F32)

            tq = op(DVE, [], nc.vector.memset, cum[:, 0, :], 0.0)
            for t in range(1, NOWN):
                tq = op(DVE, [tq], nc.vector.tensor_tensor, out=cum[:, t, :], in0=cum[:, t - 1, :], in1=maskall[:, t - 1, :], op=ALU.add)
            PE.wait(flat([tq, t_cst]))
            for t in range(NOWN):
                nc.tensor.matmul(pos_ps[:, t, :], lhsT=U128, rhs=maskall[:, t, :], start=True, stop=False)
                mm = nc.tensor.matmul(pos_ps[:, t, :], lhsT=ONES, rhs=cum[:, t, :], start=False, stop=True)
            nc.tensor.matmul(cnt_ps[:, :], lhsT=ONES, rhs=cum[:, NOWN - 1, :], start=True, stop=False)
            tpos = PE.fin(nc.tensor.matmul(cnt_ps[:, :], lhsT=ONES, rhs=maskall[:, NOWN - 1, :], start=False, stop=True))
            q = op(DVE, [tpos], nc.vector.tensor_copy, out=cnt_sb[:], in_=cnt_ps[:])
            q = op(DVE, [q], nc.vector.tensor_scalar, out=nt[:], in0=cnt_sb[:], scalar1=0.0, scalar2=None, op0=ALU.is_gt)
            for thr in [float(TS * i_) for i_ in range(1, 2048 // TS + 1) if TS * i_ < 2048]:
                q = op(DVE, [q], nc.vector.scalar_tensor_tensor, out=nt[:], in0=cnt_sb[:], scalar=thr, in1=nt[:], op0=ALU.is_gt, op1=ALU.add)
            PE.wait(flat([q, t_idf]))
            tnt = PE.fin(nc.tensor.transpose(ntT_ps[:, :], nt[:], identf[:]))
            q2 = op(DVE, [tnt], nc.vector.tensor_copy, out=ntT[:], in_=ntT_ps[:])
            PE.wait(flat([q2]))
            tts = PE.fin(nc.tensor.matmul(ts_ps[:, :], lhsT=ntT[:, :], rhs=USTR, start=True, stop=True))
            q = op(DVE, [tts], nc.vector.tensor_copy, out=tstart[:], in_=ts_ps[:])
            q = op(DVE, [q], nc.vector.tensor_tensor, out=tend[:], in0=tstart[:], in1=nt[:], op=ALU.add)
            q = op(DVE, [q], nc.vector.scalar_tensor_tensor, out=slotf[:], in0=tstart[:].unsqueeze(1).to_broadcast([128, NOWN, NE]), scalar=float(TS), in1=pos_ps[:],
                   op0=ALU.mult, op1=ALU.add)
            for k in range(4):
                q = op(DVE, [q], nc.vector.tensor_tensor, out=oh[:], in0=lg_all[:], in1=m8_all[:, :, k:k + 1].to_broadcast([128, NOWN, NE]), op=ALU.is_equal)
                q = op(DVE, [q], nc.vector.tensor_tensor, out=tmpm[:], in0=oh[:], in1=slotf[:], op=ALU.mult)
                q = op(DVE, [q], nc.vector.tensor_reduce, out=slotsel[:, :, k], in_=tmpm[:], axis=AX.X, op=ALU.add)
                q = op(DVE, [q], nc.vector.tensor_tensor, out=tmpm[:], in0=oh[:], in1=comb[:], op=ALU.mult)
                q = op(DVE, [q], nc.vector.tensor_reduce, out=gate[:, :, k], in_=tmpm[:], axis=AX.X, op=ALU.add)
            q = op(DVE, [q], nc.vector.tensor_copy, out=slot_i[:], in_=slotsel[:].rearrange("p t k -> p (t k)"))
            q = op(DVE, [q], nc.vector.tensor_scalar, out=gate2[:], in0=gate[:], scalar1=float(1.0 / 1.702), scalar2=None, op0=ALU.mult)
            q = op(DVE, [q], nc.vector.tensor_tensor, out=A3[:], in0=tstart[:].unsqueeze(1).to_broadcast([128, NTILE, NE]),
                   in1=jrow.unsqueeze(2).to_broadcast([128, NTILE, NE]), op=ALU.is_le)
            q = op(DVE, [q], nc.vector.tensor_tensor, out=B3[:], in0=tend[:].unsqueeze(1).to_broadcast([128, NTILE, NE]),
                   in1=jrow.unsqueeze(2).to_broadcast([128, NTILE, NE]), op=ALU.is_gt)
            q = op(DVE, [q], nc.vector.tensor_tensor, out=A3[:], in0=A3[:], in1=B3[:], op=ALU.mult)
            q = op(DVE, [q], nc.vector.tensor_reduce, out=used[:], in_=A3[:], axis=AX.X, op=ALU.add)
            q = op(DVE, [q], nc.vector.tensor_tensor, out=B3[:], in0=A3[:], in1=erow.unsqueeze(1).to_broadcast([128, NTILE, NE]), op=ALU.mult)
            q = op(DVE, [q], nc.vector.tensor_reduce, out=texp[:], in_=B3[:], axis=AX.X, op=ALU.add)
            q = op(DVE, [q], nc.vector.tensor_scalar, out=cfill[:], in0=used[:], scalar1=-1.0, scalar2=1.0, op0=ALU.mult, op1=ALU.add)
            q = op(DVE, [q], nc.vector.tensor_scalar, out=cfill[:], in0=cfill[:], scalar1=bigp, scalar2=None, op0=ALU.mult)
            q = op(DVE, [q], nc.vector.tensor_scalar, out=bf_[:], in0=texp[:], scalar1=1024.0, scalar2=None, op0=ALU.mult)
            q = op(DVE, [q], nc.vector.tensor_tensor, out=wf[:], in0=bf_[:].unsqueeze(2).to_broadcast([128, NTILE, 8]),
                   in1=pk.unsqueeze(1).to_broadcast([128, NTILE, 8]), op=ALU.add)
            q = op(DVE, [q], nc.vector.tensor_tensor, out=wf[:], in0=wf[:], in1=used[:].unsqueeze(2).to_broadcast([128, NTILE, 8]), op=ALU.mult)
            q = op(DVE, [q], nc.vector.tensor_tensor, out=wf[:], in0=wf[:], in1=cfill[:].unsqueeze(2).to_broadcast([128, NTILE, 8]), op=ALU.add)
            q = op(DVE, [q], nc.vector.tensor_copy, out=widx_i[:], in_=wf[:].rearrange("p j k -> p (j k)"))
            q = op(DVE, [q], nc.vector.tensor_scalar, out=bf_[:], in0=texp[:], scalar1=128.0, scalar2=prow, op0=ALU.mult, op1=ALU.add)
            q = op(DVE, [q], nc.vector.tensor_tensor, out=bf_[:], in0=bf_[:], in1=used[:], op=ALU.mult)
            q = op(DVE, [q], nc.vector.tensor_tensor, out=bf_[:], in0=bf_[:], in1=cfill[:], op=ALU.add)
            t_meta = op(DVE, [q], nc.vector.tensor_copy, out=bidx_i[:], in_=bf_[:])
            barrier()

        with ExitStack() as ph:
            hb = [sbt(ph, "hb%d" % i, [128, D], BF16) for i in range(2)]
            hb_ds = [dsem() for _ in range(2)]
            hb_free = [None] * 2
            dscat = [dsem(), dsem()]
            for t in range(NOWN):
                i = t % 2
                tl = dma(SP, [t_hx, hb_free[i]], hb_ds[i], hb[i][:], Hx[t * 128:(t + 1) * 128, :])
                POOL.wait(flat([tl, t_meta, t_zero]))
                for k in range(4):
                    nc.gpsimd.indirect_dma_start(out=Hs[:, :], out_offset=bass.IndirectOffsetOnAxis(ap=slot_i[:, t * 4 + k:t * 4 + k + 1], axis=0),
                                                 in_=hb[i][:], in_offset=None, bounds_check=bcreg_slot, oob_is_err=False).then_inc(dscat[i].sem, 16)
                    dscat[i].n += 16
                hb_free[i] = (dscat[i], dscat[i].n)
            t_scat = [(d_, d_.n) for d_ in dscat]

            wg = [sbt(ph, "wg%d" % i, [128, 8 * 2048], BF16) for i in range(3)]
            wd = [sbt(ph, "wd%d" % i, [128, 8 * 1024], BF16) for i in range(2)]
            bgt = [sbt(ph, "bgt%d" % i, [128, 16], F32) for i in range(3)]
            wg_ds = [dsem() for _ in range(3)]
            wd_ds = [dsem() for _ in range(2)]
            wg_free = [None] * 3
            wd_free = [None] * 2
            wg_tok = {}
            wd_tok = {}
            hrow = [sbt(ph, "hrow%d" % i, [128, D], BF16) for i in range(3)]
            hrow_ds = [dsem() for _ in range(3)]
            hrow_free = [None] * 3
            hT = [sbt(ph, "hTm%d" % i, [128, 8, TS], BF16) for i in range(2)]
            hT_free = [None, None]
            hT_tok = {}
            actT = [sbt(ph, "actT%d" % i, [128, 8, TS], BF16) for i in range(2)]
            actT_free = [None, None]
            tp_ps = Ring([pst(ph, "tp5_ps%d" % i, [128, 8, 128], BF16) for i in range(2)])
            g_ps = Ring([pst(ph, "g_ps%d" % i, [128, 512], F32) for i in range(2)])
            l_ps = Ring([pst(ph, "l_ps%d" % i, [128, 512], F32) for i in range(2)])
            y_ps = Ring([pst(ph, "y_ps%d" % i, [128, 512], F32) for i in range(2)])
            gc = Ring([sbt(ph, "gc%d" % i, [128, TS], F32) for i in range(2)])
            sg = Ring([sbt(ph, "sg%d" % i, [128, TS], F32) for i in range(2)])
            lc = Ring([sbt(ph, "lc%d" % i, [128, TS], F32) for i in range(2)])
            ystage = [sbt(ph, "ystage%d" % i, [128, D], F32) for i in range(2)]
            ys_free = [None, None]
            dys = [dsem(), dsem()]
            ysn = [0]
            pend = {}
            bias_tok = {}
            nrow = [0]

            ORDER = []
            lo_, hi_ = 0, NTILE - 1
            while lo_ <= hi_:
                if len(ORDER) % 3 == 2:
                    ORDER.append(hi_)
                    hi_ -= 1
                else:
                    ORDER.append(lo_)
                    lo_ += 1
            assert sorted(ORDER) == list(range(NTILE))

            def load_wg(j):
                b = j % 3
                tj = ORDER[j]
                POOL.wait(flat([wg_free[b], t_meta]))
                for k in range(8):
                    nc.gpsimd.indirect_dma_start(out=wg[b][:, k * 2048:(k + 1) * 2048], out_offset=None, in_=wgu_rows[:, :],
                                                 in_offset=bass.IndirectOffsetOnAxis(ap=widx_i[:, tj * 8 + k:tj * 8 + k + 1], axis=0),
                                                 bounds_check=bcreg_w, oob_is_err=False).then_inc(wg_ds[b].sem, 16)
                    wg_ds[b].n += 16
                nc.gpsimd.indirect_dma_start(out=bgt[b][:], out_offset=None, in_=bgu_rows[:, :],
                                             in_offset=bass.IndirectOffsetOnAxis(ap=bidx_i[:, tj:tj + 1], axis=0),
                                             bounds_check=bcreg_b, oob_is_err=False).then_inc(wg_ds[b].sem, 16)
                wg_ds[b].n += 16
                wg_tok[j] = (wg_ds[b], wg_ds[b].n)

            def load_wd(j):
                b = j % 2
                tj = ORDER[j]
                POOL.wait(flat([wd_free[b], t_meta]))
                for k in range(8):
                    nc.gpsimd.indirect_dma_start(out=wd[b][:, k * 1024:(k + 1) * 1024], out_offset=None, in_=wd_rows[:, :],
                                                 in_offset=bass.IndirectOffsetOnAxis(ap=widx_i[:, tj * 8 + k:tj * 8 + k + 1], axis=0),
                                                 bounds_check=bcreg_w, oob_is_err=False).then_inc(wd_ds[b].sem, 16)
                    wd_ds[b].n += 16
                wd_tok[j] = (wd_ds[b], wd_ds[b].n)

            def emit_rows(j):
                jb = j % 2
                tev = None
                for sidx in range(NSUB):
                    i = nrow[0] % 3
                    nrow[0] += 1
                    r0 = ORDER[j] * TS + sidx * 128
                    tl = dma(SP, [t_scat, hrow_free[i]], hrow_ds[i], hrow[i][:], Hs[r0:r0 + 128, :])
                    pi, tpp, tpfree = tp_ps.next()
                    PE.wait(flat([tl, tpfree, t_idb]))
                    for k in range(8):
                        mm = nc.tensor.transpose(tpp[:, k, :], hrow[i][:, k * 128:(k + 1) * 128], identb[:])
                    ttp = PE.fin(mm)
                    hrow_free[i] = ttp
                    tev = op(ACT, [ttp, hT_free[jb] if sidx == 0 else None], nc.scalar.activation, out=hT[jb][:, :, sidx * 128:(sidx + 1) * 128], in_=tpp[:], func=AF.Identity)
                    tp_ps.rel(pi, tev)
                hT_tok[j] = tev

            def emit_gu(j):
                b = j % 3
                jb = j % 2
                ab = j % 2
                tbias = op(DVE, [wg_tok[j]], nc.vector.tensor_scalar, out=bgt[b][:, 8:16], in0=bgt[b][:, 8:16], scalar1=1.0, scalar2=None, op0=ALU.add)
                last_tok = None
                for jc in range(8):
                    gi, gp, gfree = g_ps.next()
                    li, lp, lfree = l_ps.next()
                    PE.wait(flat([wg_tok[j], hT_tok[j], gfree, lfree]))
                    for k in range(8):
                        nc.tensor.matmul(gp[:, 0:TS], lhsT=wg[b][:, k * 2048 + jc * 128:k * 2048 + (jc + 1) * 128], rhs=hT[jb][:, k, :], start=(k == 0), stop=(k == 7))
                    for k in range(8):
                        mm = nc.tensor.matmul(lp[:, 0:TS], lhsT=wg[b][:, k * 2048 + 1024 + jc * 128:k * 2048 + 1024 + (jc + 1) * 128], rhs=hT[jb][:, k, :], start=(k == 0), stop=(k == 7))
                    tmm = PE.fin(mm)
                    bg_ap = bgt[b][:, jc:jc + 1]
                    bl_ap = bgt[b][:, 8 + jc:9 + jc]
                    ci, gct, gcfree = gc.next()
                    a1 = op(DVE, [tmm, tbias, gcfree], nc.vector.tensor_scalar, out=gct[:, 0:TS], in0=gp[:, 0:TS], scalar1=bg_ap, scalar2=7.0, op0=ALU.add, op1=ALU.min)
                    g_ps.rel(gi, a1)
                    xi, sgt, sgfree = sg.next()
                    a2 = op(ACT, [a1, sgfree], nc.scalar.activation, out=sgt[:, 0:TS], in_=gct[:, 0:TS], func=AF.Silu, scale=1.702)
                    gc.rel(ci, a2)
                    yi, lct, lcfree = lc.next()
                    a3 = op(DVE, [tmm, tbias, lcfree], nc.vector.tensor_scalar, out=lct[:, 0:TS], in0=lp[:, 0:TS], scalar1=bl_ap, scalar2=8.0, op0=ALU.add, op1=ALU.min)
                    l_ps.rel(li, a3)
                    a6 = op(DVE, [a2, a3, actT_free[ab] if jc == 0 else None], nc.vector.scalar_tensor_tensor, out=actT[ab][:, jc, :], in0=lct[:, 0:TS], scalar=-6.0, in1=sgt[:, 0:TS],
                            op0=ALU.max, op1=ALU.mult)
                    sg.rel(xi, a6)
                    lc.rel(yi, a6)
                    last_tok = a6
                hT_free[jb] = tmm
                pend[j] = last_tok
                wg_free[b] = [tmm, last_tok]
                if j + 3 < NTILE:
                    load_wg(j + 3)

            def emit_down(j):
                b = j % 2
                ab = j % 2
                tmm = None
                for sidx in range(NSUB):
                    yb = ysn[0] % 2
                    ysn[0] += 1
                    evs = []
                    for hc in range(2):
                        yi, yp, yfree = y_ps.next()
                        PE.wait(flat([pend[j], yfree, wd_tok[j]]))
                        for jc in range(8):
                            mm = nc.tensor.matmul(yp[:, :], lhsT=actT[ab][:, jc, sidx * 128:(sidx + 1) * 128], rhs=wd[b][:, jc * 1024 + hc * 512:jc * 1024 + (hc + 1) * 512],
                                                  start=(jc == 0), stop=(jc == 7))
                        tmm = PE.fin(mm)
                        ev = op(DVE, [tmm, ys_free[yb]], nc.vector.tensor_tensor, out=ystage[yb][:, hc * 512:(hc + 1) * 512], in0=yp[:, :], in1=g2bc[:, hc * 512:(hc + 1) * 512], op=ALU.mult)
                        y_ps.rel(yi, ev)
                        evs.append(ev)
                    r0 = ORDER[j] * TS + sidx * 128
                    ys_free[yb] = dma(SP, evs, dys[yb], Ys[r0:r0 + 128, :], ystage[yb][:])
                actT_free[ab] = tmm
                wd_free[b] = tmm
                del pend[j]
                if j + 2 < NTILE:
                    load_wd(j + 2)

            load_wg(0)
            load_wd(0)
            load_wg(1)
            load_wd(1)
            load_wg(2)
            emit_rows(0)
            for j in range(NTILE):
                emit_gu(j)
                if j + 1 < NTILE:
                    emit_rows(j + 1)
                emit_down(j)
            t_ys = [(d_, d_.n) for d_ in dys]
            SP.wait(t_ys)
            barrier()

        with ExitStack() as ph:
            ln2gbc = sbt(ph, "ln2gbc", [128, D], F32)
            ln2bbc = sbt(ph, "ln2bbc", [128, D], F32)
            dcc = dsem()
            dma(SP, [], dcc, ln2gbc[:], ln2_g.partition_broadcast(128))
            t_cc = dma(SP, [], dcc, ln2bbc[:], ln2_b.partition_broadcast(128))
            st = [sbt(ph, "st6_%d" % i, [128, 12], F32) for i in range(2)]
            mv = [sbt(ph, "mv6_%d" % i, [128, 2], F32) for i in range(2)]
            rs = [sbt(ph, "rs6_%d" % i, [128, 2], F32) for i in range(2)]
            ob = [sbt(ph, "ob%d" % i, [128, D], F32) for i in range(2)]
            ob_free = [None, None]
            accs = [sbt(ph, "accs%d" % i, [128, D], F32) for i in range(2)]
            accs_ds = [dsem(), dsem()]
            accs_free = [None, None]
            yk = [[sbt(ph, "yk%d_%d" % (i, k), [128, D], F32) for k in range(4)] for i in range(3)]
            yk_ds = [dsem(), dsem(), dsem()]
            yk_free = [None, None, None]
            ssum = [sbt(ph, "ssum%d" % i, [128, D], F32) for i in range(2)]
            ssum_free = [None, None]
            dout = [dsem(), dsem()]
            tyk = {}

            def issue_gather(t):
                b = t % 3
                POOL.wait(flat([yk_free[b], t_ys]))
                for k in range(4):
                    nc.gpsimd.indirect_dma_start(out=yk[b][k][:], out_offset=None, in_=Ys[:, :],
                                                 in_offset=bass.IndirectOffsetOnAxis(ap=slot_i[:, t * 4 + k:t * 4 + k + 1], axis=0),
                                                 bounds_check=bcreg_slot, oob_is_err=False).then_inc(yk_ds[b].sem, 16)
                    yk_ds[b].n += 16
                tyk[t] = (yk_ds[b], yk_ds[b].n)

            issue_gather(0)
            issue_gather(1)
            issue_gather(2)
            trs_t = {}

            def stage_a(t):
                b = t % 2
                tla = dma(SP, [t_accd, accs_free[b]], accs_ds[b], accs[b][:], accd[t * 128:(t + 1) * 128, :])
                yb = t % 3
                q = op(DVE, [tyk[t], tla, ssum_free[b]], nc.vector.scalar_tensor_tensor, out=ssum[b][:], in0=yk[yb][0][:], scalar=gate2[:, t, 0:1], in1=accs[b][:], op0=ALU.mult, op1=ALU.add)
                accs_free[b] = q
                for k in range(1, 4):
                    q = op(DVE, [q], nc.vector.scalar_tensor_tensor, out=ssum[b][:], in0=yk[yb][k][:], scalar=gate2[:, t, k:k + 1], in1=ssum[b][:], op0=ALU.mult, op1=ALU.add)
                yk_free[yb] = q
                trs_t[t] = ln_stats(ssum[b], D, [q], st[b], mv[b], rs[b])
                if t + 3 < NOWN:
                    issue_gather(t + 3)

            def stage_b(t):
                b = t % 2
                trs = trs_t.pop(t)
                tn_ = op(DVE, [trs], nc.vector.scalar_tensor_tensor, out=rs[b][:, 1:2], in0=mv[b][:, 0:1], scalar=-1.0, in1=rs[b][:, 0:1], op0=ALU.mult, op1=ALU.mult)
                t1_ = op(ACT, [tn_, ob_free[b]], nc.scalar.activation, out=ob[b][:], in_=ssum[b][:], func=AF.Identity, scale=rs[b][:, 0:1], bias=rs[b][:, 1:2])
                ssum_free[b] = t1_
                t2_ = op(DVE, [t1_, t_cc], nc.vector.tensor_tensor, out=ob[b][:], in0=ob[b][:], in1=ln2gbc[:], op=ALU.mult)
                t3_ = op(DVE, [t2_], nc.vector.tensor_tensor, out=ob[b][:], in0=ob[b][:], in1=ln2bbc[:], op=ALU.add)
                ob_free[b] = dma(SP, [t3_], dout[b], out[t * 128:(t + 1) * 128, :], ob[b][:])

            stage_a(0)
            for t in range(NOWN):
                if t + 1 < NOWN:
                    stage_a(t + 1)
                stage_b(t)
            SP.wait([(d_, d_.n) for d_ in dout])
    return nc


def _bias_tables(rpb):
    c = np.arange(64)
    cs = np.clip(c - 8, 0, 48)
    kc = np.arange(64)
    colvalid = (kc[None, :] >= cs[:, None]) & (kc[None, :] < cs[:, None] + 16)
    dcidx = np.clip(kc[None, :] - c[:, None] + 15, 0, 30)

    def table(r0, lr0, a, nrows, width):
        T = np.full((2, 64, 8, width), NEG, np.float32)
        for i in range(2):
            r = r0 + lr0 + i
            rs = min(max(r - 4, 0), 120)
            for w in range(nrows):
                kr = r0 - 4 + a + w
                if kr < rs or kr >= rs + 8:
                    continue
                dr = kr - r + 7
                vals = rpb[:, dr, :][:, dcidx]
                vals = np.where(colvalid[None], vals, np.float32(NEG))
                T[i, :, :, w * 64:(w + 1) * 64] = vals.transpose(1, 0, 2)
            T[i, :, :, nrows * 64:nrows * 64 + 256] = 0.0
        return T.reshape(128, 8 * width)

    gen = table(32, 8, 8, 9, 832)
    sp = {}
    for q in range(4):
        r0 = 32 * q
        sp[q] = np.stack([table(r0, 2 * p, SPECIAL[p][0], SPECIAL[p][1], 1024) for p in SP_ORDER])
    return gen, sp


def _consts(core):
    c = np.zeros((128, CW), np.float32)
    p = np.arange(128)
    c[:, 0:128] = (p[:, None] < p[None, :]).astype(np.float32)
    c[:, 128:256] = 1.0
    e = np.arange(NE)
    r = (e - 4 * core) % NE
    c[0:NE, 256:288] = (r[:, None] < r[None, :]).astype(np.float32)
    c[:, C_J:C_J + NTILE] = np.arange(NTILE)[None, :]
    c[:, C_E:C_E + NE] = e[None, :]
    c[:, C_PK:C_PK + 8] = np.arange(8)[None, :] * 128 + p[:, None]
    c[:, C_BIG] = np.where(p == 0, 0.0, BIGIDX)
    c[:, C_P] = p
    return c


_NC_CACHE = {}


def kernel(x, c, ctx, c_ctx, ada_w, ada_b, w_in, rpb, sgu_ln_g, sgu_ln_b, sgu_w, sgu_b,
           w_out, ln1_g, ln1_b, ln2_g, ln2_b, router_w, router_b,
           exp_w_gu, exp_b_gu, exp_w_down, exp_b_down, _debug=False):
    f = lambda a: np.ascontiguousarray(np.asarray(a, dtype=np.float32))
    x, c, ctx, c_ctx = f(x), f(c), f(ctx), f(c_ctx)
    ada_w0, ada_b0 = f(ada_w)[0], f(ada_b)[0]
    gen, sp = _bias_tables(f(rpb)[0])
    shared = {
        "ada_w": ada_w0,
        "adabT": np.ascontiguousarray(ada_b0.reshape(48, 128).T),
        "ada_b": ada_b0,
        "w_in": f(w_in)[0],
        "w_out": f(w_out)[0],
        "bias_gen": gen,
        "sgu_ln_g": f(sgu_ln_g)[0],
        "sgu_ln_b": f(sgu_ln_b)[0],
        "wsT": np.ascontiguousarray(f(sgu_w)[0].transpose(2, 0, 1).reshape(128, 512)),
        "sgu_bv": np.ascontiguousarray(f(sgu_b)[0].reshape(512)),
        "ln1_g": f(ln1_g)[0], "ln1_b": f(ln1_b)[0], "ln2_g": f(ln2_g)[0], "ln2_b": f(ln2_b)[0],
        "router_w": f(router_w)[0], "router_b": f(router_b)[0],
        "exp_w_gu": f(exp_w_gu)[0],
        "bgu_rows": np.ascontiguousarray(f(exp_b_gu)[0].reshape(NE, 16, 128).transpose(0, 2, 1).reshape(NE * 128, 16)),
        "zeros": np.zeros((512, D), dtype=ml_dtypes.bfloat16),
        "exp_w_down": f(exp_w_down)[0],
        "exp_b_down": f(exp_b_down)[0],
        "ident": np.eye(128, dtype=np.float32),
    }
    in_maps = []
    for j in range(NCORES):
        b, q = j // 4, j % 4
        r0 = 32 * q
        xg = x[b].reshape(128, 64, D)
        xe = np.zeros((NT_ALL * 128, D), np.float32)
        xev = xe[:NEXT_T * 128].reshape(40, 64, D)
        lo, hi = r0 - 4, r0 + 36
        slo, shi = max(lo, 0), min(hi, 128)
        xev[slo - lo:shi - lo] = xg[slo:shi]
        xe[NEXT_T * 128:] = ctx[b]
        cc = np.stack([c[b], c_ctx], axis=1)
        cT = np.ascontiguousarray(cc.reshape(8, 128, 2).transpose(1, 0, 2).reshape(128, 16))
        m = dict(shared)
        m["xe"] = xe
        m["cT"] = cT
        m["bias_sp"] = sp[q]
        m["cst"] = _consts(j)
        in_maps.append(m)
    key = bool(_debug)
    if key not in _NC_CACHE:
        _NC_CACHE[key] = build(debug=key)
    nc = _NC_CACHE[key]
    res = run_bass_kernel_spmd(nc, in_maps, core_ids=list(range(NCORES)))
    outs = [np.asarray(r["out"], dtype=np.float32) for r in res.results]
    full = np.concatenate(outs, axis=0).reshape(2, 8192, D)
    if _debug:
        return full, res.results
    return full
```

```python
import numpy as np
import ml_dtypes
from contextlib import ExitStack
import concourse.bass as bass
import concourse.mybir as mybir
from concourse.bass_utils import run_bass_kernel_spmd

F32 = mybir.dt.float32
BF16 = mybir.dt.bfloat16
I32 = mybir.dt.int32
AF = mybir.ActivationFunctionType
ALU = mybir.AluOpType
AX = mybir.AxisListType

NCORES = 8
D = 1024
NEXT_T = 20
NT_ALL = 22
OWN0 = 2
NOWN = 16
NTOK = NOWN * 128
NEG = -30000.0
ALPHA = 2.0 ** 0.25
LN_EPS = 1e-5
NE = 32
TS = 384
NSUB = TS // 128
NTILE = (4 * 2048 + NE * (TS - 1)) // TS
NSLOT = NTILE * TS
C_J = 288
C_E = C_J + NTILE
C_PK = C_E + NE
C_BIG = C_PK + 8
C_P = C_BIG + 1
CW = 384
assert C_P < CW
BIGIDX = 40000.0
SPECIAL = {0: (0, 12), 1: (2, 10), 14: (28, 9), 15: (28, 11)}
SP_ORDER = [0, 1, 14, 15]
PAIR_ORDER = [0, 2, 3, 4, 1, 5, 6, 7, 8, 14, 9, 10, 11, 15, 12, 13]


class Eng:
    def __init__(s, es, nc, eng, name):
        s.eng = eng
        s.name = name
        s.sem = es.enter_context(nc.semaphore("sem_" + name))
        s.n = 0
        s.seen = {}

    def wait(s, toks):
        for t in toks:
            if t is None:
                continue
            e, c = t
            if s.seen.get(e, 0) < c:
                s.eng.wait_ge(e.sem, c)
                s.seen[e] = c

    def fin(s, inst):
        inst.then_inc(s.sem, 1)
        s.n += 1
        return (s, s.n)

    def last(s):
        return (s, s.n) if s.n > 0 else None


class DSem:
    def __init__(s, es, nc, name):
        s.sem = es.enter_context(nc.semaphore("dsem_" + name))
        s.n = 0


def flat(deps):
    out = []
    for d in deps:
        if d is None:
            continue
        if isinstance(d, list):
            out.extend(flat(d))
        else:
            out.append(d)
    return out


class Ring:
    def __init__(s, bufs):
        s.bufs = bufs
        s.free = [None] * len(bufs)
        s.i = 0

    def next(s):
        i = s.i % len(s.bufs)
        s.i += 1
        return i, s.bufs[i], s.free[i]

    def rel(s, i, tok):
        s.free[i] = tok


def build(debug=False):
    nc = bass.Bass("TRN2", target_bir_lowering=False)

    def din(name, shape):
        return nc.dram_tensor(name, shape, F32, kind="ExternalInput").ap()

    xe = din("xe", [NT_ALL * 128, D])
    cT = din("cT", [128, 16])
    ada_w = din("ada_w", [D, 6 * D])
    adabT = din("adabT", [128, 48])
    ada_b = din("ada_b", [6 * D])
    w_in = din("w_in", [D, 2560])
    w_out = din("w_out", [D, D])
    bias_gen = din("bias_gen", [128, 8 * 832])
    bias_sp = din("bias_sp", [4, 128, 8 * 1024])
    sgu_ln_g = din("sgu_ln_g", [512])
    sgu_ln_b = din("sgu_ln_b", [512])
    wsT = din("wsT", [128, 512])
    sgu_bv = din("sgu_bv", [512])
    ln1_g = din("ln1_g", [D])
    ln1_b = din("ln1_b", [D])
    ln2_g = din("ln2_g", [D])
    ln2_b = din("ln2_b", [D])
    router_w = din("router_w", [D, NE])
    router_b = din("router_b", [NE])
    exp_w_gu = din("exp_w_gu", [NE, D, 2 * D])
    exp_w_down = din("exp_w_down", [NE, D, D])
    exp_b_down = din("exp_b_down", [NE, D])
    ident = din("ident", [128, 128])
    cst = din("cst", [128, CW])
    zeros = nc.dram_tensor("zeros", [512, D], BF16, kind="ExternalInput").ap()
    bgu_rows = din("bgu_rows", [NE * 128, 16])
    out = nc.dram_tensor("out", [NTOK, D], F32, kind="ExternalOutput").ap()
    Hx = nc.dram_tensor("Hx", [NTOK, D], BF16, kind="Internal").ap()
    Hs = nc.dram_tensor("Hs", [NSLOT, D], BF16, kind="Internal").ap()
    Ys = nc.dram_tensor("Ys", [NSLOT, D], F32, kind="Internal").ap()
    accd = nc.dram_tensor("accd", [NTOK, D], F32, kind="Internal").ap()
    wgu_rows = exp_w_gu.rearrange("e r n -> (e r) n")
    wd_rows = exp_w_down.rearrange("e r n -> (e r) n")
    dbg = {}
    if debug:
        for nm, shp in [("d_KT", [128, 4 * 2816]), ("d_V", [128, 22 * 512]), ("d_QT", [128, 4 * 2048]),
                        ("d_sguT", [128, 4 * 2048]), ("d_attT", [128, 4 * 2048]), ("d_acc", [128, 16 * 1024]),
                        ("d_h2T", [128, 8 * 2048]), ("d_comb", [128, 16 * 32]), ("d_ada", [128, 96]),
                        ("d_g1bc", [128, 1024])]:
            dbg[nm] = nc.dram_tensor(nm, shp, F32, kind="ExternalOutput").ap()

    with ExitStack() as es:
        PE = Eng(es, nc, nc.tensor, "pe")
        ACT = Eng(es, nc, nc.scalar, "act")
        DVE = Eng(es, nc, nc.vector, "dve")
        POOL = Eng(es, nc, nc.gpsimd, "pool")
        SP = Eng(es, nc, nc.sync, "sp")
        ENGS = [PE, ACT, DVE, POOL, SP]
        nds = [0]

        def dsem():
            nds[0] += 1
            return DSem(es, nc, "d%d" % nds[0])

        def op(E, deps, fn, *a, **k):
            E.wait(flat(deps))
            return E.fin(fn(*a, **k))

        def dma(Q, deps, ds, out_, in_):
            Q.wait(flat(deps))
            Q.eng.dma_start(out=out_, in_=in_).then_inc(ds.sem, 16)
            ds.n += 16
            return (ds, ds.n)

        def barrier():
            toks = [e.last() for e in ENGS]
            for e in ENGS:
                e.wait([t for t in toks if t is not None and t[0] is not e])

        dbg_sem = dsem()

        dummy = es.enter_context(nc.sbuf_tensor("dummy_t", [128, 8], F32))

        def dump(name, src_ap, deps, dt=F32, shape=None):
            if not debug:
                return
            W = src_ap.shape[-1]
            with ExitStack() as tmp:
                CH = 1024
                stg = tmp.enter_context(nc.sbuf_tensor("stg_" + name, [128, CH], F32))
                t2 = None
                for c0 in range(0, W, CH):
                    c1 = min(W, c0 + CH)
                    t = op(DVE, flat([deps, t2]), nc.vector.tensor_copy, out=stg[:, 0:c1 - c0], in_=src_ap[:, c0:c1])
                    t2 = dma(SP, [t], dbg_sem, dbg[name][:, c0:c1], stg[:, 0:c1 - c0])
                SP.wait([t2])
                op(DVE, [t2], nc.vector.memset, dummy[:], 0.0)

        def sbt(stack, name, shape, dt):
            return stack.enter_context(nc.sbuf_tensor(name, shape, dt))

        def pst(stack, name, shape, dt):
            return stack.enter_context(nc.psum_tensor(name, shape, dt))

        identf = sbt(es, "identf", [128, 128], F32)
        identb = sbt(es, "identb", [128, 128], BF16)
        adaT = sbt(es, "adaT", [128, 48, 2], F32)
        modv = sbt(es, "modv", [128, 6, 8], F32)
        g1bc = sbt(es, "g1bc", [128, D], F32)
        g2bc = sbt(es, "g2bc", [128, D], F32)
        m05 = sbt(es, "m05", [128, 1], F32)
        sh2bc = sbt(es, "sh2bc", [128, D], F32)
        sc2bc = sbt(es, "sc2bc", [128, D], F32)
        comb = sbt(es, "comb", [128, NOWN, NE], F32)
        slot_i = sbt(es, "slot_i", [128, NOWN * 4], I32)
        gate2 = sbt(es, "gate2", [128, NOWN, 4], F32)
        widx_i = sbt(es, "widx_i", [128, NTILE * 8], I32)
        bidx_i = sbt(es, "bidx_i", [128, NTILE], I32)
        dc0 = dsem()
        dc1 = dsem()
        t_idf = dma(SP, [], dc0, identf[:], ident[:, :])
        t_idb = dma(POOL, [], dc1, identb[:], ident[:, :])
        t_m05 = op(POOL, [], nc.gpsimd.memset, m05[:], -0.5)

        def ln_stats(src, width, deps, st_t, mv_t, rs_t):
            nchunk = width // 512
            toks = []
            for c in range(nchunk):
                toks.append(op(DVE, deps, nc.vector.bn_stats, out=st_t[:, c * 6:(c + 1) * 6], in_=src[:, c * 512:(c + 1) * 512]))
            t = op(DVE, toks, nc.vector.bn_aggr, out=mv_t[:, 0:2], in_=st_t[:, 0:6 * nchunk])
            t = op(POOL, [t, t_m05], nc.gpsimd.tensor_scalar, out=rs_t[:, 1:2], in0=mv_t[:, 1:2], scalar1=LN_EPS, scalar2=None, op0=ALU.add)
            t = op(POOL, [t], nc.gpsimd.tensor_tensor, out=rs_t[:, 0:1], in0=rs_t[:, 1:2], in1=m05[:], op=ALU.pow)
            return t

        with ExitStack() as ph:
            cT_sb = sbt(ph, "cT_sb", [128, 8, 2], F32)
            sT = sbt(ph, "sT", [128, 8, 2], F32)
            srep = sbt(ph, "srep", [128, 8, 128], F32)
            adab_sb = sbt(ph, "adab_sb", [128, 48], F32)
            wb = [sbt(ph, "adaw%d" % i, [128, 8, D], F32) for i in range(2)]
            wb_ds = [dsem(), dsem()]
            bb = [sbt(ph, "adabb%d" % i, [128, D], F32) for i in range(4)]
            ada_ps = pst(ph, "ada_ps", [128, 48, 2], F32)
            bc_ps = pst(ph, "bc_ps", [128, D], F32)
            t1 = dma(SP, [], dc0, cT_sb[:], cT.rearrange("p (k m) -> p k m", m=2))
            t2 = dma(SP, [], dc0, adab_sb[:], adabT[:, :])
            for i_, s_ in enumerate((2, 3, 4, 5)):
                dma(SP, [], dc0, bb[i_][:], ada_b[s_ * D:(s_ + 1) * D].partition_broadcast(128))
            tc_all = (dc0, dc0.n)
            t_idf = tc_all
            t_s = op(ACT, [tc_all], nc.scalar.activation, out=sT[:], in_=cT_sb[:], func=AF.Silu)
            t_rep = op(DVE, [t_s], nc.vector.tensor_copy, out=srep[:], in_=sT[:, :, 0:1].to_broadcast([128, 8, 128]))
            ada_w_v = ada_w.rearrange("(k p) n -> p k n", p=128)
            wfree = [None, None]
            t_last_mm = None
            bc_toks = {}
            for s in range(6):
                b = s % 2
                tl = dma(SP, [wfree[b]], wb_ds[b], wb[b][:], ada_w_v[:, :, s * D:(s + 1) * D])
                PE.wait([tl, t_s])
                for cc in range(8):
                    for k in range(8):
                        mm = nc.tensor.matmul(ada_ps[:, s * 8 + cc, :], lhsT=wb[b][:, k, cc * 128:(cc + 1) * 128], rhs=sT[:, k, :],
                                              start=(k == 0), stop=(k == 7))
                t_last_mm = PE.fin(mm)
                if s in (2, 3, 4, 5):
                    PE.wait([t_rep])
                    for hf in range(2):
                        for k in range(8):
                            mm = nc.tensor.matmul(bc_ps[:, hf * 512:(hf + 1) * 512], lhsT=srep[:, k, :], rhs=wb[b][:, k, hf * 512:(hf + 1) * 512],
                                                  start=(k == 0), stop=(k == 7))
                    t_last_mm = PE.fin(mm)
                    dst = {2: g1bc, 3: sh2bc, 4: sc2bc, 5: g2bc}[s]
                    if s == 4:
                        tt = op(DVE, [t_last_mm, tc_all], nc.vector.scalar_tensor_tensor, out=dst[:], in0=bc_ps[:], scalar=1.0, in1=bb[s - 2][:], op0=ALU.add, op1=ALU.add)
                    else:
                        tt = op(DVE, [t_last_mm, tc_all], nc.vector.tensor_tensor, out=dst[:], in0=bc_ps[:], in1=bb[s - 2][:], op=ALU.add)
                    bc_toks[s] = tt
                    PE.wait([tt])
                wfree[b] = t_last_mm
            t_ada = op(DVE, [t_last_mm, tc_all], nc.vector.tensor_tensor, out=adaT[:], in0=ada_ps[:],
                       in1=adab_sb[:].unsqueeze(2).to_broadcast([128, 48, 2]), op=ALU.add)
            tm = []
            tm.append(op(DVE, [t_ada], nc.vector.tensor_scalar, out=modv[:, 0, :], in0=adaT[:, 8:16, 0], scalar1=1.0, scalar2=None, op0=ALU.add))
            tm.append(op(DVE, [t_ada], nc.vector.tensor_copy, out=modv[:, 1, :], in_=adaT[:, 0:8, 0]))
            tm.append(op(DVE, [t_ada], nc.vector.tensor_scalar, out=modv[:, 2, :], in0=adaT[:, 8:16, 1], scalar1=1.0, scalar2=None, op0=ALU.add))
            tm.append(op(DVE, [t_ada], nc.vector.tensor_copy, out=modv[:, 3, :], in_=adaT[:, 0:8, 1]))
            tm.append(op(DVE, [t_ada], nc.vector.tensor_scalar, out=modv[:, 4, :], in0=adaT[:, 32:40, 0], scalar1=1.0, scalar2=None, op0=ALU.add))
            t_mod = op(DVE, [t_ada], nc.vector.tensor_copy, out=modv[:, 5, :], in_=adaT[:, 24:32, 0])
            dump("d_ada", adaT[:].rearrange("p a b -> p (a b)"), [t_mod])
            dump("d_g1bc", g1bc[:], [t_mod])
            barrier()

        lg_all = sbt(es, "lg_all", [128, NOWN, NE], F32)
        m8_all = sbt(es, "m8_all", [128, NOWN, 8], F32)
        maskall = sbt(es, "maskall", [128, NOWN, NE], F32)
        mid = ExitStack()
        mixT = sbt(mid, "mixT", [128, 8, NTOK], BF16)
        front = ExitStack()
        KT = sbt(front, "KT", [128, 4, 2816], BF16)
        V = sbt(front, "V", [128, NT_ALL, 512], BF16)
        QT = sbt(front, "QT", [128, 4, NTOK], BF16)

        with ExitStack() as ph:
            w_in_bf = sbt(ph, "w_in_bf", [128, 8, 2560], BF16)
            wsT_bf = sbt(ph, "wsT_bf", [128, 4, 128], BF16)
            lngbc = sbt(ph, "lngbc", [128, 512], F32)
            lnbbc = sbt(ph, "lnbbc", [128, 512], F32)
            bsbc = sbt(ph, "bsbc", [128, 512], F32)
            dw = dsem()
            dcc = dsem()
            w_in_v = w_in.rearrange("(k p) n -> p k n", p=128)
            for k in range(8):
                t_win = dma(POOL, [], dw, w_in_bf[:, k, :], w_in_v[:, k, :])
            t_win = dma(POOL, [], dw, wsT_bf[:], wsT.rearrange("p (g i) -> p g i", g=4))
            dma(SP, [], dcc, lngbc[:], sgu_ln_g.partition_broadcast(128))
            dma(SP, [], dcc, lnbbc[:], sgu_ln_b.partition_broadcast(128))
            t_cc = dma(SP, [], dcc, bsbc[:], sgu_bv.partition_broadcast(128))

            xt = [sbt(ph, "xt%d" % i, [128, D], F32) for i in range(2)]
            xt_ds = [dsem(), dsem()]
            xt_free = [None, None]
            xn = [sbt(ph, "xn%d" % i, [128, D], BF16) for i in range(2)]
            xn_free = [None, None]
            st = [sbt(ph, "st%d" % i, [128, 12], F32) for i in range(2)]
            mv = [sbt(ph, "mv%d" % i, [128, 2], F32) for i in range(2)]
            rs = [sbt(ph, "rs%d" % i, [128, 2], F32) for i in range(2)]
            tp_ps = Ring([pst(ph, "tp_ps%d" % i, [128, 8, 128], BF16) for i in range(2)])
            hT = [sbt(ph, "hT%d" % i, [128, 8, 512], BF16) for i in range(2)]
            hT_free = [None, None]
            acc_ps = Ring([pst(ph, "acc_ps%d" % i, [128, 512], F32) for i in range(4)])
            sg_ps = pst(ph, "sg_ps", [128, 4, 128], F32)
            sg_free = [None]
            Gg = [sbt(ph, "Gg%d" % i, [128, 512], F32) for i in range(2)]
            Gn = [sbt(ph, "Gn%d" % i, [128, 512], F32) for i in range(2)]
            Gb = [sbt(ph, "Gb%d" % i, [128, 512], BF16) for i in range(2)]
            Gfree = [None, None]
            gst = [sbt(ph, "gst%d" % i, [128, 6], F32) for i in range(2)]
            gmv = [sbt(ph, "gmv%d" % i, [128, 2], F32) for i in range(2)]
            grs = [sbt(ph, "grs%d" % i, [128, 2], F32) for i in range(2)]
            stmp = [sbt(ph, "stmp%d" % i, [128, 512], F32) for i in range(2)]
            stmp_free = [None, None]
            evq = [0]

            def evac_copy(deps, out_, in_, scale=None, func=None):
                evq[0] += 1
                if func is not None:
                    return op(ACT, deps, nc.scalar.activation, out=out_, in_=in_, func=func)
                if evq[0] % 2 == 0:
                    if scale is None:
                        return op(ACT, deps, nc.scalar.activation, out=out_, in_=in_, func=AF.Identity)
                    return op(ACT, deps, nc.scalar.activation, out=out_, in_=in_, func=AF.Identity, scale=float(scale))
                if scale is None:
                    return op(DVE, deps, nc.vector.tensor_copy, out=out_, in_=in_)
                return op(DVE, deps, nc.vector.tensor_scalar, out=out_, in0=in_, scalar1=float(scale), scalar2=None, op0=ALU.mult)

            groups = [list(range(g * 4, min(g * 4 + 4, NT_ALL))) for g in range(6)]
            ti_glob = 0
            gcount = 0
            for gi, tiles in enumerate(groups):
                hb = gi % 2
                ntok = 128 * len(tiles)
                ev_toks = []
                for sl, ti in enumerate(tiles):
                    b = ti_glob % 2
                    ti_glob += 1
                    is_ctx = ti >= NEXT_T
                    tl = dma(SP, [xt_free[b]], xt_ds[b], xt[b][:], xe[ti * 128:(ti + 1) * 128, :])
                    trs = ln_stats(xt[b], D, [tl], st[b], mv[b], rs[b])
                    tn = op(DVE, [trs, xn_free[b]], nc.vector.tensor_scalar, out=xn[b][:], in0=xt[b][:], scalar1=mv[b][:, 0:1], scalar2=rs[b][:, 0:1],
                            op0=ALU.subtract, op1=ALU.mult)
                    xt_free[b] = tn
                    pi, pt, pfree = tp_ps.next()
                    PE.wait(flat([tn, pfree, t_idb]))
                    for k in range(8):
                        mm = nc.tensor.transpose(pt[:, k, :], xn[b][:, k * 128:(k + 1) * 128], identb[:])
                    ttp = PE.fin(mm)
                    xn_free[b] = ttp
                    msc, msh = (2, 3) if is_ctx else (0, 1)
                    ACT.wait(flat([ttp, hT_free[hb], t_mod]))
                    for k in range(8):
                        a = nc.scalar.activation(out=hT[hb][:, k, sl * 128:(sl + 1) * 128], in_=pt[:, k, :], func=AF.Identity,
                                                 scale=modv[:, msc, k:k + 1], bias=modv[:, msh, k:k + 1])
                    tev = ACT.fin(a)
                    tp_ps.rel(pi, tev)
                    ev_toks.append(tev)
                hready = ev_toks[-1]
                tok0 = tiles[0] * 128
                mm_last = None
                for c in range(4):
                    ai, ap_, afree = acc_ps.next()
                    PE.wait(flat([hready, afree, t_win]))
                    for k in range(8):
                        mm = nc.tensor.matmul(ap_[:, 0:ntok], lhsT=w_in_bf[:, k, 512 + c * 128:512 + (c + 1) * 128], rhs=hT[hb][:, k, 0:ntok],
                                              start=(k == 0), stop=(k == 7))
                    tmm = PE.fin(mm)
                    te = evac_copy([tmm], KT[:, c, tok0:tok0 + ntok], ap_[:, 0:ntok])
                    acc_ps.rel(ai, te)
                for sl, ti in enumerate(tiles):
                    ai, ap_, afree = acc_ps.next()
                    PE.wait(flat([hready, afree, t_win]))
                    for k in range(8):
                        mm = nc.tensor.matmul(ap_[:, :], lhsT=hT[hb][:, k, sl * 128:(sl + 1) * 128], rhs=w_in_bf[:, k, 1024:1536],
                                              start=(k == 0), stop=(k == 7))
                    tmm = PE.fin(mm)
                    te = evac_copy([tmm], V[:, ti, :], ap_[:, :])
                    acc_ps.rel(ai, te)
                    mm_last = tmm
                own = [(sl, ti) for sl, ti in enumerate(tiles) if OWN0 <= ti < OWN0 + NOWN]
                if own:
                    s0 = own[0][0] * 128
                    nown = 128 * len(own)
                    o0 = (own[0][1] - OWN0) * 128
                    for c in range(4):
                        ai, ap_, afree = acc_ps.next()
                        PE.wait(flat([hready, afree]))
                        for k in range(8):
                            mm = nc.tensor.matmul(ap_[:, 0:nown], lhsT=w_in_bf[:, k, c * 128:(c + 1) * 128], rhs=hT[hb][:, k, s0:s0 + nown],
                                                  start=(k == 0), stop=(k == 7))
                        tmm = PE.fin(mm)
                        te = evac_copy([tmm], QT[:, c, o0:o0 + nown], ap_[:, 0:nown], scale=0.125)
                        acc_ps.rel(ai, te)
                    ut_toks = []
                    for c in range(4):
                        ai, ap_, afree = acc_ps.next()
                        PE.wait(flat([hready, afree]))
                        for k in range(8):
                            mm = nc.tensor.matmul(ap_[:, 0:nown], lhsT=w_in_bf[:, k, 1536 + c * 128:1536 + (c + 1) * 128], rhs=hT[hb][:, k, s0:s0 + nown],
                                                  start=(k == 0), stop=(k == 7))
                        tmm = PE.fin(mm)
                        te = evac_copy([tmm], mixT[:, 4 + c, o0:o0 + nown], ap_[:, 0:nown], func=AF.Gelu_apprx_tanh)
                        acc_ps.rel(ai, te)
                        ut_toks.append(te)
                    for sl, ti in own:
                        gb = gcount % 2
                        gcount += 1
                        ot = (ti - OWN0) * 128
                        ai, ap_, afree = acc_ps.next()
                        PE.wait(flat([hready, afree]))
                        for k in range(8):
                            mm = nc.tensor.matmul(ap_[:, :], lhsT=hT[hb][:, k, sl * 128:(sl + 1) * 128], rhs=w_in_bf[:, k, 2048:2560],
                                                  start=(k == 0), stop=(k == 7))
                        tmm = PE.fin(mm)
                        mm_last = tmm
                        tg = op(ACT, [tmm, Gfree[gb]], nc.scalar.activation, out=Gg[gb][:], in_=ap_[:, :], func=AF.Gelu_apprx_tanh)
                        acc_ps.rel(ai, tg)
                        trs = ln_stats(Gg[gb], 512, [tg], gst[gb], gmv[gb], grs[gb])
                        t1_ = op(DVE, [trs], nc.vector.tensor_scalar, out=Gn[gb][:], in0=Gg[gb][:], scalar1=gmv[gb][:, 0:1], scalar2=grs[gb][:, 0:1],
                                 op0=ALU.subtract, op1=ALU.mult)
                        t2_ = op(DVE, [t1_, t_cc], nc.vector.tensor_tensor, out=Gn[gb][:], in0=Gn[gb][:], in1=lngbc[:], op=ALU.mult)
                        t3_ = op(DVE, [t2_], nc.vector.tensor_tensor, out=Gb[gb][:], in0=Gn[gb][:], in1=lnbbc[:], op=ALU.add)
                        PE.wait(flat([t3_, sg_free[0], t_win]))
                        for g in range(4):
                            mm = nc.tensor.matmul(sg_ps[:, g, :], lhsT=Gb[gb][:, g * 128:(g + 1) * 128], rhs=wsT_bf[:, g, :], start=True, stop=True)
                        tsg = PE.fin(mm)
                        Gfree[gb] = tsg
                        t4_ = op(DVE, [tsg, stmp_free[gb], t_cc], nc.vector.tensor_tensor, out=stmp[gb][:], in0=sg_ps[:].rearrange("p g i -> p (g i)"), in1=bsbc[:], op=ALU.add)
                        sg_free[0] = t4_
                        t5_ = op(DVE, [t4_, ut_toks], nc.vector.tensor_tensor, out=mixT[:, 4:8, ot:ot + 128],
                                 in0=stmp[gb][:].rearrange("p (g i) -> p g i", g=4), in1=mixT[:, 4:8, ot:ot + 128], op=ALU.mult)
                        stmp_free[gb] = t5_
                hT_free[hb] = mm_last
            barrier()
            if debug:
                dump("d_KT", KT[:].rearrange("p a b -> p (a b)"), [], BF16, [128, 4 * 2816])
                dump("d_V", V[:].rearrange("p a b -> p (a b)"), [], BF16, [128, 22 * 512])
                dump("d_QT", QT[:].rearrange("p a b -> p (a b)"), [], BF16, [128, 4 * 2048])
                dump("d_sguT", mixT[:, 4:8, :].rearrange("p a b -> p (a b)"), [], BF16, [128, 4 * 2048])
                barrier()

        with ExitStack() as ph:
            bgen = sbt(ph, "bgen", [128, 8, 832], F32)
            bsp = sbt(ph, "bsp", [128, 8, 1024], F32)
            db = dsem()
            dbs = dsem()
            t_bgen = dma(SP, [], db, bgen[:], bias_gen.rearrange("p (h k) -> p h k", h=8))
            dz = dsem()
            S_ps = Ring([pst(ph, "S_ps%d" % i, [128, 1024], F32) for i in range(2)])
            PT_ps = Ring([pst(ph, "PT_ps%d" % i, [128, 8, 128], BF16) for i in range(2)])
            O_ps = pst(ph, "O_ps", [128, 8, 64], F32)
            O_free = [None]
            AT_ps = pst(ph, "AT_ps", [128, 4, 128], BF16)
            AT_free = [None]
            S_sb = Ring([sbt(ph, "S_sb%d" % i, [128, 1024], F32) for i in range(2)])
            P_sb = Ring([sbt(ph, "P_sb%d" % i, [128, 1024], BF16) for i in range(2)])
            PT_sb = Ring([sbt(ph, "PT_sb%d" % i, [128, 8, 128], BF16) for i in range(3)])
            att_sb = Ring([sbt(ph, "att_sb%d" % i, [128, 512], BF16) for i in range(2)])
            mx_all = sbt(ph, "mx_all", [128, 128], F32)
            nmx_all = sbt(ph, "nmx_all", [128, 128], F32)
            rs_all = sbt(ph, "rs_all", [128, 128], F32)
            rinv_all = sbt(ph, "rinv_all", [128, 128], F32)

            items = []
            sp_load_tok = {}
            for p in PAIR_ORDER:
                for h in range(8):
                    items.append((p, h))
            N = len(items)
            st_ = {}
            bsp_free = [None]
            sp_next = [0]

            def issue_sp_load():
                i = sp_next[0]
                if i >= len(SP_ORDER):
                    return
                p = SP_ORDER[i]
                sp_load_tok[p] = dma(SP, [bsp_free[0]], dbs, bsp[:], bias_sp[i].rearrange("p (h k) -> p h k", h=8))
                sp_next[0] += 1

            issue_sp_load()
            for i_ in range(NTILE):
                dma(POOL, [], dz, Hs[i_ * TS:(i_ + 1) * TS, :], zeros[0:TS, :])
            t_zero = (dz, dz.n)

            def blocks_for(p):
                a, nrows = SPECIAL.get(p, (2 * p, 9))
                nk = nrows * 64
                bl = []
                for j in range(nk // 128):
                    bl.append((j * 128, 128, a // 2 + j))
                if nk % 128:
                    bl.append((nk - 64, 64, a // 2 + nk // 128))
                bl.append((nk, 128, NEXT_T))
                bl.append((nk + 128, 128, NEXT_T + 1))
                return a, nrows, nk, bl

            def emit_qk(i):
                p, h = items[i]
                a, nrows, nk, bl = blocks_for(p)
                nt = nk + 256
                c = h // 2
                pb = (h % 2) * 64
                si, sp_, sfree = S_ps.next()
                PE.wait(flat([sfree]))
                q_ap = QT[pb:pb + 64, c, p * 128:(p + 1) * 128]
                k0 = a * 64
                nc.tensor.matmul(sp_[:, 0:512], lhsT=q_ap, rhs=KT[pb:pb + 64, c, k0:k0 + 512], start=True, stop=True)
                nc.tensor.matmul(sp_[:, 512:nk], lhsT=q_ap, rhs=KT[pb:pb + 64, c, k0 + 512:k0 + nk], start=True, stop=True)
                tqk = PE.fin(nc.tensor.matmul(sp_[:, nk:nt], lhsT=q_ap, rhs=KT[pb:pb + 64, c, 2560:2816], start=True, stop=True))
                if p in SPECIAL:
                    btab = bsp[:, h, 0:nt]
                    bdep = sp_load_tok[p]
                else:
                    btab = bgen[:, h, 0:nt]
                    bdep = t_bgen
                bi, sb_, sbfree = S_sb.next()
                ts = op(DVE, [tqk, bdep, sbfree], nc.vector.tensor_tensor, out=sb_[:, 0:nt], in0=sp_[:, 0:nt], in1=btab, op=ALU.add)
                S_ps.rel(si, ts)
                col = i
                tm1 = op(DVE, [ts], nc.vector.tensor_reduce, out=mx_all[:, col:col + 1], in_=sb_[:, 0:nt], axis=AX.X, op=ALU.max)
                tm2 = op(DVE, [tm1], nc.vector.tensor_scalar, out=nmx_all[:, col:col + 1], in0=mx_all[:, col:col + 1], scalar1=-1.0, scalar2=None, op0=ALU.mult)
                pi, pp, pfree = P_sb.next()
                tp = op(ACT, [tm2, pfree], nc.scalar.activation, out=pp[:, 0:nt], in_=sb_[:, 0:nt], func=AF.Exp, bias=nmx_all[:, col:col + 1], scale=1.0,
                        accum_out=rs_all[:, col:col + 1])
                S_sb.rel(bi, tp)
                st_[i] = dict(pi=pi, pp=pp, tp=tp, bl=bl, last_sp=(p in SPECIAL and h == 7))
                if p in SPECIAL and h == 7:
                    bsp_free[0] = ts
                    issue_sp_load()

            def emit_tr(i):
                d = st_[i]
                ti_, tps, tfree = PT_ps.next()
                PE.wait(flat([d["tp"], tfree, t_idb]))
                for j, (off, ln, vt) in enumerate(d["bl"]):
                    mm = nc.tensor.transpose(tps[0:ln, j, :], d["pp"][:, off:off + ln], identb[:])
                ttr = PE.fin(mm)
                P_sb.rel(d["pi"], ttr)
                nb = len(d["bl"])
                qi, qsb, qfree = PT_sb.next()
                if i % 2 == 0:
                    te = op(ACT, [ttr, qfree], nc.scalar.activation, out=qsb[:, 0:nb, :], in_=tps[:, 0:nb, :], func=AF.Identity)
                else:
                    te = op(DVE, [ttr, qfree], nc.vector.tensor_copy, out=qsb[:, 0:nb, :], in_=tps[:, 0:nb, :])
                PT_ps.rel(ti_, te)
                d["qi"] = qi
                d["qsb"] = qsb
                d["te"] = te

            def emit_pv(i):
                p, h = items[i]
                d = st_[i]
                PE.wait(flat([d["te"], O_free[0] if h == 0 else None]))
                nb = len(d["bl"])
                for j, (off, ln, vt) in enumerate(d["bl"]):
                    mm = nc.tensor.matmul(O_ps[:, h, :], lhsT=d["qsb"][0:ln, j, :], rhs=V[0:ln, vt, h * 64:(h + 1) * 64], start=(j == 0), stop=(j == nb - 1))
                tpv = PE.fin(mm)
                PT_sb.rel(d["qi"], tpv)
                if h == 7:
                    c0 = i - 7
                    tr_ = op(DVE, [d["tp"]], nc.vector.reciprocal, out=rinv_all[:, c0:c0 + 8], in_=rs_all[:, c0:c0 + 8])
                    ai, asb, afree = att_sb.next()
                    ta = op(DVE, [tr_, tpv, afree], nc.vector.tensor_tensor, out=asb[:].rearrange("p (h d) -> p h d", h=8), in0=O_ps[:],
                            in1=rinv_all[:, c0:c0 + 8].unsqueeze(2).to_broadcast([128, 8, 64]), op=ALU.mult)
                    O_free[0] = ta
                    PE.wait(flat([ta, AT_free[0]]))
                    for c in range(4):
                        mm = nc.tensor.transpose(AT_ps[:, c, :], asb[:, c * 128:(c + 1) * 128], identb[:])
                    tat = PE.fin(mm)
                    att_sb.rel(ai, tat)
                    te = op(ACT, [tat], nc.scalar.activation, out=mixT[:, 0:4, p * 128:(p + 1) * 128], in_=AT_ps[:], func=AF.Identity)
                    AT_free[0] = te
                del st_[i]

            for i in range(N + 2):
                if i < N:
                    emit_qk(i)
                if 1 <= i <= N:
                    emit_tr(i - 1)
                if i >= 2:
                    emit_pv(i - 2)
            barrier()
            if debug:
                dump("d_attT", mixT[:, 0:4, :].rearrange("p a b -> p (a b)"), [], BF16, [128, 4 * 2048])
                barrier()
        front.close()

        hrb_ds = [dsem(), dsem()]
        acct_ds = [dsem(), dsem(), dsem()]
        with ExitStack() as ph:
            w_out_bf = sbt(ph, "w_out_bf", [128, 8, D], BF16)
            ln1gbc = sbt(ph, "ln1gbc", [128, D], F32)
            ln1bbc = sbt(ph, "ln1bbc", [128, D], F32)
            rw = sbt(ph, "rw", [128, 8, NE], F32)
            rbbc = sbt(ph, "rbbc", [128, NE], F32)
            bd_sb = sbt(ph, "bd_sb", [NE, D], F32)
            dw = dsem()
            dcc = dsem()
            t_wout = dma(POOL, [], dw, w_out_bf[:], w_out.rearrange("(k p) n -> p k n", p=128))
            dma(SP, [], dcc, ln1gbc[:], ln1_g.partition_broadcast(128))
            dma(SP, [], dcc, ln1bbc[:], ln1_b.partition_broadcast(128))
            dma(SP, [], dcc, rw[:], router_w.rearrange("(k p) n -> p k n", p=128))
            dma(SP, [], dcc, rbbc[:], router_b.partition_broadcast(128))
            t_cc = dma(SP, [], dcc, bd_sb[:], exp_b_down[:, :])
            xr = [sbt(ph, "xr%d" % i, [128, D], F32) for i in range(2)]
            xr_ds = [dsem(), dsem()]
            xr_free = [None, None]
            mix_ps = pst(ph, "mix_ps", [128, D], F32)
            mix_free = [None]
            tr_ps = pst(ph, "tr_ps", [128, 8, 128], F32)
            tr_free = [None]
            lg_ps = pst(ph, "lg_ps", [128, NE], F32)
            lg_free = [None]
            ct_ps = pst(ph, "ct_ps", [NE, 128], F32)
            ct_free = [None]
            bd_ps = pst(ph, "bd_ps", [128, D], F32)
            bd_free = [None]
            wk = sbt(ph, "wk", [128, D], F32)
            wk_free = [None]
            x1n = sbt(ph, "x1n", [128, D], F32)
            x1n_free = [None]
            h2f = sbt(ph, "h2f", [128, 8, 128], F32)
            h2f_free = [None]
            hrf = sbt(ph, "hrf", [128, D], F32)
            hrb = [sbt(ph, "hrb%d" % i, [128, D], BF16) for i in range(2)]
            hrb_free = [None, None]
            acct = [sbt(ph, "acct%d" % i, [128, D], F32) for i in range(3)]
            acct_free = [None, None, None]
            st = sbt(ph, "st4", [128, 12], F32)
            mv = sbt(ph, "mv4", [128, 2], F32)
            rs = sbt(ph, "rs4", [128, 2], F32)
            st2 = sbt(ph, "st5", [128, 12], F32)
            mv2 = sbt(ph, "mv5", [128, 2], F32)
            rs2 = sbt(ph, "rs5", [128, 2], F32)
            rt = sbt(ph, "rt", [128, 4], F32)
            ex = sbt(ph, "ex", [128, NE], F32)
            combT = sbt(ph, "combT", [NE, 128], F32)
            combT_free = [None]
            ex_free = [None]

            ones1 = sbt(ph, "ones1", [1, 128], F32)
            rb1 = sbt(ph, "rb1", [1, NE], F32)
            bdt = sbt(ph, "bdt", [128, D], F32)
            bdt_free = [None]
            t_ones = op(DVE, [], nc.vector.memset, ones1[:], 1.0)
            drb = dsem()
            t_rb1 = dma(SP, [], drb, rb1[:], router_b.rearrange("(o n) -> o n", o=1))
            s1 = {}

            def outproj(t):
                b = t % 2
                tok = slice(t * 128, (t + 1) * 128)
                tl = dma(SP, [xr_free[b]], xr_ds[b], xr[b][:], xe[(OWN0 + t) * 128:(OWN0 + t + 1) * 128, :])
                PE.wait(flat([mix_free[0], t_wout]))
                for hf in range(2):
                    for k in range(8):
                        mm = nc.tensor.matmul(mix_ps[:, hf * 512:(hf + 1) * 512], lhsT=mixT[:, k, tok], rhs=w_out_bf[:, k, hf * 512:(hf + 1) * 512],
                                              start=(k == 0), stop=(k == 7))
                tmm = PE.fin(mm)
                s1[t] = dict(tl=tl, tmm=tmm)

            def stage1a(t):
                b = t % 2
                b3 = t % 3
                tl = s1[t]["tl"]
                tmm = s1[t]["tmm"]
                ta = op(DVE, [tmm, wk_free[0]], nc.vector.tensor_tensor, out=wk[:], in0=mix_ps[:], in1=g1bc[:], op=ALU.mult)
                mix_free[0] = ta
                tb_ = op(DVE, [ta, tl], nc.vector.scalar_tensor_tensor, out=wk[:], in0=xr[b][:], scalar=float(ALPHA), in1=wk[:], op0=ALU.mult, op1=ALU.add)
                xr_free[b] = tb_
                trs = ln_stats(wk, D, [tb_], st, mv, rs)
                tc_ = op(DVE, [trs], nc.vector.tensor_scalar, out=wk[:], in0=wk[:], scalar1=mv[:, 0:1], scalar2=rs[:, 0:1], op0=ALU.subtract, op1=ALU.mult)
                td_ = op(DVE, [tc_, t_cc], nc.vector.tensor_tensor, out=wk[:], in0=wk[:], in1=ln1gbc[:], op=ALU.mult)
                te_ = op(DVE, [td_], nc.vector.tensor_tensor, out=wk[:], in0=wk[:], in1=ln1bbc[:], op=ALU.add)
                tf_ = op(ACT, [te_, acct_free[b3]], nc.scalar.activation, out=acct[b3][:], in_=wk[:], func=AF.Identity, scale=float(ALPHA))
                trs2 = ln_stats(wk, D, [te_], st2, mv2, rs2)
                tg_ = op(DVE, [trs2, x1n_free[0]], nc.vector.tensor_scalar, out=x1n[:], in0=wk[:], scalar1=mv2[:, 0:1], scalar2=rs2[:, 0:1],
                         op0=ALU.subtract, op1=ALU.mult)
                wk_free[0] = [tf_, tg_]
                hr1 = op(DVE, [tg_], nc.vector.tensor_tensor, out=hrf[:], in0=x1n[:], in1=sc2bc[:], op=ALU.mult)
                hr2 = op(DVE, [hr1, hrb_free[b]], nc.vector.tensor_tensor, out=hrb[b][:], in0=hrf[:], in1=sh2bc[:], op=ALU.add)
                hrb_free[b] = dma(SP, [hr2], hrb_ds[b], Hx[t * 128:(t + 1) * 128, :], hrb[b][:])
                s1[t].update(tf_=tf_, tg_=tg_, hr1=hr1)

            def stage1b(t):
                tg_ = s1[t]["tg_"]
                PE.wait(flat([tg_, tr_free[0], t_idf]))
                for k in range(8):
                    mm = nc.tensor.transpose(tr_ps[:, k, :], x1n[:, k * 128:(k + 1) * 128], identf[:])
                ttr = PE.fin(mm)
                x1n_free[0] = [ttr, s1[t]["hr1"]]
                ACT.wait(flat([ttr, h2f_free[0]]))
                for k in range(8):
                    a = nc.scalar.activation(out=h2f[:, k, :], in_=tr_ps[:, k, :], func=AF.Identity, scale=modv[:, 4, k:k + 1], bias=modv[:, 5, k:k + 1])
                th = ACT.fin(a)
                tr_free[0] = th
                PE.wait(flat([th, lg_free[0], t_cc, t_ones, t_rb1]))
                for k in range(8):
                    nc.tensor.matmul(lg_ps[:, :], lhsT=h2f[:, k, :], rhs=rw[:, k, :], start=(k == 0), stop=False)
                tlg = PE.fin(nc.tensor.matmul(lg_ps[:, :], lhsT=ones1[0:1, :], rhs=rb1[0:1, :], start=False, stop=True))
                h2f_free[0] = [tlg]
                r1 = op(ACT, [tlg], nc.scalar.activation, out=lg_all[:, t, :], in_=lg_ps[:], func=AF.Identity)
                lg_free[0] = r1
                s1[t]["r1"] = r1

            def stage2a(t):
                lg = lg_all[:, t, :]
                m8 = m8_all[:, t, :]
                msk = maskall[:, t, :]
                r1 = s1[t]["r1"]
                r2 = op(DVE, [r1], nc.vector.max, out=m8, in_=lg)
                r3 = op(DVE, [r2], nc.vector.tensor_scalar, out=rt[:, 0:1], in0=m8_all[:, t, 0:1], scalar1=-1.0, scalar2=None, op0=ALU.mult)
                r4 = op(ACT, [r3, ex_free[0]], nc.scalar.activation, out=ex[:], in_=lg, func=AF.Exp, bias=rt[:, 0:1], scale=1.0)
                r5 = op(DVE, [r2], nc.vector.tensor_scalar, out=msk, in0=lg, scalar1=m8_all[:, t, 3:4], scalar2=None, op0=ALU.is_ge)
                r6 = op(DVE, [r4, r5], nc.vector.tensor_tensor, out=ex[:], in0=ex[:], in1=msk, op=ALU.mult)
                r7 = op(DVE, [r6], nc.vector.tensor_reduce, out=rt[:, 1:2], in_=ex[:], axis=AX.X, op=ALU.add)
                r8 = op(DVE, [r7], nc.vector.reciprocal, out=rt[:, 2:3], in_=rt[:, 1:2])
                r9 = op(DVE, [r8], nc.vector.tensor_scalar, out=comb[:, t, :], in0=ex[:], scalar1=rt[:, 2:3], scalar2=None, op0=ALU.mult)
                ex_free[0] = r9
                PE.wait(flat([r9, ct_free[0]]))
                tct = PE.fin(nc.tensor.transpose(ct_ps[:, :], comb[:, t, :], identf[:]))
                tcc_ = op(ACT, [tct, combT_free[0]], nc.scalar.activation, out=combT[:], in_=ct_ps[:], func=AF.Identity)
                ct_free[0] = tcc_
                PE.wait(flat([tcc_, bd_free[0]]))
                for hf in range(2):
                    mm = nc.tensor.matmul(bd_ps[:, hf * 512:(hf + 1) * 512], lhsT=combT[:, :], rhs=bd_sb[:, hf * 512:(hf + 1) * 512], start=True, stop=True)
                tbd = PE.fin(mm)
                combT_free[0] = tbd
                s1[t]["tbd"] = tbd

            def stage2b(t):
                b3 = t % 3
                u1 = op(DVE, [s1[t]["tbd"], bdt_free[0]], nc.vector.tensor_tensor, out=bdt[:], in0=bd_ps[:], in1=g2bc[:], op=ALU.mult)
                bd_free[0] = u1
                u2 = op(DVE, [u1, s1[t]["tf_"]], nc.vector.tensor_tensor, out=acct[b3][:], in0=acct[b3][:], in1=bdt[:], op=ALU.add)
                bdt_free[0] = u2
                acct_free[b3] = dma(SP, [u2], acct_ds[b3], accd[t * 128:(t + 1) * 128, :], acct[b3][:])
                del s1[t]

            outproj(0)
            stage1a(0)
            outproj(1)
            for t in range(NOWN):
                stage1b(t)
                if t + 1 < NOWN:
                    stage1a(t + 1)
                if t + 2 < NOWN:
                    outproj(t + 2)
                if t >= 1:
                    stage2b(t - 1)
                stage2a(t)
            stage2b(NOWN - 1)
            SP.wait([(d_, d_.n) for d_ in hrb_ds + acct_ds])
            barrier()
        t_hx = [(d_, d_.n) for d_ in hrb_ds]
        t_accd = [(d_, d_.n) for d_ in acct_ds]
        mid.close()

        bcreg_slot = nc.gpsimd.alloc_register("bc_slot")
        nc.gpsimd.reg_mov(bcreg_slot, NSLOT - 1)
        bcreg_w = nc.gpsimd.alloc_register("bc_w")
        nc.gpsimd.reg_mov(bcreg_w, NE * 1024 - 1)
        bcreg_b = nc.gpsimd.alloc_register("bc_b")
        nc.gpsimd.reg_mov(bcreg_b, NE * 128 - 1)
        with ExitStack() as ph:
            cst_sb = sbt(ph, "cst_sb", [128, CW], F32)
            dcs = dsem()
            t_cst = dma(SP, [], dcs, cst_sb[:], cst[:, :])
            U128 = cst_sb[:, 0:128]
            ONES = cst_sb[:, 128:256]
            USTR = cst_sb[0:NE, 256:288]
            jrow = cst_sb[:, C_J:C_J + NTILE]
            erow = cst_sb[:, C_E:C_E + NE]
            pk = cst_sb[:, C_PK:C_PK + 8]
            bigp = cst_sb[:, C_BIG:C_BIG + 1]
            prow = cst_sb[:, C_P:C_P + 1]
            cum = sbt(ph, "cum", [128, NOWN, NE], F32)
            pos_ps = pst(ph, "pos_ps", [128, NOWN, NE], F32)
            cnt_ps = pst(ph, "cnt_ps", [128, NE], F32)
            ntT_ps = pst(ph, "ntT_ps", [NE, 128], F32)
            ts_ps = pst(ph, "ts_ps", [128, NE], F32)
            cnt_sb = sbt(ph, "cnt_sb", [128, NE], F32)
            nt = sbt(ph, "nt", [128, NE], F32)
            ntT = sbt(ph, "ntT", [NE, 128], F32)
            tstart = sbt(ph, "tstart", [128, NE], F32)
            tend = sbt(ph, "tend", [128, NE], F32)
            slotf = sbt(ph, "slotf", [128, NOWN, NE], F32)
            oh = sbt(ph, "oh", [128, NOWN, NE], F32)
            tmpm = sbt(ph, "tmpm", [128, NOWN, NE], F32)
            slotsel = sbt(ph, "slotsel", [128, NOWN, 4], F32)
            gate = sbt(ph, "gate", [128, NOWN, 4], F32)
            A3 = sbt(ph, "A3", [128, NTILE, NE], F32)
            B3 = sbt(ph, "B3", [128, NTILE, NE], F32)
            used = sbt(ph, "used", [128, NTILE], F32)
            texp = sbt(ph, "texp", [128, NTILE], F32)
            cfill = sbt(ph, "cfill", [128, NTILE], F32)
            wf = sbt(ph, "wf", [128, NTILE, 8], F32)
            bf_ = sbt(ph, "bf_", [128, NTILE], F32)

            tq = op(DVE, [], nc.vector.memset, cum[:, 0, :], 0.0)
            for t in range(1, NOWN):
                tq = op(DVE, [tq], nc.vector.tensor_tensor, out=cum[:, t, :], in0=cum[:, t - 1, :], in1=maskall[:, t - 1, :], op=ALU.add)
            PE.wait(flat([tq, t_cst]))
            for t in range(NOWN):
                nc.tensor.matmul(pos_ps[:, t, :], lhsT=U128, rhs=maskall[:, t, :], start=True, stop=False)
                mm = nc.tensor.matmul(pos_ps[:, t, :], lhsT=ONES, rhs=cum[:, t, :], start=False, stop=True)
            nc.tensor.matmul(cnt_ps[:, :], lhsT=ONES, rhs=cum[:, NOWN - 1, :], start=True, stop=False)
            tpos = PE.fin(nc.tensor.matmul(cnt_ps[:, :], lhsT=ONES, rhs=maskall[:, NOWN - 1, :], start=False, stop=True))
            q = op(DVE, [tpos], nc.vector.tensor_copy, out=cnt_sb[:], in_=cnt_ps[:])
            q = op(DVE, [q], nc.vector.tensor_scalar, out=nt[:], in0=cnt_sb[:], scalar1=0.0, scalar2=None, op0=ALU.is_gt)
            for thr in [float(TS * i_) for i_ in range(1, 2048 // TS + 1) if TS * i_ < 2048]:
                q = op(DVE, [q], nc.vector.scalar_tensor_tensor, out=nt[:], in0=cnt_sb[:], scalar=thr, in1=nt[:], op0=ALU.is_gt, op1=ALU.add)
            PE.wait(flat([q, t_idf]))
            tnt = PE.fin(nc.tensor.transpose(ntT_ps[:, :], nt[:], identf[:]))
            q2 = op(DVE, [tnt], nc.vector.tensor_copy, out=ntT[:], in_=ntT_ps[:])
            PE.wait(flat([q2]))
            tts = PE.fin(nc.tensor.matmul(ts_ps[:, :], lhsT=ntT[:, :], rhs=USTR, start=True, stop=True))
            q = op(DVE, [tts], nc.vector.tensor_copy, out=tstart[:], in_=ts_ps[:])
            q = op(DVE, [q], nc.vector.tensor_tensor, out=tend[:], in0=tstart[:], in1=nt[:], op=ALU.add)
            q = op(DVE, [q], nc.vector.scalar_tensor_tensor, out=slotf[:], in0=tstart[:].unsqueeze(1).to_broadcast([128, NOWN, NE]), scalar=float(TS), in1=pos_ps[:],
                   op0=ALU.mult, op1=ALU.add)
            for k in range(4):
                q = op(DVE, [q], nc.vector.tensor_tensor, out=oh[:], in0=lg_all[:], in1=m8_all[:, :, k:k + 1].to_broadcast([128, NOWN, NE]), op=ALU.is_equal)
                q = op(DVE, [q], nc.vector.tensor_tensor, out=tmpm[:], in0=oh[:], in1=slotf[:], op=ALU.mult)
                q = op(DVE, [q], nc.vector.tensor_reduce, out=slotsel[:, :, k], in_=tmpm[:], axis=AX.X, op=ALU.add)
                q = op(DVE, [q], nc.vector.tensor_tensor, out=tmpm[:], in0=oh[:], in1=comb[:], op=ALU.mult)
                q = op(DVE, [q], nc.vector.tensor_reduce, out=gate[:, :, k], in_=tmpm[:], axis=AX.X, op=ALU.add)
            q = op(DVE, [q], nc.vector.tensor_copy, out=slot_i[:], in_=slotsel[:].rearrange("p t k -> p (t k)"))
            q = op(DVE, [q], nc.vector.tensor_scalar, out=gate2[:], in0=gate[:], scalar1=float(1.0 / 1.702), scalar2=None, op0=ALU.mult)
            q = op(DVE, [q], nc.vector.tensor_tensor, out=A3[:], in0=tstart[:].unsqueeze(1).to_broadcast([128, NTILE, NE]),
                   in1=jrow.unsqueeze(2).to_broadcast([128, NTILE, NE]), op=ALU.is_le)
            q = op(DVE, [q], nc.vector.tensor_tensor, out=B3[:], in0=tend[:].unsqueeze(1).to_broadcast([128, NTILE, NE]),
                   in1=jrow.unsqueeze(2).to_broadcast([128, NTILE, NE]), op=ALU.is_gt)
            q = op(DVE, [q], nc.vector.tensor_tensor, out=A3[:], in0=A3[:], in1=B3[:], op=ALU.mult)
            q = op(DVE, [q], nc.vector.tensor_reduce, out=used[:], in_=A3[:], axis=AX.X, op=ALU.add)
            q = op(DVE, [q], nc.vector.tensor_tensor, out=B3[:], in0=A3[:], in1=erow.unsqueeze(1).to_broadcast([128, NTILE, NE]), op=ALU.mult)
            q = op(DVE, [q], nc.vector.tensor_reduce, out=texp[:], in_=B3[:], axis=AX.X, op=ALU.add)
            q = op(DVE, [q], nc.vector.tensor_scalar, out=cfill[:], in0=used[:], scalar1=-1.0, scalar2=1.0, op0=ALU.mult, op1=ALU.add)
            q = op(DVE, [q], nc.vector.tensor_scalar, out=cfill[:], in0=cfill[:], scalar1=bigp, scalar2=None, op0=ALU.mult)
            q = op(DVE, [q], nc.vector.tensor_scalar, out=bf_[:], in0=texp[:], scalar1=1024.0, scalar2=None, op0=ALU.mult)
            q = op(DVE, [q], nc.vector.tensor_tensor, out=wf[:], in0=bf_[:].unsqueeze(2).to_broadcast([128, NTILE, 8]),
                   in1=pk.unsqueeze(1).to_broadcast([128, NTILE, 8]), op=ALU.add)
            q = op(DVE, [q], nc.vector.tensor_tensor, out=wf[:], in0=wf[:], in1=used[:].unsqueeze(2).to_broadcast([128, NTILE, 8]), op=ALU.mult)
            q = op(DVE, [q], nc.vector.tensor_tensor, out=wf[:], in0=wf[:], in1=cfill[:].unsqueeze(2).to_broadcast([128, NTILE, 8]), op=ALU.add)
            q = op(DVE, [q], nc.vector.tensor_copy, out=widx_i[:], in_=wf[:].rearrange("p j k -> p (j k)"))
            q = op(DVE, [q], nc.vector.tensor_scalar, out=bf_[:], in0=texp[:], scalar1=128.0, scalar2=prow, op0=ALU.mult, op1=ALU.add)
            q = op(DVE, [q], nc.vector.tensor_tensor, out=bf_[:], in0=bf_[:], in1=used[:], op=ALU.mult)
            q = op(DVE, [q], nc.vector.tensor_tensor, out=bf_[:], in0=bf_[:], in1=cfill[:], op=ALU.add)
            t_meta = op(DVE, [q], nc.vector.tensor_copy, out=bidx_i[:], in_=bf_[:])
            barrier()

        with ExitStack() as ph:
            hb = [sbt(ph, "hb%d" % i, [128, D], BF16) for i in range(2)]
            hb_ds = [dsem() for _ in range(2)]
            hb_free = [None] * 2
            dscat = [dsem(), dsem()]
            for t in range(NOWN):
                i = t % 2
                tl = dma(SP, [t_hx, hb_free[i]], hb_ds[i], hb[i][:], Hx[t * 128:(t + 1) * 128, :])
                POOL.wait(flat([tl, t_meta, t_zero]))
                for k in range(4):
                    nc.gpsimd.indirect_dma_start(out=Hs[:, :], out_offset=bass.IndirectOffsetOnAxis(ap=slot_i[:, t * 4 + k:t * 4 + k + 1], axis=0),
                                                 in_=hb[i][:], in_offset=None, bounds_check=bcreg_slot, oob_is_err=False).then_inc(dscat[i].sem, 16)
                    dscat[i].n += 16
                hb_free[i] = (dscat[i], dscat[i].n)
            t_scat = [(d_, d_.n) for d_ in dscat]

            wg = [sbt(ph, "wg%d" % i, [128, 8 * 2048], BF16) for i in range(3)]
            wd = [sbt(ph, "wd%d" % i, [128, 8 * 1024], BF16) for i in range(2)]
            bgt = [sbt(ph, "bgt%d" % i, [128, 16], F32) for i in range(3)]
            wg_ds = [dsem() for _ in range(3)]
            wd_ds = [dsem() for _ in range(2)]
            wg_free = [None] * 3
            wd_free = [None] * 2
            wg_tok = {}
            wd_tok = {}
            hrow = [sbt(ph, "hrow%d" % i, [128, D], BF16) for i in range(3)]
            hrow_ds = [dsem() for _ in range(3)]
            hrow_free = [None] * 3
            hT = [sbt(ph, "hTm%d" % i, [128, 8, TS], BF16) for i in range(2)]
            hT_free = [None, None]
            hT_tok = {}
            actT = [sbt(ph, "actT%d" % i, [128, 8, TS], BF16) for i in range(2)]
            actT_free = [None, None]
            tp_ps = Ring([pst(ph, "tp5_ps%d" % i, [128, 8, 128], BF16) for i in range(2)])
            g_ps = Ring([pst(ph, "g_ps%d" % i, [128, 512], F32) for i in range(2)])
            l_ps = Ring([pst(ph, "l_ps%d" % i, [128, 512], F32) for i in range(2)])
            y_ps = Ring([pst(ph, "y_ps%d" % i, [128, 512], F32) for i in range(2)])
            gc = Ring([sbt(ph, "gc%d" % i, [128, TS], F32) for i in range(2)])
            sg = Ring([sbt(ph, "sg%d" % i, [128, TS], F32) for i in range(2)])
            lc = Ring([sbt(ph, "lc%d" % i, [128, TS], F32) for i in range(2)])
            ystage = [sbt(ph, "ystage%d" % i, [128, D], F32) for i in range(2)]
            ys_free = [None, None]
            dys = [dsem(), dsem()]
            ysn = [0]
            pend = {}
            bias_tok = {}
            nrow = [0]

            ORDER = []
            lo_, hi_ = 0, NTILE - 1
            while lo_ <= hi_:
                if len(ORDER) % 3 == 2:
                    ORDER.append(hi_)
                    hi_ -= 1
                else:
                    ORDER.append(lo_)
                    lo_ += 1
            assert sorted(ORDER) == list(range(NTILE))

            def load_wg(j):
                b = j % 3
                tj = ORDER[j]
                POOL.wait(flat([wg_free[b], t_meta]))
                for k in range(8):
                    nc.gpsimd.indirect_dma_start(out=wg[b][:, k * 2048:(k + 1) * 2048], out_offset=None, in_=wgu_rows[:, :],
                                                 in_offset=bass.IndirectOffsetOnAxis(ap=widx_i[:, tj * 8 + k:tj * 8 + k + 1], axis=0),
                                                 bounds_check=bcreg_w, oob_is_err=False).then_inc(wg_ds[b].sem, 16)
                    wg_ds[b].n += 16
                nc.gpsimd.indirect_dma_start(out=bgt[b][:], out_offset=None, in_=bgu_rows[:, :],
                                             in_offset=bass.IndirectOffsetOnAxis(ap=bidx_i[:, tj:tj + 1], axis=0),
                                             bounds_check=bcreg_b, oob_is_err=False).then_inc(wg_ds[b].sem, 16)
                wg_ds[b].n += 16
                wg_tok[j] = (wg_ds[b], wg_ds[b].n)

            def load_wd(j):
                b = j % 2
                tj = ORDER[j]
                POOL.wait(flat([wd_free[b], t_meta]))
                for k in range(8):
                    nc.gpsimd.indirect_dma_start(out=wd[b][:, k * 1024:(k + 1) * 1024], out_offset=None, in_=wd_rows[:, :],
                                                 in_offset=bass.IndirectOffsetOnAxis(ap=widx_i[:, tj * 8 + k:tj * 8 + k + 1], axis=0),
                                                 bounds_check=bcreg_w, oob_is_err=False).then_inc(wd_ds[b].sem, 16)
                    wd_ds[b].n += 16
                wd_tok[j] = (wd_ds[b], wd_ds[b].n)

            def emit_rows(j):
                jb = j % 2
                tev = None
                for sidx in range(NSUB):
                    i = nrow[0] % 3
                    nrow[0] += 1
                    r0 = ORDER[j] * TS + sidx * 128
                    tl = dma(SP, [t_scat, hrow_free[i]], hrow_ds[i], hrow[i][:], Hs[r0:r0 + 128, :])
                    pi, tpp, tpfree = tp_ps.next()
                    PE.wait(flat([tl, tpfree, t_idb]))
                    for k in range(8):
                        mm = nc.tensor.transpose(tpp[:, k, :], hrow[i][:, k * 128:(k + 1) * 128], identb[:])
                    ttp = PE.fin(mm)
                    hrow_free[i] = ttp
                    tev = op(ACT, [ttp, hT_free[jb] if sidx == 0 else None], nc.scalar.activation, out=hT[jb][:, :, sidx * 128:(sidx + 1) * 128], in_=tpp[:], func=AF.Identity)
                    tp_ps.rel(pi, tev)
                hT_tok[j] = tev

            def emit_gu(j):
                b = j % 3
                jb = j % 2
                ab = j % 2
                tbias = op(DVE, [wg_tok[j]], nc.vector.tensor_scalar, out=bgt[b][:, 8:16], in0=bgt[b][:, 8:16], scalar1=1.0, scalar2=None, op0=ALU.add)
                last_tok = None
                for jc in range(8):
                    gi, gp, gfree = g_ps.next()
                    li, lp, lfree = l_ps.next()
                    PE.wait(flat([wg_tok[j], hT_tok[j], gfree, lfree]))
                    for k in range(8):
                        nc.tensor.matmul(gp[:, 0:TS], lhsT=wg[b][:, k * 2048 + jc * 128:k * 2048 + (jc + 1) * 128], rhs=hT[jb][:, k, :], start=(k == 0), stop=(k == 7))
                    for k in range(8):
                        mm = nc.tensor.matmul(lp[:, 0:TS], lhsT=wg[b][:, k * 2048 + 1024 + jc * 128:k * 2048 + 1024 + (jc + 1) * 128], rhs=hT[jb][:, k, :], start=(k == 0), stop=(k == 7))
                    tmm = PE.fin(mm)
                    bg_ap = bgt[b][:, jc:jc + 1]
                    bl_ap = bgt[b][:, 8 + jc:9 + jc]
                    ci, gct, gcfree = gc.next()
                    a1 = op(DVE, [tmm, tbias, gcfree], nc.vector.tensor_scalar, out=gct[:, 0:TS], in0=gp[:, 0:TS], scalar1=bg_ap, scalar2=7.0, op0=ALU.add, op1=ALU.min)
                    g_ps.rel(gi, a1)
                    xi, sgt, sgfree = sg.next()
                    a2 = op(ACT, [a1, sgfree], nc.scalar.activation, out=sgt[:, 0:TS], in_=gct[:, 0:TS], func=AF.Silu, scale=1.702)
                    gc.rel(ci, a2)
                    yi, lct, lcfree = lc.next()
                    a3 = op(DVE, [tmm, tbias, lcfree], nc.vector.tensor_scalar, out=lct[:, 0:TS], in0=lp[:, 0:TS], scalar1=bl_ap, scalar2=8.0, op0=ALU.add, op1=ALU.min)
                    l_ps.rel(li, a3)
                    a6 = op(DVE, [a2, a3, actT_free[ab] if jc == 0 else None], nc.vector.scalar_tensor_tensor, out=actT[ab][:, jc, :], in0=lct[:, 0:TS], scalar=-6.0, in1=sgt[:, 0:TS],
                            op0=ALU.max, op1=ALU.mult)
                    sg.rel(xi, a6)
                    lc.rel(yi, a6)
                    last_tok = a6
                hT_free[jb] = tmm
                pend[j] = last_tok
                wg_free[b] = [tmm, last_tok]
                if j + 3 < NTILE:
                    load_wg(j + 3)

            def emit_down(j):
                b = j % 2
                ab = j % 2
                tmm = None
                for sidx in range(NSUB):
                    yb = ysn[0] % 2
                    ysn[0] += 1
                    evs = []
                    for hc in range(2):
                        yi, yp, yfree = y_ps.next()
                        PE.wait(flat([pend[j], yfree, wd_tok[j]]))
                        for jc in range(8):
                            mm = nc.tensor.matmul(yp[:, :], lhsT=actT[ab][:, jc, sidx * 128:(sidx + 1) * 128], rhs=wd[b][:, jc * 1024 + hc * 512:jc * 1024 + (hc + 1) * 512],
                                                  start=(jc == 0), stop=(jc == 7))
                        tmm = PE.fin(mm)
                        ev = op(DVE, [tmm, ys_free[yb]], nc.vector.tensor_tensor, out=ystage[yb][:, hc * 512:(hc + 1) * 512], in0=yp[:, :], in1=g2bc[:, hc * 512:(hc + 1) * 512], op=ALU.mult)
                        y_ps.rel(yi, ev)
                        evs.append(ev)
                    r0 = ORDER[j] * TS + sidx * 128
                    ys_free[yb] = dma(SP, evs, dys[yb], Ys[r0:r0 + 128, :], ystage[yb][:])
                actT_free[ab] = tmm
                wd_free[b] = tmm
                del pend[j]
                if j + 2 < NTILE:
                    load_wd(j + 2)

            load_wg(0)
            load_wd(0)
            load_wg(1)
            load_wd(1)
            load_wg(2)
            emit_rows(0)
            for j in range(NTILE):
                emit_gu(j)
                if j + 1 < NTILE:
                    emit_rows(j + 1)
                emit_down(j)
            t_ys = [(d_, d_.n) for d_ in dys]
            SP.wait(t_ys)
            barrier()

        with ExitStack() as ph:
            ln2gbc = sbt(ph, "ln2gbc", [128, D], F32)
            ln2bbc = sbt(ph, "ln2bbc", [128, D], F32)
            dcc = dsem()
            dma(SP, [], dcc, ln2gbc[:], ln2_g.partition_broadcast(128))
            t_cc = dma(SP, [], dcc, ln2bbc[:], ln2_b.partition_broadcast(128))
            st = [sbt(ph, "st6_%d" % i, [128, 12], F32) for i in range(2)]
            mv = [sbt(ph, "mv6_%d" % i, [128, 2], F32) for i in range(2)]
            rs = [sbt(ph, "rs6_%d" % i, [128, 2], F32) for i in range(2)]
            ob = [sbt(ph, "ob%d" % i, [128, D], F32) for i in range(2)]
            ob_free = [None, None]
            accs = [sbt(ph, "accs%d" % i, [128, D], F32) for i in range(2)]
            accs_ds = [dsem(), dsem()]
            accs_free = [None, None]
            yk = [[sbt(ph, "yk%d_%d" % (i, k), [128, D], F32) for k in range(4)] for i in range(3)]
            yk_ds = [dsem(), dsem(), dsem()]
            yk_free = [None, None, None]
            ssum = [sbt(ph, "ssum%d" % i, [128, D], F32) for i in range(2)]
            ssum_free = [None, None]
            dout = [dsem(), dsem()]
            tyk = {}

            def issue_gather(t):
                b = t % 3
                POOL.wait(flat([yk_free[b], t_ys]))
                for k in range(4):
                    nc.gpsimd.indirect_dma_start(out=yk[b][k][:], out_offset=None, in_=Ys[:, :],
                                                 in_offset=bass.IndirectOffsetOnAxis(ap=slot_i[:, t * 4 + k:t * 4 + k + 1], axis=0),
                                                 bounds_check=bcreg_slot, oob_is_err=False).then_inc(yk_ds[b].sem, 16)
                    yk_ds[b].n += 16
                tyk[t] = (yk_ds[b], yk_ds[b].n)

            issue_gather(0)
            issue_gather(1)
            issue_gather(2)
            trs_t = {}

            def stage_a(t):
                b = t % 2
                tla = dma(ACT, [t_accd, accs_free[b]], accs_ds[b], accs[b][:], accd[t * 128:(t + 1) * 128, :])
                yb = t % 3
                q = op(DVE, [tyk[t], tla, ssum_free[b]], nc.vector.scalar_tensor_tensor, out=ssum[b][:], in0=yk[yb][0][:], scalar=gate2[:, t, 0:1], in1=accs[b][:], op0=ALU.mult, op1=ALU.add)
                accs_free[b] = q
                for k in range(1, 4):
                    q = op(DVE, [q], nc.vector.scalar_tensor_tensor, out=ssum[b][:], in0=yk[yb][k][:], scalar=gate2[:, t, k:k + 1], in1=ssum[b][:], op0=ALU.mult, op1=ALU.add)
                yk_free[yb] = q
                trs_t[t] = ln_stats(ssum[b], D, [q], st[b], mv[b], rs[b])
                if t + 3 < NOWN:
                    issue_gather(t + 3)

            def stage_b(t):
                b = t % 2
                trs = trs_t.pop(t)
                tn_ = op(DVE, [trs], nc.vector.scalar_tensor_tensor, out=rs[b][:, 1:2], in0=mv[b][:, 0:1], scalar=-1.0, in1=rs[b][:, 0:1], op0=ALU.mult, op1=ALU.mult)
                t1_ = op(ACT, [tn_, ob_free[b]], nc.scalar.activation, out=ob[b][:], in_=ssum[b][:], func=AF.Identity, scale=rs[b][:, 0:1], bias=rs[b][:, 1:2])
                ssum_free[b] = t1_
                t2_ = op(DVE, [t1_, t_cc], nc.vector.tensor_tensor, out=ob[b][:], in0=ob[b][:], in1=ln2gbc[:], op=ALU.mult)
                t3_ = op(DVE, [t2_], nc.vector.tensor_tensor, out=ob[b][:], in0=ob[b][:], in1=ln2bbc[:], op=ALU.add)
                ob_free[b] = dma(SP, [t3_], dout[b], out[t * 128:(t + 1) * 128, :], ob[b][:])

            stage_a(0)
            for t in range(NOWN):
                if t + 1 < NOWN:
                    stage_a(t + 1)
                stage_b(t)
            SP.wait([(d_, d_.n) for d_ in dout])
    return nc


def _bias_tables(rpb):
    c = np.arange(64)
    cs = np.clip(c - 8, 0, 48)
    kc = np.arange(64)
    colvalid = (kc[None, :] >= cs[:, None]) & (kc[None, :] < cs[:, None] + 16)
    dcidx = np.clip(kc[None, :] - c[:, None] + 15, 0, 30)

    def table(r0, lr0, a, nrows, width):
        T = np.full((2, 64, 8, width), NEG, np.float32)
        for i in range(2):
            r = r0 + lr0 + i
            rs = min(max(r - 4, 0), 120)
            for w in range(nrows):
                kr = r0 - 4 + a + w
                if kr < rs or kr >= rs + 8:
                    continue
                dr = kr - r + 7
                vals = rpb[:, dr, :][:, dcidx]
                vals = np.where(colvalid[None], vals, np.float32(NEG))
                T[i, :, :, w * 64:(w + 1) * 64] = vals.transpose(1, 0, 2)
            T[i, :, :, nrows * 64:nrows * 64 + 256] = 0.0
        return T.reshape(128, 8 * width)

    gen = table(32, 8, 8, 9, 832)
    sp = {}
    for q in range(4):
        r0 = 32 * q
        sp[q] = np.stack([table(r0, 2 * p, SPECIAL[p][0], SPECIAL[p][1], 1024) for p in SP_ORDER])
    return gen, sp


def _consts(core):
    c = np.zeros((128, CW), np.float32)
    p = np.arange(128)
    c[:, 0:128] = (p[:, None] < p[None, :]).astype(np.float32)
    c[:, 128:256] = 1.0
    e = np.arange(NE)
    r = (e - 4 * core) % NE
    c[0:NE, 256:288] = (r[:, None] < r[None, :]).astype(np.float32)
    c[:, C_J:C_J + NTILE] = np.arange(NTILE)[None, :]
    c[:, C_E:C_E + NE] = e[None, :]
    c[:, C_PK:C_PK + 8] = np.arange(8)[None, :] * 128 + p[:, None]
    c[:, C_BIG] = np.where(p == 0, 0.0, BIGIDX)
    c[:, C_P] = p
    return c


_NC_CACHE = {}


def kernel(x, c, ctx, c_ctx, ada_w, ada_b, w_in, rpb, sgu_ln_g, sgu_ln_b, sgu_w, sgu_b,
           w_out, ln1_g, ln1_b, ln2_g, ln2_b, router_w, router_b,
           exp_w_gu, exp_b_gu, exp_w_down, exp_b_down, _debug=False):
    f = lambda a: np.ascontiguousarray(np.asarray(a, dtype=np.float32))
    x, c, ctx, c_ctx = f(x), f(c), f(ctx), f(c_ctx)
    ada_w0, ada_b0 = f(ada_w)[0], f(ada_b)[0]
    gen, sp = _bias_tables(f(rpb)[0])
    shared = {
        "ada_w": ada_w0,
        "adabT": np.ascontiguousarray(ada_b0.reshape(48, 128).T),
        "ada_b": ada_b0,
        "w_in": f(w_in)[0],
        "w_out": f(w_out)[0],
        "bias_gen": gen,
        "sgu_ln_g": f(sgu_ln_g)[0],
        "sgu_ln_b": f(sgu_ln_b)[0],
        "wsT": np.ascontiguousarray(f(sgu_w)[0].transpose(2, 0, 1).reshape(128, 512)),
        "sgu_bv": np.ascontiguousarray(f(sgu_b)[0].reshape(512)),
        "ln1_g": f(ln1_g)[0], "ln1_b": f(ln1_b)[0], "ln2_g": f(ln2_g)[0], "ln2_b": f(ln2_b)[0],
        "router_w": f(router_w)[0], "router_b": f(router_b)[0],
        "exp_w_gu": f(exp_w_gu)[0],
        "bgu_rows": np.ascontiguousarray(f(exp_b_gu)[0].reshape(NE, 16, 128).transpose(0, 2, 1).reshape(NE * 128, 16)),
        "zeros": np.zeros((512, D), dtype=ml_dtypes.bfloat16),
        "exp_w_down": f(exp_w_down)[0],
        "exp_b_down": f(exp_b_down)[0],
        "ident": np.eye(128, dtype=np.float32),
    }
    in_maps = []
    for j in range(NCORES):
        b, q = j // 4, j % 4
        r0 = 32 * q
        xg = x[b].reshape(128, 64, D)
        xe = np.zeros((NT_ALL * 128, D), np.float32)
        xev = xe[:NEXT_T * 128].reshape(40, 64, D)
        lo, hi = r0 - 4, r0 + 36
        slo, shi = max(lo, 0), min(hi, 128)
        xev[slo - lo:shi - lo] = xg[slo:shi]
        xe[NEXT_T * 128:] = ctx[b]
        cc = np.stack([c[b], c_ctx], axis=1)
        cT = np.ascontiguousarray(cc.reshape(8, 128, 2).transpose(1, 0, 2).reshape(128, 16))
        m = dict(shared)
        m["xe"] = xe
        m["cT"] = cT
        m["bias_sp"] = sp[q]
        m["cst"] = _consts(j)
        in_maps.append(m)
    key = bool(_debug)
    if key not in _NC_CACHE:
        _NC_CACHE[key] = build(debug=key)
    nc = _NC_CACHE[key]
    res = run_bass_kernel_spmd(nc, in_maps, core_ids=list(range(NCORES)))
    outs = [np.asarray(r["out"], dtype=np.float32) for r in res.results]
    full = np.concatenate(outs, axis=0).reshape(2, 8192, D)
    if _debug:
        return full, res.results
    return full
```
